# Optimizing a Trainium2 kernel written in Bass

```python
import math
import jax, jax.numpy as jnp
from jax import lax
import numpy as np

D_MODEL = 1024
BATCH = 8
SEQ = 2048
DEPTH = 1
DEC_BATCH = 128
DEC_SEQ = 4
PAST_LEN = 2048
PAGE_SIZE = 128

F32 = jnp.float32
HEAD_DIM = 64
A_HEADS = 8
B_HEADS = 4
B_VDIM = 2 * HEAD_DIM
IDX_HEADS = 8
IDX_DIM = 64
TOPK_MAX = 256
N_GROUPS = 4
EXPERTS_PER_GROUP = 8
N_EXPERTS = N_GROUPS * EXPERTS_PER_GROUP
TOP_K_INNER = 2
D_EXPERT = 256
ROPE_THETA = 10000.0
NORM_EPS = 1e-6
Q_BLOCK = 128
MIX_WIDTH = A_HEADS * HEAD_DIM + B_HEADS * B_VDIM
SPLITS = (A_HEADS * HEAD_DIM, A_HEADS * HEAD_DIM, A_HEADS * HEAD_DIM,
          B_HEADS * 2 * HEAD_DIM, B_HEADS * 2 * HEAD_DIM, B_HEADS * B_VDIM,
          IDX_HEADS * IDX_DIM, IDX_DIM, IDX_HEADS)
IN_WIDTH = sum(SPLITS)
SPLIT_POINTS = tuple(int(v) for v in np.cumsum(SPLITS)[:-1])

kernel_name = "hymba_dsa_diffattn_hiermoe_step"


def rms_norm(x, g):
    xf = x.astype(F32)
    y = xf * lax.rsqrt(jnp.mean(xf * xf, axis=-1, keepdims=True) + NORM_EPS)
    return (y * g.astype(F32)).astype(x.dtype)


def rope(x, pos):
    half = x.shape[-1] // 2
    inv = ROPE_THETA ** (-jnp.arange(half, dtype=F32) / half)
    ang = pos.astype(F32)[:, None] * inv[None, :]
    shape = (1, x.shape[1]) + (1,) * (x.ndim - 3) + (half,)
    cos = jnp.cos(ang).reshape(shape)
    sin = jnp.sin(ang).reshape(shape)
    xf = x.astype(F32)
    x1, x2 = xf[..., :half], xf[..., half:]
    return jnp.concatenate([x1 * cos - x2 * sin, x2 * cos + x1 * sin], axis=-1).astype(x.dtype)


def project(xn, w_in, pos, qn_a, kn_a, qn_b, kn_b):
    B, T, _ = xn.shape
    h = xn @ w_in
    aq, ak, av, bq, bk, bv, iq, ik, iw = jnp.split(h, SPLIT_POINTS, axis=-1)
    aq = rope(rms_norm(aq.reshape(B, T, A_HEADS, HEAD_DIM), qn_a), pos)
    ak = rope(rms_norm(ak.reshape(B, T, A_HEADS, HEAD_DIM), kn_a), pos)
    av = av.reshape(B, T, A_HEADS, HEAD_DIM)
    bq = rope(rms_norm(bq.reshape(B, T, B_HEADS, 2, HEAD_DIM), qn_b), pos)
    bk = rope(rms_norm(bk.reshape(B, T, B_HEADS, 2, HEAD_DIM), kn_b), pos)
    bv = bv.reshape(B, T, B_HEADS, B_VDIM)
    iq = rope(iq.reshape(B, T, IDX_HEADS, IDX_DIM), pos)
    ik = rope(ik, pos)
    iw = iw * (IDX_HEADS ** -0.5)
    return aq, ak, av, bq, bk, bv, iq, ik, iw


def sweep_query_blocks(fn, q_args, q_pos):
    T = q_pos.shape[0]
    blk = min(Q_BLOCK, T)
    nb = T // blk

    def split(a):
        return jnp.swapaxes(a.reshape((a.shape[0], nb, blk) + a.shape[2:]), 0, 1)

    out = lax.map(lambda args: fn(args[0], args[1]),
                  (tuple(split(a) for a in q_args), q_pos.reshape(nb, blk)))
    out = jnp.swapaxes(out, 0, 1)
    return out.reshape((out.shape[0], T) + out.shape[3:])


def contiguous_gather_fn(k, v):
    B, S = k.shape[:2]
    kf = k.reshape(B, S, -1)
    vf = v.reshape(B, S, -1)
    take = jax.vmap(lambda a, i: a[i])

    def gather(sel):
        shp = sel.shape + (A_HEADS, HEAD_DIM)
        return take(kf, sel).reshape(shp), take(vf, sel).reshape(shp)
    return gather


def paged_gather_fn(pool_k, pool_v, page_table, k_new, v_new):
    n_pool, ps = pool_k.shape[:2]
    past_len = page_table.shape[1] * ps
    kp = pool_k.reshape(n_pool * ps, -1)
    vp = pool_v.reshape(n_pool * ps, -1)
    B, Tn = k_new.shape[:2]
    kn = k_new.reshape(B, Tn, -1)
    vn = v_new.reshape(B, Tn, -1)
    take = jax.vmap(lambda a, i: a[i])

    def gather(sel):
        in_past = (sel < past_len)[..., None]
        s_past = jnp.minimum(sel, past_len - 1)
        phys = take(page_table, s_past // ps) * ps + s_past % ps
        s_new = jnp.clip(sel - past_len, 0, Tn - 1)
        kg = jnp.where(in_past, kp[phys], take(kn, s_new))
        vg = jnp.where(in_past, vp[phys], take(vn, s_new))
        shp = sel.shape + (A_HEADS, HEAD_DIM)
        return kg.reshape(shp), vg.reshape(shp)
    return gather


def paged_rows(pool, page_table):
    g = pool[page_table]
    return g.reshape((g.shape[0], g.shape[1] * g.shape[2]) + g.shape[3:])


def dsa_attention(q, iq, iw, ik, q_pos, gather_kv):
    L = ik.shape[1]
    topk = min(TOPK_MAX, L // 4)
    k_pos = jnp.arange(L)

    def block(args, qp):
        qb, iqb, iwb = args
        s = jnp.einsum('bqhd,bsd->bqhs', iqb, ik, preferred_element_type=F32) * (IDX_DIM ** -0.5)
        score = jnp.einsum('bqh,bqhs->bqs', iwb.astype(F32), jax.nn.relu(s))
        score = jnp.where(k_pos[None, None, :] <= qp[None, :, None], score, -jnp.inf)
        _, sel = lax.top_k(score, topk)
        kg, vg = gather_kv(sel)
        valid = sel <= qp[None, :, None]
        logits = jnp.einsum('bqhd,bqkhd->bqhk', qb, kg, preferred_element_type=F32) * (HEAD_DIM ** -0.5)
        logits = jnp.where(valid[:, :, None, :], logits, -jnp.inf)
        p = jax.nn.softmax(logits, axis=-1)
        return jnp.einsum('bqhk,bqkhd->bqhd', p.astype(vg.dtype), vg)
    return sweep_query_blocks(block, (q, iq, iw), q_pos)


def diff_attention(q, k, v, q_pos, lam):
    L = k.shape[1]
    k_pos = jnp.arange(L)

    def block(args, qp):
        (qb,) = args
        logits = jnp.einsum('bqhcd,bkhcd->bchqk', qb, k, preferred_element_type=F32) * (HEAD_DIM ** -0.5)
        mask = k_pos[None, None, None, None, :] <= qp[None, None, None, :, None]
        p = jax.nn.softmax(jnp.where(mask, logits, -jnp.inf), axis=-1)
        a = p[:, 0] - lam * p[:, 1]
        return jnp.einsum('bhqk,bkhe->bqhe', a.astype(v.dtype), v)
    return sweep_query_blocks(block, (q,), q_pos)


def hier_moe(x, w_rg, b_rg, w_re, b_re, w_g, w_u, w_d):
    n = x.shape[0]
    g_logits = jnp.einsum('nd,dg->ng', x, w_rg, preferred_element_type=F32) + b_rg.astype(F32)
    g_prob = jax.nn.softmax(g_logits, axis=-1)
    g_sel = jnp.argmax(g_logits, axis=-1)
    g_gate = jnp.take_along_axis(g_prob, g_sel[:, None], axis=-1)
    e_logits = (jnp.einsum('nd,de->ne', x, w_re, preferred_element_type=F32)
                + b_re.astype(F32)).reshape(n, N_GROUPS, EXPERTS_PER_GROUP)
    e_logits = jnp.take_along_axis(e_logits, g_sel[:, None, None], axis=1)[:, 0]
    e_prob = jax.nn.softmax(e_logits, axis=-1)
    top_p, top_i = lax.top_k(e_prob, TOP_K_INNER)
    gates = g_gate * top_p / jnp.sum(top_p, axis=-1, keepdims=True)
    ids = g_sel[:, None] * EXPERTS_PER_GROUP + top_i
    dense_gate = jnp.sum(jax.nn.one_hot(ids, N_EXPERTS, dtype=F32) * gates[..., None], axis=1)
    hg = jnp.einsum('nd,edf->nef', x, w_g)
    hu = jnp.einsum('nd,edf->nef', x, w_u)
    act = jax.nn.silu(hg) * hu * dense_gate[..., None].astype(x.dtype)
    return jnp.einsum('nef,efd->nd', act, w_d)


def merge_and_ffn(x, a_out, b_out, lam_init, subln, w_out, g_ffn, w_rg, b_rg, w_re, b_re, w_g, w_u, w_d):
    B, T, _ = x.shape
    b_out = rms_norm(b_out, subln) * (1.0 - lam_init)
    mixed = jnp.concatenate([a_out.reshape(B, T, -1), b_out.reshape(B, T, -1).astype(a_out.dtype)], axis=-1)
    h = x + mixed @ w_out
    f = hier_moe(rms_norm(h, g_ffn).reshape(B * T, -1), w_rg, b_rg, w_re, b_re, w_g, w_u, w_d)
    return h + f.reshape(B, T, -1)


def setup_inputs(seed: int = 0) -> dict:
    key = jax.random.key(seed)
    ks = jax.random.split(key, 32)
    n_pages = PAST_LEN // PAGE_SIZE
    n_used = DEC_BATCH * n_pages
    n_pool = n_used + max(1, n_used // 4)

    def nrm(k, shape, scale=1.0):
        return jax.random.normal(k, shape, F32) * scale

    def gain(k, shape):
        return 1.0 + 0.01 * jax.random.normal(k, shape, F32)

    page_table = jax.random.permutation(ks[7], n_pool)[:n_used].reshape(DEC_BATCH, n_pages).astype(jnp.int32)
    return {
        "x_prompt": nrm(ks[0], (BATCH, SEQ, D_MODEL)),
        "x_sample": nrm(ks[1], (DEC_BATCH, DEC_SEQ, D_MODEL)),
        "cache_a_k": nrm(ks[2], (DEPTH, n_pool, PAGE_SIZE, A_HEADS, HEAD_DIM)),
        "cache_a_v": nrm(ks[3], (DEPTH, n_pool, PAGE_SIZE, A_HEADS, HEAD_DIM)),
        "cache_idx_k": nrm(ks[4], (DEPTH, n_pool, PAGE_SIZE, IDX_DIM)),
        "cache_b_k": nrm(ks[5], (DEPTH, n_pool, PAGE_SIZE, B_HEADS, 2, HEAD_DIM)),
        "cache_b_v": nrm(ks[6], (DEPTH, n_pool, PAGE_SIZE, B_HEADS, B_VDIM)),
        "page_table": page_table,
        "g_mix": gain(ks[8], (DEPTH, D_MODEL)),
        "w_in": nrm(ks[9], (DEPTH, D_MODEL, IN_WIDTH), D_MODEL ** -0.5),
        "q_norm_a": gain(ks[10], (DEPTH, HEAD_DIM)),
        "k_norm_a": gain(ks[11], (DEPTH, HEAD_DIM)),
        "q_norm_b": gain(ks[12], (DEPTH, HEAD_DIM)),
        "k_norm_b": gain(ks[13], (DEPTH, HEAD_DIM)),
        "lambda_q1": nrm(ks[14], (DEPTH, HEAD_DIM), 0.1),
        "lambda_k1": nrm(ks[15], (DEPTH, HEAD_DIM), 0.1),
        "lambda_q2": nrm(ks[16], (DEPTH, HEAD_DIM), 0.1),
        "lambda_k2": nrm(ks[17], (DEPTH, HEAD_DIM), 0.1),
        "subln_b": gain(ks[18], (DEPTH, B_VDIM)),
        "w_out": nrm(ks[19], (DEPTH, MIX_WIDTH, D_MODEL), MIX_WIDTH ** -0.5),
        "g_ffn": gain(ks[20], (DEPTH, D_MODEL)),
        "w_router_group": nrm(ks[21], (DEPTH, D_MODEL, N_GROUPS), D_MODEL ** -0.5),
        "b_router_group": nrm(ks[22], (DEPTH, N_GROUPS), 0.01),
        "w_router_expert": nrm(ks[23], (DEPTH, D_MODEL, N_EXPERTS), D_MODEL ** -0.5),
        "b_router_expert": nrm(ks[24], (DEPTH, N_EXPERTS), 0.01),
        "w_exp_gate": nrm(ks[25], (DEPTH, N_EXPERTS, D_MODEL, D_EXPERT), D_MODEL ** -0.5),
        "w_exp_up": nrm(ks[26], (DEPTH, N_EXPERTS, D_MODEL, D_EXPERT), D_MODEL ** -0.5),
        "w_exp_down": nrm(ks[27], (DEPTH, N_EXPERTS, D_EXPERT, D_MODEL), D_EXPERT ** -0.5),
    }


def reference(x_prompt, x_sample, cache_a_k, cache_a_v, cache_idx_k, cache_b_k, cache_b_v, page_table,
              g_mix, w_in, q_norm_a, k_norm_a, q_norm_b, k_norm_b, lambda_q1, lambda_k1, lambda_q2, lambda_k2,
              subln_b, w_out, g_ffn, w_router_group, b_router_group, w_router_expert, b_router_expert,
              w_exp_gate, w_exp_up, w_exp_down):
    hp, hs = x_prompt, x_sample
    past_len = page_table.shape[1] * PAGE_SIZE
    pos_p = jnp.arange(hp.shape[1])
    pos_s = past_len + jnp.arange(hs.shape[1])
    ak_p, av_p, ik_p, bk_p, bv_p = [], [], [], [], []
    ak_s, av_s, ik_s, bk_s, bv_s = [], [], [], [], []
    for l in range(DEPTH):
        lam_init = 0.8 - 0.6 * math.exp(-0.3 * l)
        lam = (jnp.exp(jnp.sum(lambda_q1[l].astype(F32) * lambda_k1[l].astype(F32)))
               - jnp.exp(jnp.sum(lambda_q2[l].astype(F32) * lambda_k2[l].astype(F32))) + lam_init)
        ffn_args = (lam_init, subln_b[l], w_out[l], g_ffn[l], w_router_group[l], b_router_group[l],
                    w_router_expert[l], b_router_expert[l], w_exp_gate[l], w_exp_up[l], w_exp_down[l])
        aq, ak, av, bq, bk, bv, iq, ik, iw = project(rms_norm(hp, g_mix[l]), w_in[l], pos_p,
                                                     q_norm_a[l], k_norm_a[l], q_norm_b[l], k_norm_b[l])
        a_out = dsa_attention(aq, iq, iw, ik, pos_p, contiguous_gather_fn(ak, av))
        b_out = diff_attention(bq, bk, bv, pos_p, lam)
        hp = merge_and_ffn(hp, a_out, b_out, *ffn_args)
        ak_p.append(ak); av_p.append(av); ik_p.append(ik); bk_p.append(bk); bv_p.append(bv)
        aq, ak, av, bq, bk, bv, iq, ik, iw = project(rms_norm(hs, g_mix[l]), w_in[l], pos_s,
                                                     q_norm_a[l], k_norm_a[l], q_norm_b[l], k_norm_b[l])
        ik_all = jnp.concatenate([paged_rows(cache_idx_k[l], page_table).astype(ik.dtype), ik], axis=1)
        a_out = dsa_attention(aq, iq, iw, ik_all, pos_s,
                              paged_gather_fn(cache_a_k[l].astype(ak.dtype), cache_a_v[l].astype(av.dtype), page_table, ak, av))
        bk_all = jnp.concatenate([paged_rows(cache_b_k[l], page_table).astype(bk.dtype), bk], axis=1)
        bv_all = jnp.concatenate([paged_rows(cache_b_v[l], page_table).astype(bv.dtype), bv], axis=1)
        b_out = diff_attention(bq, bk_all, bv_all, pos_s, lam)
        hs = merge_and_ffn(hs, a_out, b_out, *ffn_args)
        ak_s.append(ak); av_s.append(av); ik_s.append(ik); bk_s.append(bk); bv_s.append(bv)
    return (hp, hs,
            jnp.stack(ak_p), jnp.stack(av_p), jnp.stack(ik_p), jnp.stack(bk_p), jnp.stack(bv_p),
            jnp.stack(ak_s), jnp.stack(av_s), jnp.stack(ik_s), jnp.stack(bk_s), jnp.stack(bv_s))
```

```python
import contextlib
import numpy as np
import concourse.bass as bass
import concourse.mybir as mybir
from concourse.bass_utils import run_bass_kernel_spmd

F32 = mybir.dt.float32
BF16 = mybir.dt.bfloat16
I32 = mybir.dt.int32
ALU = mybir.AluOpType
AF = mybir.ActivationFunctionType
AX = mybir.AxisListType

D = 1024
T = 2048
NT = 16
NS = 64
NB = 16
NPG = 16
INW = 3656
NEXP = 32
DE = 256
NEG = -30000.0
BIG = 1.0e30
EPS = 1e-6
TOPK = 256
NBIS = 14


class Buf:
    __slots__ = ("name", "last_w", "reads")

    def __init__(self, name):
        self.name = name
        self.last_w = None
        self.reads = []


class Sched:
    ENG = ("pe", "dve", "act", "pool", "sp")

    def __init__(self, nc, es, n_dma_sems=32, same_engine_sync=("pool", "dve", "act")):
        self.nc = nc
        self.count = {e: 0 for e in self.ENG}
        self.waited = {e: {} for e in self.ENG}
        self.n_dma_sems = n_dma_sems
        self.dma_rr = 0
        self.dma_cnt = [0] * n_dma_sems
        self.same_engine_sync = set(same_engine_sync)
        self.out_tokens = []
        self.sem = {}
        for e in self.ENG:
            self.sem["e_" + e] = es.enter_context(nc.semaphore("se_" + e))
        for i in range(n_dma_sems):
            self.sem["d%d" % i] = es.enter_context(nc.semaphore("sd%d" % i))
        self.eobj = {"pe": nc.tensor, "dve": nc.vector, "act": nc.scalar,
                     "pool": nc.gpsimd, "sp": nc.sync}

    def _deps(self, eng, reads, writes):
        deps = []
        for b in list(reads) + list(writes):
            if b.last_w is not None:
                deps.append(b.last_w)
        for b in writes:
            deps.extend(b.reads)
        waits = []
        for (sk, val, src_eng) in deps:
            if src_eng == eng and eng not in self.same_engine_sync and not sk.startswith("d"):
                continue
            if self.waited[eng].get(sk, 0) >= val:
                continue
            self.waited[eng][sk] = val
            waits.append((sk, val))
        return waits

    def _commit(self, tok, reads, writes):
        for b in reads:
            b.reads.append(tok)
            if len(b.reads) > 64:
                best = {}
                for t in b.reads:
                    if t[0] not in best or best[t[0]][1] < t[1]:
                        best[t[0]] = t
                b.reads = list(best.values())
        for b in writes:
            b.last_w = tok
            b.reads = []

    def _emit(self, eng, waits, fn, inc):
        e = self.eobj[eng]
        for (wk, wv) in waits:
            e.wait_ge(self.sem[wk], wv)
        fn(e).then_inc(self.sem[inc[0]], inc[1])

    def op(self, eng, fn, r=(), w=()):
        waits = self._deps(eng, r, w)
        self.count[eng] += 1
        tok = ("e_" + eng, self.count[eng], eng)
        self._emit(eng, waits, fn, ("e_" + eng, 1))
        self._commit(tok, r, w)
        return tok

    def dma(self, eng, fn, r=(), w=(), is_output=False):
        waits = self._deps(eng, r, w)
        i = self.dma_rr
        self.dma_rr = (self.dma_rr + 1) % self.n_dma_sems
        sk = "d%d" % i
        prev = self.dma_cnt[i]
        if prev > 0 and self.waited[eng].get(sk, 0) < prev:
            self.waited[eng][sk] = prev
            waits.append((sk, prev))
        self.dma_cnt[i] += 16
        tok = (sk, self.dma_cnt[i], eng)
        self._emit(eng, waits, fn, (sk, 16))
        self._commit(tok, r, w)
        if is_output:
            self.out_tokens.append(tok)
        return tok

    def barrier(self):
        for eng in self.ENG:
            e = self.eobj[eng]
            for f in self.ENG:
                if f == eng or self.count[f] == 0:
                    continue
                sk = "e_" + f
                if self.waited[eng].get(sk, 0) < self.count[f]:
                    self.waited[eng][sk] = self.count[f]
                    e.wait_ge(self.sem[sk], self.count[f])
            for i in range(self.n_dma_sems):
                sk = "d%d" % i
                if self.dma_cnt[i] > 0 and self.waited[eng].get(sk, 0) < self.dma_cnt[i]:
                    self.waited[eng][sk] = self.dma_cnt[i]
                    e.wait_ge(self.sem[sk], self.dma_cnt[i])

    def finish(self):
        seen = {}
        for (sk, val, _e) in self.out_tokens:
            seen[sk] = max(seen.get(sk, 0), val)
        for sk, val in seen.items():
            self.eobj["sp"].wait_ge(self.sem[sk], val)


def build(n_pool, phases=("A", "S", "B"), debug=False):
    nc = bass.Bass("TRN2", target_bir_lowering=False)

    def din(name, shape, dt=F32):
        return nc.dram_tensor(name, list(shape), dt, kind="ExternalInput").ap()

    def dout(name, shape, dt=F32):
        return nc.dram_tensor(name, list(shape), dt, kind="ExternalOutput").ap()

    xp = din("xp", [T, D]); xs = din("xs", [NS, D])
    cak = din("cak", [n_pool * 128, 512]); cav = din("cav", [n_pool * 128, 512])
    cbk = din("cbk", [n_pool * 128, 512]); cbv = din("cbv", [n_pool * 128, 512])
    cik = din("cik", [n_pool * 128, 64])
    ptab = din("ptab", [1, NB * NPG], I32)
    ptabT = din("ptabT", [1, NPG * NB], I32)
    g_mix = din("g_mix", [1, D]); w_in = din("w_in", [D, INW])
    gains = din("gains", [1, 4 * 64])
    lams = din("lams", [1, 4 * 64])
    subln = din("subln", [1, 128]); w_out = din("w_out", [D, D]); g_ffn = din("g_ffn", [1, D])
    w_r = din("w_r", [D, 36]); b_r = din("b_r", [1, 36])
    w_g = din("w_g", [NEXP, D, DE]); w_u = din("w_u", [NEXP, D, DE]); w_d = din("w_d", [NEXP, DE, D])
    rope_p = din("rope_p", [T, 128]); rope_s = din("rope_s", [NS, 128])
    c_ident = din("c_ident", [128, 128]); c_cm = din("c_cm", [128, 128]); c_cm30 = din("c_cm30", [128, 128])
    c_nbq = din("c_nbq", [64, 64]); c_nb30 = din("c_nb30", [64, 4]); c_bdiag = din("c_bdiag", [64, 64])
    c_bsel = din("c_bsel", [1, NB * 512]); c_i8 = din("c_i8", [64, 512])

    y_p = dout("y_p", [T, D]); y_s = dout("y_s", [NS, D])
    o_p = {k: dout("o%s_p" % k, [T, n]) for k, n in (("ak", 512), ("av", 512), ("ik", 64), ("bk", 512), ("bv", 512))}
    o_s = {k: dout("o%s_s" % k, [NS, n]) for k, n in (("ak", 512), ("av", 512), ("ik", 64), ("bk", 512), ("bv", 512))}
    mbuf = nc.dram_tensor("mbuf", [T + NS, D], BF16, kind="Internal").ap()
    if debug:
        dbg_mixed = dout("dbg_mixed", [NS, D])
        dbg_scs = dout("dbg_scs", [NS, 2052])
        dbg_mbs = dout("dbg_mbs", [NS, 2052])
        dbg_z = dout("dbg_z", [NS, 16])
    sgnbuf = nc.dram_tensor("sgnbuf", [NS, 8], F32, kind="Internal").ap()

    with contextlib.ExitStack() as es:
        S = Sched(nc, es)

        def sbt(stack, name, shape, dt):
            t = stack.enter_context(nc.sbuf_tensor(name, list(shape), dt))
            return t, Buf(name)

        def pst(stack, name, shape, dt):
            t = stack.enter_context(nc.psum_tensor(name, list(shape), dt))
            return t, Buf(name)

        def Vv(fn, r=(), w=()): return S.op("dve", fn, r, w)
        def Aa(fn, r=(), w=()): return S.op("act", fn, r, w)
        def Gg(fn, r=(), w=()): return S.op("pool", fn, r, w)
        def Pp(fn, r=(), w=()): return S.op("pe", fn, r, w)

        pP, pP_b = pst(es, "pP", [128, 2, 512], F32)
        pPb = [Buf("pP0"), Buf("pP1")]
        pT, pT_b = pst(es, "pT", [128, 1024], BF16)
        pS, _ = pst(es, "pS", [128, 2, 512], F32)
        pSb = [Buf("pS0"), Buf("pS1")]
        pO, _ = pst(es, "pO", [128, 3, 512], F32)
        pOb = [Buf("pO0"), Buf("pO1"), Buf("pO2")]

        identf, identf_b = sbt(es, "identf", [128, 128], F32)
        ident, ident_b = sbt(es, "ident", [128, 128], BF16)
        cm, cm_b = sbt(es, "cm", [128, 128], BF16)
        cm30, cm30_b = sbt(es, "cm30", [128, 128], F32)
        gmix_t, gmix_b = sbt(es, "gmix_t", [128, D], F32)
        Gt, Gt_b = sbt(es, "Gt", [128, 5, 64], F32)
        Gs, Gs_b = sbt(es, "Gs", [128, 5, 64], F32)
        subln_t, subln_b = sbt(es, "subln_t", [128, 128], F32)
        br_t, br_b = sbt(es, "br_t", [128, 36], F32)
        lam_t, lam_b = sbt(es, "lam_t", [128, 4, 64], F32)
        lamv, lamv_b = sbt(es, "lamv", [128, 4], F32)
        half_t, half_b = sbt(es, "half_t", [128, 1], F32)

        S.dma("sp", lambda e: e.dma_start(out=identf[:], in_=c_ident[:, :]), w=[identf_b])
        Vv(lambda e: e.tensor_copy(out=ident[:], in_=identf[:]), r=[identf_b], w=[ident_b])
        S.dma("pool", lambda e: e.dma_start(out=cm[:], in_=c_cm[:, :]), w=[cm_b])
        S.dma("sp", lambda e: e.dma_start(out=cm30[:], in_=c_cm30[:, :]), w=[cm30_b])
        S.dma("sp", lambda e: e.dma_start(out=gmix_t[:], in_=g_mix.partition_broadcast(128)), w=[gmix_b])
        S.dma("sp", lambda e: e.dma_start(out=subln_t[:], in_=subln.partition_broadcast(128)), w=[subln_b])
        S.dma("sp", lambda e: e.dma_start(out=br_t[:], in_=b_r.partition_broadcast(128)), w=[br_b])
        S.dma("sp", lambda e: e.dma_start(out=lam_t[:].rearrange("p a b -> p (a b)"), in_=lams.partition_broadcast(128)), w=[lam_b])
        Vv(lambda e: e.memset(Gt[:], 1.0), w=[Gt_b])
        Vv(lambda e: e.memset(half_t[:], 0.5), w=[half_b])
        S.dma("sp", lambda e: e.dma_start(out=Gt[:, 0:4, :].rearrange("p a b -> p (a b)"), in_=gains.partition_broadcast(128)), w=[Gt_b])
        Vv(lambda e: e.tensor_copy(out=Gs[:, :, 0:32], in_=Gt[:, :, 32:64]), r=[Gt_b], w=[Gs_b])
        Vv(lambda e: e.tensor_copy(out=Gs[:, :, 32:64], in_=Gt[:, :, 0:32]), r=[Gt_b], w=[Gs_b])
        Vv(lambda e: e.tensor_tensor(out=lam_t[:, 0, :], in0=lam_t[:, 0, :], in1=lam_t[:, 1, :], op=ALU.mult), r=[lam_b], w=[lam_b])
        Vv(lambda e: e.tensor_tensor(out=lam_t[:, 2, :], in0=lam_t[:, 2, :], in1=lam_t[:, 3, :], op=ALU.mult), r=[lam_b], w=[lam_b])
        Vv(lambda e: e.tensor_reduce(out=lamv[:, 0:1], in_=lam_t[:, 0, :], axis=AX.X, op=ALU.add), r=[lam_b], w=[lamv_b])
        Vv(lambda e: e.tensor_reduce(out=lamv[:, 1:2], in_=lam_t[:, 2, :], axis=AX.X, op=ALU.add), r=[lam_b], w=[lamv_b])
        Aa(lambda e: e.activation(out=lamv[:, 0:2], in_=lamv[:, 0:2], func=AF.Exp), r=[lamv_b], w=[lamv_b])
        Vv(lambda e: e.tensor_tensor(out=lamv[:, 2:3], in0=lamv[:, 1:2], in1=lamv[:, 0:1], op=ALU.subtract), r=[lamv_b], w=[lamv_b])
        Vv(lambda e: e.tensor_scalar(out=lamv[:, 3:4], in0=lamv[:, 2:3], scalar1=-0.2, scalar2=None, op0=ALU.add), r=[lamv_b], w=[lamv_b])
        neglam = lamv[:, 3:4]

        def rstd_from_ssq(t_ap, n_inv, bufs):
            Vv(lambda e: e.tensor_scalar(out=t_ap, in0=t_ap, scalar1=n_inv, scalar2=EPS, op0=ALU.mult, op1=ALU.add), r=bufs, w=bufs)
            Aa(lambda e: e.sqrt(out=t_ap, in_=t_ap), r=bufs, w=bufs)
            Vv(lambda e: e.reciprocal(out=t_ap, in_=t_ap), r=bufs, w=bufs)

        mixed_s, mixed_s_b = sbt(es, "mixed_s", [NS, D], BF16)

        hbuf_b = Buf("hbuf")
        mbuf_b = Buf("mbuf")

        sA = es.enter_context(contextlib.ExitStack())
        sq_s = {}
        for nm, shp, dt in (("aqT_s", [128, 4, NS], BF16), ("bqT_s", [128, 4, NS], BF16), ("iqT_s8", [64, 8, NS], BF16),
                            ("akT_s", [128, 4, NS], BF16), ("bkT_s", [128, 4, NS], BF16), ("ikT_s", [128, NS], BF16),
                            ("av_s", [NS, 512], BF16), ("bv_s", [NS, 512], BF16), ("wsg_s", [NS, 8], F32)):
            sq_s[nm] = sbt(es, nm, shp, dt)

        with contextlib.ExitStack() as pa:
            win_sb, win_b = sbt(pa, "win_sb", [128, 8, INW], BF16)
            akT, _ = sbt(pa, "akT", [128, 4, T], BF16)
            bkT, _ = sbt(pa, "bkT", [128, 4, T], BF16)
            ikT2, _ = sbt(pa, "ikT2", [128, T], BF16)
            av_sb, _ = sbt(pa, "av_sb", [128, NT, 8, 65], BF16)
            bv_sb, _ = sbt(pa, "bv_sb", [128, NT, 4, 129], BF16)
            kvb = [Buf("kv%d" % i) for i in range(NT)]
            xt, xt_b = sbt(pa, "xt", [128, D], F32)
            xn, xn_b = sbt(pa, "xn", [128, D], BF16)
            xnT, xnT_b = sbt(pa, "xnT", [128, 8, 128], BF16)
            hs, _ = sbt(pa, "hs", [128, 2, 512], F32)
            hsb = [Buf("hs0"), Buf("hs1")]
            kvout, kvout_b = sbt(pa, "kvout", [128, 1088], F32)
            qtm, qtm_b = sbt(pa, "qtm", [128, 5 * 512 + 128], BF16)
            qTs = [sbt(pa, "qT%d" % k, [128, 3, 4, 128], BF16) for k in range(2)]
            rp, rp_b = sbt(pa, "rp", [128, 128], F32)
            Ct, Ct_b = sbt(pa, "Ct", [128, 5, 64], F32)
            St, St_b = sbt(pa, "St", [128, 5, 64], F32)
            st8, st8_b = sbt(pa, "st8", [128, 16], F32)
            wsg, wsg_b = sbt(pa, "wsg", [128, 3, 8], F32)
            t2, t2_b = sbt(pa, "t2", [128, 512], F32)
            sc, sc_b = sbt(pa, "sc", [128, T], F32)
            rr, _ = sbt(pa, "rr", [128, 3, 512], BF16)
            rrb = [Buf("rr0"), Buf("rr1"), Buf("rr2")]
            dg, dg_b = sbt(pa, "dg", [128, 8, 128], BF16)
            mbs2 = [sbt(pa, "mb%d" % k, [128, T], BF16) for k in range(2)]
            bis, bis_b = sbt(pa, "bis", [128, 8], F32)
            PT, _ = sbt(pa, "PT", [128, 3, 512], BF16)
            PTb = [Buf("PT0"), Buf("PT1"), Buf("PT2")]
            mixed, mixed_b = sbt(pa, "mixed", [128, D], BF16)
            osm, osm_b = sbt(pa, "osm", [128, 8], F32)
            bo, bo_b = sbt(pa, "bo", [128, 2, 128], F32)

            iqs, iqs_b = sbt(pa, "iqs", [128, 512], F32)
            print("phaseA sbuf remaining", nc.sbuf_bytes_remaining)
            S.dma("pool", lambda e: e.dma_start(out=win_sb[:, :, 0:2048], in_=w_in.rearrange("(c p) n -> p c n", p=128)[:, :, 0:2048]), w=[win_b])
            S.dma("pool", lambda e: e.dma_start(out=win_sb[:, :, 2048:INW], in_=w_in.rearrange("(c p) n -> p c n", p=128)[:, :, 2048:INW]), w=[win_b])
            Vv(lambda e: e.memset(av_sb[:, :, :, 64:65], 1.0), w=kvb)
            Vv(lambda e: e.memset(bv_sb[:, :, :, 128:129], 1.0), w=kvb)

            def project_tile(x_src, rope_src, n, outs, row0, ti):
                S.dma("sp", lambda e: e.dma_start(out=xt[0:n, :], in_=x_src), w=[xt_b])
                S.dma("sp", lambda e: e.dma_start(out=rp[0:n, :], in_=rope_src), w=[rp_b])
                Aa(lambda e: e.activation(out=xn[0:n, :], in_=xt[0:n, :], func=AF.Square, accum_out=st8[0:n, 0:1]), r=[xt_b], w=[xn_b, st8_b])
                rstd_from_ssq(st8[0:n, 0:1], 1.0 / D, [st8_b])
                Vv(lambda e: e.scalar_tensor_tensor(out=xn[0:n, :], in0=xt[0:n, :], scalar=st8[0:n, 0:1], in1=gmix_t[0:n, :], op0=ALU.mult, op1=ALU.mult),
                   r=[xt_b, st8_b, gmix_b], w=[xn_b])
                for c in range(8):
                    Pp(lambda e, c=c: e.transpose(out=pT[:, c * 128:c * 128 + n], in_=xn[0:n, c * 128:(c + 1) * 128], identity=ident[0:n, 0:n]),
                       r=[xn_b, ident_b], w=[pT_b])
                Aa(lambda e: e.copy(out=xnT[:, :, 0:n], in_=pT[:, :].rearrange("p (c t) -> p c t", c=8)[:, :, 0:n]), r=[pT_b], w=[xnT_b])
                Vv(lambda e: e.tensor_tensor(out=Ct[0:n], in0=Gt[0:n], in1=rp[0:n, 0:64].unsqueeze(1).to_broadcast([n, 5, 64]), op=ALU.mult), r=[Gt_b, rp_b], w=[Ct_b])
                Vv(lambda e: e.tensor_tensor(out=St[0:n], in0=Gs[0:n], in1=rp[0:n, 64:128].unsqueeze(1).to_broadcast([n, 5, 64]), op=ALU.mult), r=[Gs_b, rp_b], w=[St_b])

                chunks = [("aq", 0, 512), ("ak", 512, 512), ("av", 1024, 512), ("bq", 1536, 512), ("bk", 2048, 512),
                          ("bv", 2560, 512), ("iq", 3072, 512), ("ikw", 3584, 72)]
                kvo = {"ak": 0, "ik": 512, "bk": 576}
                for ci, (nm, c0, cw) in enumerate(chunks):
                    pb = ci % 2
                    for c in range(8):
                        Pp(lambda e, c=c, pb=pb, c0=c0, cw=cw: e.matmul(out=pP[0:n, pb, 0:cw], lhsT=xnT[:, c, 0:n], rhs=win_sb[:, c, c0:c0 + cw], start=(c == 0), stop=(c == 7)),
                           r=[xnT_b, win_b], w=[pPb[pb]])
                    hsl = hs[0:n, pb, 0:cw]
                    if ci % 2 == 0:
                        Aa(lambda e, pb=pb, cw=cw, hsl=hsl: e.copy(out=hsl, in_=pP[0:n, pb, 0:cw]), r=[pPb[pb]], w=[hsb[pb]])
                    else:
                        Vv(lambda e, pb=pb, cw=cw, hsl=hsl: e.tensor_copy(out=hsl, in_=pP[0:n, pb, 0:cw]), r=[pPb[pb]], w=[hsb[pb]])
                    hb = hsb[pb]
                    if nm in ("av", "bv"):
                        S.dma("sp", lambda e, hsl=hsl, nm=nm: e.dma_start(out=outs[nm][row0:row0 + n, :], in_=hsl), r=[hb], is_output=True)
                        if ti is not None:
                            if nm == "av":
                                Vv(lambda e, hsl=hsl: e.tensor_copy(out=av_sb[:, ti, :, 0:64], in_=hsl.rearrange("p (h d) -> p h d", h=8)), r=[hb], w=[kvb[ti]])
                            else:
                                Vv(lambda e, hsl=hsl: e.tensor_copy(out=bv_sb[:, ti, :, 0:128], in_=hsl.rearrange("p (h d) -> p h d", h=4)), r=[hb], w=[kvb[ti]])
                        else:
                            dst = sq_s["av_s"] if nm == "av" else sq_s["bv_s"]
                            Vv(lambda e, hsl=hsl, dst=dst: e.tensor_copy(out=dst[0][:, :], in_=hsl), r=[hb], w=[dst[1]])
                        continue
                    if nm == "ikw":
                        Vv(lambda e: e.tensor_scalar(out=wsg[0:n, 0, :], in0=hs[0:n, pb, 64:72], scalar1=float(8 ** -0.5 * 64 ** -0.5), scalar2=None, op0=ALU.mult), r=[hb], w=[wsg_b])
                        Vv(lambda e: e.tensor_scalar(out=wsg[0:n, 2, :], in0=wsg[0:n, 0, :], scalar1=0.0, scalar2=2.0, op0=ALU.is_ge, op1=ALU.mult), r=[wsg_b], w=[wsg_b])
                        Vv(lambda e: e.tensor_scalar(out=wsg[0:n, 2, :], in0=wsg[0:n, 2, :], scalar1=-1.0, scalar2=None, op0=ALU.add), r=[wsg_b], w=[wsg_b])
                        Vv(lambda e: e.tensor_tensor(out=wsg[0:n, 1, :], in0=wsg[0:n, 0, :], in1=wsg[0:n, 2, :], op=ALU.mult), r=[wsg_b], w=[wsg_b])
                    sec = {"aq": 0, "ak": 1, "bq": 2, "bk": 3, "iq": 4, "ikw": 4}[nm]
                    nh = 1 if nm == "ikw" else 8
                    w_ = 64 * nh
                    src = hs[0:n, pb, 0:w_]
                    src3 = src.rearrange("p (h d) -> p h d", h=nh)
                    if sec < 4:
                        Aa(lambda e, src=src, w_=w_: e.activation(out=t2[0:n, 0:w_], in_=src, func=AF.Square), r=[hb], w=[t2_b])
                        Vv(lambda e, w_=w_, nh=nh: e.tensor_reduce(out=st8[0:n, 8:8 + nh], in_=t2[0:n, 0:w_].rearrange("p (h d) -> p h d", h=nh), axis=AX.X, op=ALU.add), r=[t2_b], w=[st8_b])
                        rstd_from_ssq(st8[0:n, 8:8 + nh], 1.0 / 64, [st8_b])
                        Vv(lambda e, src3=src3, nh=nh: e.tensor_tensor(out=src3, in0=src3, in1=st8[0:n, 8:8 + nh].unsqueeze(2).to_broadcast([n, nh, 64]), op=ALU.mult), r=[hb, st8_b], w=[hb])
                    t23 = t2[0:n, 0:w_].rearrange("p (h d) -> p h d", h=nh)
                    Gg(lambda e, src3=src3, t23=t23, nh=nh, sec=sec: e.tensor_tensor(out=t23[:, :, 0:32], in0=src3[:, :, 32:64], in1=St[0:n, sec, 0:32].unsqueeze(1).to_broadcast([n, nh, 32]), op=ALU.mult), r=[hb, St_b], w=[t2_b])
                    Gg(lambda e, src3=src3, t23=t23, nh=nh, sec=sec: e.tensor_tensor(out=t23[:, :, 32:64], in0=src3[:, :, 0:32], in1=St[0:n, sec, 32:64].unsqueeze(1).to_broadcast([n, nh, 32]), op=ALU.mult), r=[hb, St_b], w=[t2_b])
                    Vv(lambda e, src3=src3, nh=nh, sec=sec: e.tensor_tensor(out=src3, in0=src3, in1=Ct[0:n, sec, :].unsqueeze(1).to_broadcast([n, nh, 64]), op=ALU.mult), r=[hb, Ct_b], w=[hb])
                    if nm == "iq":
                        Vv(lambda e, src=src, w_=w_: e.tensor_tensor(out=iqs[0:n, 0:512], in0=src, in1=t2[0:n, 0:w_], op=ALU.add), r=[hb, t2_b], w=[iqs_b])
                        continue
                    if nm in ("ak", "bk"):
                        Vv(lambda e, src=src, nm=nm: e.tensor_tensor(out=kvout[0:n, kvo[nm]:kvo[nm] + 512], in0=src, in1=t2[0:n, 0:512], op=ALU.add), r=[hb, t2_b], w=[kvout_b])
                        q0 = 512 if nm == "ak" else 1536
                        Aa(lambda e, nm=nm, q0=q0: e.copy(out=qtm[0:n, q0:q0 + 512], in_=kvout[0:n, kvo[nm]:kvo[nm] + 512]), r=[kvout_b], w=[qtm_b])
                    elif nm == "ikw":
                        Vv(lambda e, src=src: e.tensor_tensor(out=kvout[0:n, 512:576], in0=src, in1=t2[0:n, 0:64], op=ALU.add), r=[hb, t2_b], w=[kvout_b])
                        Aa(lambda e: e.copy(out=qtm[0:n, 2560:2624], in_=kvout[0:n, 512:576]), r=[kvout_b], w=[qtm_b])
                        Aa(lambda e: e.copy(out=qtm[0:n, 2624:2688], in_=kvout[0:n, 512:576]), r=[kvout_b], w=[qtm_b])
                        Vv(lambda e: e.tensor_tensor(out=qtm[0:n, 2048:2560].rearrange("p (h d) -> p h d", h=8), in0=iqs[0:n, 0:512].rearrange("p (h d) -> p h d", h=8),
                                                      in1=wsg[0:n, 1, :].unsqueeze(2).to_broadcast([n, 8, 64]), op=ALU.mult), r=[iqs_b, wsg_b], w=[qtm_b])
                    else:
                        q0 = 0 if nm == "aq" else 1024
                        Vv(lambda e, src=src, q0=q0: e.tensor_tensor(out=qtm[0:n, q0:q0 + 512], in0=src, in1=t2[0:n, 0:512], op=ALU.add), r=[hb, t2_b], w=[qtm_b])
                for k, (o0, ow) in (("ak", (0, 512)), ("ik", (512, 64)), ("bk", (576, 512))):
                    S.dma("sp", lambda e, k=k, o0=o0, ow=ow: e.dma_start(out=outs[k][row0:row0 + n, :], in_=kvout[0:n, o0:o0 + ow]), r=[kvout_b], is_output=True)

            def transposes_tile(n, ti):
                qT, qT_b = qTs[(ti or 0) % 2]
                for gi, (q0, dst) in enumerate(((0, 0), (1024, 1), (2048, 2))):
                    for j in range(4):
                        Pp(lambda e, j=j, q0=q0: e.transpose(out=pT[:, j * 128:j * 128 + n], in_=qtm[0:n, q0 + j * 128:q0 + (j + 1) * 128], identity=ident[0:n, 0:n]), r=[qtm_b, ident_b], w=[pT_b])
                    if ti is not None:
                        Vv(lambda e, dst=dst: e.tensor_copy(out=qT[:, dst, :, :], in_=pT[:, 0:512].rearrange("p (j t) -> p j t", j=4)), r=[pT_b], w=[qT_b])
                    else:
                        if dst < 2:
                            d_, db_ = sq_s["aqT_s" if dst == 0 else "bqT_s"]
                            Vv(lambda e, d_=d_: e.tensor_copy(out=d_[:, :, :], in_=pT[:, 0:512].rearrange("p (j t) -> p j t", j=4)[:, :, 0:n]), r=[pT_b], w=[db_])
                for gi, q0 in enumerate((512, 1536)):
                    for j in range(4):
                        Pp(lambda e, j=j, q0=q0: e.transpose(out=pT[:, j * 128:j * 128 + n], in_=qtm[0:n, q0 + j * 128:q0 + (j + 1) * 128], identity=ident[0:n, 0:n]), r=[qtm_b, ident_b], w=[pT_b])
                    if ti is not None:
                        d_ = akT if gi == 0 else bkT
                        Aa(lambda e, d_=d_: e.copy(out=d_[:, :, ti * 128:(ti + 1) * 128], in_=pT[:, 0:512].rearrange("p (j t) -> p j t", j=4)), r=[pT_b], w=[kvb[ti]])
                    else:
                        d_, db_ = sq_s["akT_s" if gi == 0 else "bkT_s"]
                        Aa(lambda e, d_=d_: e.copy(out=d_[:, :, :], in_=pT[:, 0:512].rearrange("p (j t) -> p j t", j=4)[:, :, 0:n]), r=[pT_b], w=[db_])
                Pp(lambda e: e.transpose(out=pT[:, 0:n], in_=qtm[0:n, 2560:2688], identity=ident[0:n, 0:n]), r=[qtm_b, ident_b], w=[pT_b])
                if ti is not None:
                    Vv(lambda e: e.tensor_copy(out=ikT2[:, ti * 128:(ti + 1) * 128], in_=pT[:, 0:128]), r=[pT_b], w=[kvb[ti]])
                else:
                    Vv(lambda e: e.tensor_copy(out=sq_s["ikT_s"][0][:, :], in_=pT[:, 0:n]), r=[pT_b], w=[sq_s["ikT_s"][1]])
                    for h in range(8):
                        Pp(lambda e, h=h: e.transpose(out=pT[0:64, h * 64:h * 64 + n], in_=qtm[0:n, 2048 + h * 64:2048 + (h + 1) * 64], identity=ident[0:n, 0:n]), r=[qtm_b, ident_b], w=[pT_b])
                    Vv(lambda e: e.tensor_copy(out=sq_s["iqT_s8"][0][:, :, :], in_=pT[0:64, 0:512].rearrange("p (h t) -> p h t", h=8)), r=[pT_b], w=[sq_s["iqT_s8"][1]])
                    Vv(lambda e: e.tensor_copy(out=sq_s["wsg_s"][0][:, :], in_=wsg[0:n, 2, :]), r=[wsg_b], w=[sq_s["wsg_s"][1]])

            def index_tile(i):
                nk = 128 * (i + 1)
                use_topk = i >= 2
                qT, qT_b = qTs[i % 2]
                mb, mb_b = mbs2[i % 2]
                if use_topk:
                    for h in range(8):
                        Vv(lambda e, h=h: e.tensor_scalar(out=dg[:, h, :], in0=identf[:], scalar1=wsg[:, 2, h:h + 1], scalar2=None, op0=ALU.mult), r=[identf_b, wsg_b], w=[dg_b])
                    nch = (nk + 511) // 512
                    ibanks = [(pP[:, 0, :], pPb[0]), (pP[:, 1, :], pPb[1]), (pO[:, 0, :], pOb[0])]
                    iunits = [(kc, h) for kc in range(nch) for h in range(8)]

                    def idx_S(u):
                        kc, h = iunits[u]
                        k0 = kc * 512; kw = min(512, nk - k0)
                        kread = [kvb[t_] for t_ in range(k0 // 128, (k0 + kw) // 128)]
                        hp = (h % 2) * 64
                        bank, bankb = ibanks[u % 3]
                        Pp(lambda e: e.matmul(out=bank[:, 0:kw], lhsT=qT[hp:hp + 64, 2, h // 2, :], rhs=ikT2[hp:hp + 64, k0:k0 + kw], start=True, stop=True), r=[qT_b] + kread, w=[bankb])
                        Aa(lambda e: e.activation(out=rr[:, u % 3, 0:kw], in_=bank[:, 0:kw], func=AF.Relu), r=[bankb], w=[rrb[u % 3]])

                    def idx_D(u):
                        kc, h = iunits[u]
                        k0 = kc * 512; kw = min(512, nk - k0)
                        Pp(lambda e: e.matmul(out=pS[:, 0, 0:kw], lhsT=dg[:, h, :], rhs=rr[:, u % 3, 0:kw], start=(h == 0), stop=(h == 7)), r=[dg_b, rrb[u % 3]], w=[pSb[0]])
                        if h == 7:
                            Vv(lambda e: e.tensor_copy(out=sc[:, k0:k0 + kw], in_=pS[:, 0, 0:kw]), r=[pSb[0]], w=[sc_b])

                    for n_ in range(len(iunits) + 2):
                        if n_ < len(iunits):
                            idx_S(n_)
                        if n_ >= 2:
                            idx_D(n_ - 2)
                    steps = []

                    def st_init():
                        Vv(lambda e: e.tensor_reduce(out=bis[:, 0:1], in_=sc[:, 0:nk], axis=AX.X, op=ALU.min), r=[sc_b], w=[bis_b])
                        Vv(lambda e: e.tensor_tensor(out=sc[:, nk - 128:nk], in0=sc[:, nk - 128:nk], in1=cm30[:], op=ALU.add), r=[sc_b, cm30_b], w=[sc_b])
                        Vv(lambda e: e.tensor_reduce(out=bis[:, 1:2], in_=sc[:, 0:nk], axis=AX.X, op=ALU.max), r=[sc_b], w=[bis_b])
                        Vv(lambda e: e.tensor_tensor(out=bis[:, 2:3], in0=bis[:, 1:2], in1=bis[:, 0:1], op=ALU.subtract), r=[bis_b], w=[bis_b])
                    steps.append(st_init)

                    def mk_it(it):
                        f = 0.5 ** (it + 1)

                        def st_it():
                            Vv(lambda e: e.scalar_tensor_tensor(out=bis[:, 3:4], in0=bis[:, 2:3], scalar=f, in1=bis[:, 0:1], op0=ALU.mult, op1=ALU.add), r=[bis_b], w=[bis_b])
                            Vv(lambda e: e.tensor_scalar(out=mb[:, 0:nk], in0=sc[:, 0:nk], scalar1=bis[:, 3:4], scalar2=None, op0=ALU.is_ge, op1=ALU.add, accum_out=bis[:, 4:5]),
                               r=[sc_b, bis_b], w=[mb_b, bis_b])
                            Vv(lambda e: e.tensor_scalar(out=bis[:, 5:6], in0=bis[:, 4:5], scalar1=float(TOPK), scalar2=f, op0=ALU.is_ge, op1=ALU.mult), r=[bis_b], w=[bis_b])
                            Vv(lambda e: e.scalar_tensor_tensor(out=bis[:, 0:1], in0=bis[:, 5:6], scalar=bis[:, 2:3], in1=bis[:, 0:1], op0=ALU.mult, op1=ALU.add), r=[bis_b], w=[bis_b])
                        return st_it
                    for it in range(NBIS):
                        steps.append(mk_it(it))

                    def st_fin():
                        Vv(lambda e: e.tensor_scalar(out=mb[:, 0:nk], in0=sc[:, 0:nk], scalar1=bis[:, 0:1], scalar2=NEG, op0=ALU.is_lt, op1=ALU.mult), r=[sc_b, bis_b], w=[mb_b])
                    steps.append(st_fin)
                    return steps
                return []

            def attend_tile(i, extra_steps=()):
                extra_steps = list(extra_steps)
                nk = 128 * (i + 1)
                use_topk = i >= 2
                qT, qT_b = qTs[i % 2]
                mb, mb_b = mbs2[i % 2]
                sbanks = [(pS[:, 0, :], pSb[0]), (pS[:, 1, :], pSb[1]), (pO[:, 2, :], pOb[2])]
                units = []
                for g in range(8):
                    kgs = list(range(0, i + 1, 4))
                    for kg in kgs:
                        units.append(("a", g, kg, kg == kgs[-1]))
                for g in range(8):
                    kgs = list(range(0, i + 1, 4))
                    for kg in kgs:
                        units.append(("b", g, kg, kg == kgs[-1]))

                def uparams(kind, g):
                    if kind == "a":
                        return dict(hp=(g % 2) * 64, blk=g // 2, kT=akT, qsel=0, ob=pOb[g % 2], oap=pO[:, g % 2, 0:65])
                    h_, c_ = g // 2, g % 2
                    return dict(hp=c_ * 64, blk=h_, kT=bkT, qsel=1, ob=pOb[c_], oap=pO[:, c_, 0:129])

                def att_QK(u):
                    kind, g, kg, _last = units[u]
                    p_ = uparams(kind, g)
                    hp, blk, kT, qsel = p_["hp"], p_["blk"], p_["kT"], p_["qsel"]
                    bank, bankb = sbanks[u % 3]
                    kts = list(range(kg, min(kg + 4, i + 1)))
                    for jj, kt in enumerate(kts):
                        need_mask = (kind == "a" and use_topk) or kt == i
                        Pp(lambda e, jj=jj, kt=kt, need_mask=need_mask: e.matmul(out=bank[:, jj * 128:(jj + 1) * 128], lhsT=kT[hp:hp + 64, blk, kt * 128:(kt + 1) * 128],
                                                                                 rhs=qT[hp:hp + 64, qsel, blk, :], start=True, stop=not need_mask),
                           r=[kvb[kt], qT_b], w=[bankb])
                        if need_mask:
                            if kind == "a" and use_topk:
                                Pp(lambda e, jj=jj, kt=kt: e.matmul(out=bank[:, jj * 128:(jj + 1) * 128], lhsT=mb[:, kt * 128:(kt + 1) * 128], rhs=ident[:], start=False, stop=True),
                                   r=[mb_b, ident_b], w=[bankb])
                            else:
                                Pp(lambda e, jj=jj: e.matmul(out=bank[:, jj * 128:(jj + 1) * 128], lhsT=cm[:], rhs=ident[:], start=False, stop=True),
                                   r=[cm_b, ident_b], w=[bankb])
                    wd = 128 * len(kts)
                    Aa(lambda e: e.activation(out=PT[:, u % 3, 0:wd], in_=bank[:, 0:wd], func=AF.Exp, scale=0.125), r=[bankb], w=[PTb[u % 3]])

                def att_PV(u):
                    kind, g, kg, last = units[u]
                    p_ = uparams(kind, g)
                    ob, oap = p_["ob"], p_["oap"]
                    kts = list(range(kg, min(kg + 4, i + 1)))
                    for jj, kt in enumerate(kts):
                        rhs = av_sb[:, kt, g, :] if kind == "a" else bv_sb[:, kt, g // 2, :]
                        Pp(lambda e, jj=jj, kt=kt, rhs=rhs: e.matmul(out=oap, lhsT=PT[:, u % 3, jj * 128:(jj + 1) * 128], rhs=rhs, start=(kt == 0), stop=(kt == i)),
                           r=[PTb[u % 3], kvb[kt]], w=[ob])
                    if not last:
                        return
                    if kind == "a":
                        Vv(lambda e: e.reciprocal(out=osm[:, g:g + 1], in_=oap[:, 64:65]), r=[ob], w=[osm_b])
                        Vv(lambda e: e.tensor_scalar(out=mixed[:, g * 64:(g + 1) * 64], in0=oap[:, 0:64], scalar1=osm[:, g:g + 1], scalar2=None, op0=ALU.mult), r=[ob, osm_b], w=[mixed_b])
                    else:
                        h_, c_ = g // 2, g % 2
                        Vv(lambda e: e.reciprocal(out=osm[:, c_:c_ + 1], in_=oap[:, 128:129]), r=[ob], w=[osm_b])
                        Vv(lambda e: e.tensor_scalar(out=bo[:, c_, :], in0=oap[:, 0:128], scalar1=osm[:, c_:c_ + 1], scalar2=None, op0=ALU.mult), r=[ob, osm_b], w=[bo_b])
                        if c_ == 1:
                            diff_finish(bo, bo_b, mixed, mixed_b, h_, 128, osm, osm_b)

                LOOK = 2
                n_total = len(units) + LOOK
                done_steps = 0
                for n_ in range(n_total):
                    if n_ < len(units):
                        att_QK(n_)
                    if n_ >= LOOK:
                        att_PV(n_ - LOOK)
                    want = (len(extra_steps) * (n_ + 1)) // n_total
                    while done_steps < want:
                        extra_steps[done_steps]()
                        done_steps += 1
                while done_steps < len(extra_steps):
                    extra_steps[done_steps]()
                    done_steps += 1

            def diff_finish(bo_, bo_b_, mixed_, mixed_b_, h_, n, osm_, osm_b_):
                Vv(lambda e: e.scalar_tensor_tensor(out=bo_[0:n, 0, :], in0=bo_[0:n, 1, :], scalar=neglam[0:n, :], in1=bo_[0:n, 0, :], op0=ALU.mult, op1=ALU.add), r=[bo_b_, lamv_b], w=[bo_b_])
                Aa(lambda e: e.activation(out=bo_[0:n, 1, :], in_=bo_[0:n, 0, :], func=AF.Square, accum_out=osm_[0:n, 2:3]), r=[bo_b_], w=[bo_b_, osm_b_])
                rstd_from_ssq(osm_[0:n, 2:3], 1.0 / 128, [osm_b_])
                Vv(lambda e: e.tensor_scalar(out=osm_[0:n, 2:3], in0=osm_[0:n, 2:3], scalar1=0.8, scalar2=None, op0=ALU.mult), r=[osm_b_], w=[osm_b_])
                Vv(lambda e: e.scalar_tensor_tensor(out=mixed_[0:n, 512 + h_ * 128:512 + (h_ + 1) * 128], in0=bo_[0:n, 0, :], scalar=osm_[0:n, 2:3], in1=subln_t[0:n, :], op0=ALU.mult, op1=ALU.mult),
                   r=[bo_b_, osm_b_, subln_b], w=[mixed_b_])

            if "A" in phases:
                project_tile(xs[:, :], rope_s[:, :], NS, o_s, 0, None)
                transposes_tile(NS, None)
                S.dma("sp", lambda e: e.dma_start(out=sgnbuf[:, :], in_=sq_s["wsg_s"][0][:, :]), r=[sq_s["wsg_s"][1]], w=[hbuf_b])
                npt = NT
                def stage_A(i):
                    project_tile(xp[i * 128:(i + 1) * 128, :], rope_p[i * 128:(i + 1) * 128, :], 128, o_p, i * 128, i)
                    transposes_tile(128, i)
                    return index_tile(i)
                for st_ in stage_A(0):
                    st_()
                for i in range(npt):
                    steps_next = stage_A(i + 1) if i + 1 < npt else []
                    attend_tile(i, steps_next)
                    S.dma("sp", lambda e, i=i: e.dma_start(out=mbuf[i * 128:(i + 1) * 128, :], in_=mixed[:, :]), r=[mixed_b], w=[mbuf_b])

        S.barrier()
        if "S" in phases:
            with contextlib.ExitStack() as ps_:
                aqT_s, aqT_s_b = sq_s["aqT_s"]; bqT_s, bqT_s_b = sq_s["bqT_s"]; iqT_s8, iqT_s8_b = sq_s["iqT_s8"]
                akT_s, akT_s_b = sq_s["akT_s"]; bkT_s, bkT_s_b = sq_s["bkT_s"]; ikT_s, ikT_s_b = sq_s["ikT_s"]
                av_s, av_s_b = sq_s["av_s"]; bv_s, bv_s_b = sq_s["bv_s"]; wsg_s, wsg_s_b = sq_s["wsg_s"]
                pti, pti_b = sbt(ps_, "pti", [128, NB * NPG], I32)
                idx, idx_b = sbt(ps_, "idx", [128, NB * NPG], I32)
                iot, iot_b = sbt(ps_, "iot", [128, 1], I32)
                signB, signB_b = sbt(ps_, "signB", [128, NB, 8, 4], F32)
                sgn_tmp, sgn_tmp_b = sbt(ps_, "sgn_tmp", [128, 512], F32)
                bselt, bselt_b = sbt(ps_, "bselt", [1, NB * 512], BF16)
                ones1, ones1_b = sbt(ps_, "ones1", [128, 128], BF16)
                zrow, zrow_b = sbt(ps_, "zrow", [1, 512], BF16)
                i8, i8_b = sbt(ps_, "i8", [64, 512], BF16)
                nbq, nbq_b = sbt(ps_, "nbq", [64, 64], F32)
                nbqh, nbqh_b = sbt(ps_, "nbqh", [64, 2, 64], BF16)
                nb30, nb30_b = sbt(ps_, "nb30", [64, 4], F32)
                bdiag, bdiag_b = sbt(ps_, "bdiag", [64, 64], F32)
                Qbd_a, Qbd_a_b = sbt(ps_, "Qbd_a", [128, 4, 2, NS], BF16)
                Qbd_b, Qbd_b_b = sbt(ps_, "Qbd_b", [128, 4, 2, NS], BF16)
                ipg = [sbt(ps_, "ipg%d" % k, [128, NPG, 64], BF16) for k in range(2)]
                pti2, pti2_b = sbt(ps_, "pti2", [128, NB], I32)
                idx2, idx2_b = sbt(ps_, "idx2", [128, NB], I32)
                iot7, iot7_b = sbt(ps_, "iot7", [128, 1], I32)
                ikTp, ikTp_b = sbt(ps_, "ikTp", [64, NPG, 128], BF16)
                Rb, Rb_b = sbt(ps_, "Rb", [128, NPG, 8, 4], F32)
                STall, STall_b = sbt(ps_, "STall", [128, NPG, NS], F32)
                scs, scs_b = sbt(ps_, "scs", [NS, 2052], F32)
                mbs, mbs_b = sbt(ps_, "mbs", [NS, 2052], BF16)
                bis2, bis2_b = sbt(ps_, "bis2", [NS, 8], F32)
                snew, snew_b = sbt(ps_, "snew", [NS, 8, 64], F32)
                sacc, sacc_b = sbt(ps_, "sacc", [NS, 64], F32)
                pg = {k: [sbt(ps_, "pg%s%d" % (k, j), [128, 512], BF16) for j in range(4)] for k in ("ak", "av", "bk", "bv")}
                kTp = {k: [sbt(ps_, "kTp%s%d" % (k, j), [128, 4, 128], BF16) for j in range(2)] for k in ("ak", "bk")}
                PTs = [sbt(ps_, "PTs%d" % j, [128, 512], BF16) for j in range(2)]
                PTd = [sbt(ps_, "PTd%d" % j, [128, 512], BF16) for j in range(2)]
                zr, zr_b = sbt(ps_, "zr", [1, 2, 512], F32)
                zc, zc_b = sbt(ps_, "zc", [NS, 16], F32)
                oa_s, oa_s_b = sbt(ps_, "oa_s", [NS, 8, 64], F32)
                bo_s, bo_s_b = sbt(ps_, "bo_s", [NS, 2, 128], F32)
                osm_s, osm_s_b = sbt(ps_, "osm_s", [NS, 8], F32)

                S.dma("sp", lambda e: e.dma_start(out=pti[:], in_=ptab.partition_broadcast(128)), w=[pti_b])
                Gg(lambda e: e.iota(out=iot[:], pattern=[[0, 1]], base=0, channel_multiplier=1), w=[iot_b])
                Gg(lambda e: e.tensor_scalar(out=idx[:], in0=pti[:], scalar1=128, scalar2=None, op0=ALU.mult), r=[pti_b], w=[idx_b])
                Gg(lambda e: e.tensor_tensor(out=idx[:], in0=idx[:], in1=iot[:].to_broadcast([128, NB * NPG]), op=ALU.add), r=[idx_b, iot_b], w=[idx_b])
                for j in range(NPG):
                    S.dma("sp", lambda e, j=j: e.dma_start(out=pti2[j * 8:(j + 1) * 8, :], in_=ptabT[:, j * NB:(j + 1) * NB].partition_broadcast(8)), w=[pti2_b])
                Vv(lambda e: e.tensor_single_scalar(out=iot7[:], in_=iot[:], scalar=7, op=ALU.bitwise_and), r=[iot_b], w=[iot7_b])
                Gg(lambda e: e.tensor_scalar(out=idx2[:], in0=pti2[:], scalar1=8, scalar2=None, op0=ALU.mult), r=[pti2_b], w=[idx2_b])
                Gg(lambda e: e.tensor_tensor(out=idx2[:], in0=idx2[:], in1=iot7[:].to_broadcast([128, NB]), op=ALU.add), r=[idx2_b, iot7_b], w=[idx2_b])
                cik2 = cik.rearrange("(r t) d -> r (t d)", t=16)
                S.dma("pool", lambda e: e.dma_start(out=bselt[:], in_=c_bsel[:, :]), w=[bselt_b])
                S.dma("pool", lambda e: e.dma_start(out=i8[:], in_=c_i8[:, :]), w=[i8_b])
                S.dma("sp", lambda e: e.dma_start(out=nbq[:], in_=c_nbq[:, :]), w=[nbq_b])
                S.dma("sp", lambda e: e.dma_start(out=nb30[:], in_=c_nb30[:, :]), w=[nb30_b])
                S.dma("sp", lambda e: e.dma_start(out=bdiag[:], in_=c_bdiag[:, :]), w=[bdiag_b])
                Vv(lambda e: e.memset(ones1[:], 1.0), w=[ones1_b])
                Vv(lambda e: e.memset(zrow[:], 0.0), w=[zrow_b])
                S.dma("sp", lambda e: e.dma_start(out=sgn_tmp[:, :], in_=sgnbuf.rearrange("(o r) h -> o (r h)", o=1).partition_broadcast(128)), r=[hbuf_b], w=[sgn_tmp_b])
                Vv(lambda e: e.tensor_copy(out=signB[:], in_=sgn_tmp[:, :].rearrange("p (b q h) -> p b h q", b=NB, q=4)), r=[sgn_tmp_b], w=[signB_b])
                Vv(lambda e: e.memset(Qbd_a[:], 0.0), w=[Qbd_a_b])
                Vv(lambda e: e.memset(Qbd_b[:], 0.0), w=[Qbd_b_b])
                for (Qd, Qd_b, src, src_b) in ((Qbd_a, Qbd_a_b, aqT_s, aqT_s_b), (Qbd_b, Qbd_b_b, bqT_s, bqT_s_b)):
                    Vv(lambda e, Qd=Qd, src=src: e.tensor_copy(out=Qd[0:64, :, 0, :], in_=src[0:64, :, :]), r=[src_b], w=[Qd_b])
                    Vv(lambda e, Qd=Qd, src=src: e.tensor_copy(out=Qd[64:128, :, 1, :], in_=src[64:128, :, :]), r=[src_b], w=[Qd_b])

                for b in range(NB):
                    ip, ip_b = ipg[b % 2]
                    S.dma("pool", lambda e, b=b, ip=ip: e.indirect_dma_start(out=ip[:, :, :].rearrange("p t d -> p (t d)"), out_offset=None, in_=cik2,
                                                                              in_offset=bass.IndirectOffsetOnAxis(ap=idx2[:, b:b + 1], axis=0)), r=[idx2_b], w=[ip_b])
                    for half in range(2):
                        for j in range(8):
                            jj = half * 8 + j
                            Pp(lambda e, j=j, jj=jj, ip=ip: e.transpose(out=pT[0:64, j * 128:(j + 1) * 128], in_=ip[:, jj, :], identity=ident[:]), r=[ip_b, ident_b], w=[pT_b])
                        Vv(lambda e, half=half: e.tensor_copy(out=ikTp[:, half * 8:(half + 1) * 8, :], in_=pT[0:64, :].rearrange("p (j t) -> p j t", j=8)), r=[pT_b], w=[ikTp_b])
                    pb = b % 2
                    for j in range(NPG):
                        Pp(lambda e, j=j, b=b, pb=pb: e.matmul(out=pP[:, pb, j * 32:(j + 1) * 32].rearrange("p (h q) -> p h q", h=8), lhsT=ikTp[:, j, :], rhs=iqT_s8[:, :, b * 4:(b + 1) * 4], start=True, stop=True),
                           r=[ikTp_b, iqT_s8_b], w=[pPb[pb]])
                    Aa(lambda e, pb=pb: e.activation(out=Rb[:].rearrange("p j h q -> p (j h q)"), in_=pP[:, pb, :], func=AF.Relu), r=[pPb[pb]], w=[Rb_b])
                    Vv(lambda e, b=b: e.tensor_tensor(out=Rb[:], in0=Rb[:], in1=signB[:, b, :, :].unsqueeze(1).to_broadcast([128, NPG, 8, 4]), op=ALU.mult), r=[Rb_b, signB_b], w=[Rb_b])
                    Vv(lambda e, b=b: e.tensor_reduce(out=STall[:, :, b * 4:(b + 1) * 4], in_=Rb[:].rearrange("p j h q -> p j q h"), axis=AX.X, op=ALU.add), r=[Rb_b], w=[STall_b])
                for j in range(NPG):
                    pb = j % 2
                    Pp(lambda e, j=j, pb=pb: e.transpose(out=pP[0:NS, pb, 0:128], in_=STall[:, j, :], identity=identf[:]), r=[STall_b, identf_b], w=[pPb[pb]])
                    Vv(lambda e, j=j, pb=pb: e.tensor_copy(out=scs[:, 0:2048].rearrange("r (g t) -> r g t", t=16)[:, :, j], in_=pP[0:NS, pb, 0:128]), r=[pPb[pb]], w=[scs_b])
                for h in range(8):
                    Pp(lambda e, h=h: e.matmul(out=pS[0:NS, 0, h * 64:(h + 1) * 64], lhsT=iqT_s8[:, h, :], rhs=ikT_s[0:64, :], start=True, stop=True), r=[iqT_s8_b, ikT_s_b], w=[pSb[0]])
                Aa(lambda e: e.activation(out=snew[:].rearrange("p h r -> p (h r)"), in_=pS[0:NS, 0, :], func=AF.Relu), r=[pSb[0]], w=[snew_b])
                Vv(lambda e: e.tensor_scalar(out=sacc[:], in0=snew[:, 0, :], scalar1=wsg_s[:, 0:1], scalar2=None, op0=ALU.mult), r=[snew_b, wsg_s_b], w=[sacc_b])
                for h in range(1, 8):
                    Vv(lambda e, h=h: e.scalar_tensor_tensor(out=sacc[:], in0=snew[:, h, :], scalar=wsg_s[:, h:h + 1], in1=sacc[:], op0=ALU.mult, op1=ALU.add), r=[snew_b, wsg_s_b, sacc_b], w=[sacc_b])
                Vv(lambda e: e.tensor_tensor(out=sacc[:], in0=sacc[:], in1=bdiag[:], op=ALU.mult), r=[sacc_b, bdiag_b], w=[sacc_b])
                Vv(lambda e: e.tensor_reduce(out=scs[:, 2048:2052], in_=sacc[:].rearrange("p (b j) -> p j b", j=4), axis=AX.X, op=ALU.add), r=[sacc_b], w=[scs_b])
                Vv(lambda e: e.tensor_reduce(out=bis2[:, 0:1], in_=scs[:, :], axis=AX.X, op=ALU.min), r=[scs_b], w=[bis2_b])
                Vv(lambda e: e.tensor_tensor(out=scs[:, 2048:2052], in0=scs[:, 2048:2052], in1=nb30[:], op=ALU.add), r=[scs_b, nb30_b], w=[scs_b])
                Vv(lambda e: e.tensor_reduce(out=bis2[:, 1:2], in_=scs[:, :], axis=AX.X, op=ALU.max), r=[scs_b], w=[bis2_b])
                Vv(lambda e: e.tensor_tensor(out=bis2[:, 2:3], in0=bis2[:, 1:2], in1=bis2[:, 0:1], op=ALU.subtract), r=[bis2_b], w=[bis2_b])
                for it in range(NBIS):
                    f = 0.5 ** (it + 1)
                    Vv(lambda e, f=f: e.scalar_tensor_tensor(out=bis2[:, 3:4], in0=bis2[:, 2:3], scalar=f, in1=bis2[:, 0:1], op0=ALU.mult, op1=ALU.add), r=[bis2_b], w=[bis2_b])
                    Vv(lambda e: e.tensor_scalar(out=mbs[:, :], in0=scs[:, :], scalar1=bis2[:, 3:4], scalar2=None, op0=ALU.is_ge, op1=ALU.add, accum_out=bis2[:, 4:5]), r=[scs_b, bis2_b], w=[mbs_b, bis2_b])
                    Vv(lambda e, f=f: e.tensor_scalar(out=bis2[:, 5:6], in0=bis2[:, 4:5], scalar1=float(TOPK), scalar2=f, op0=ALU.is_ge, op1=ALU.mult), r=[bis2_b], w=[bis2_b])
                    Vv(lambda e: e.scalar_tensor_tensor(out=bis2[:, 0:1], in0=bis2[:, 5:6], scalar=bis2[:, 2:3], in1=bis2[:, 0:1], op0=ALU.mult, op1=ALU.add), r=[bis2_b], w=[bis2_b])
                Vv(lambda e: e.tensor_scalar(out=mbs[:, :], in0=scs[:, :], scalar1=bis2[:, 0:1], scalar2=NEG, op0=ALU.is_lt, op1=ALU.mult), r=[scs_b, bis2_b], w=[mbs_b])
                Vv(lambda e: e.tensor_copy(out=nbqh[:, 0, :], in_=nbq[:]), r=[nbq_b], w=[nbqh_b])
                Vv(lambda e: e.tensor_tensor(out=nbqh[:, 1, :].rearrange("p (b j) -> p b j", j=4), in0=nbq[:].rearrange("p (b j) -> p b j", j=4),
                                              in1=mbs[:, 2048:2052].unsqueeze(1).to_broadcast([NS, NB, 4]), op=ALU.add), r=[nbq_b, mbs_b], w=[nbqh_b])

                oA = pO[0:NS, 0, :]
                oB = pO[0:NS, 1:3, :]
                zA = pP[0:1, 0, :]
                zB = pP[32:33, 0, :]
                pT2 = pP[:, 1, :].bitcast(BF16)
                steps3 = [(b, j) for b in range(NB) for j in range(NPG)] + [(NB, 0)]
                nsteps = len(steps3)

                def s3_L(i):
                    b, j = steps3[i]
                    if b == NB:
                        return
                    col = b * NPG + j
                    for k, src in (("ak", cak), ("av", cav), ("bk", cbk), ("bv", cbv)):
                        tt, tb = pg[k][i % 4]
                        S.dma("pool", lambda e, tt=tt, src=src: e.indirect_dma_start(out=tt[:, :], out_offset=None, in_=src[:, :],
                                                                                    in_offset=bass.IndirectOffsetOnAxis(ap=idx[:, col:col + 1], axis=0)), r=[idx_b], w=[tb])

                def s3_T(i):
                    b, j = steps3[i]
                    if b == NB:
                        return
                    for k, (tbank, tbank_b) in (("ak", (pT, pT_b)), ("bk", (pT2, pPb[1]))):
                        tt, tb = pg[k][i % 4]
                        dd, ddb = kTp[k][i % 2]
                        for q4 in range(4):
                            Pp(lambda e, q4=q4: e.transpose(out=tbank[:, q4 * 128:(q4 + 1) * 128], in_=tt[:, q4 * 128:(q4 + 1) * 128], identity=ident[:]), r=[tb, ident_b], w=[tbank_b])
                        if k == "ak":
                            Vv(lambda e: e.tensor_copy(out=dd[:, :, :], in_=tbank[:, 0:512].rearrange("p (j t) -> p j t", j=4)), r=[tbank_b], w=[ddb])
                        else:
                            Aa(lambda e: e.copy(out=dd[:, :, :], in_=tbank[:, 0:512].rearrange("p (j t) -> p j t", j=4)), r=[tbank_b], w=[ddb])

                def s3_ops(i):
                    b, j = steps3[i]
                    if b < NB:
                        akt, akt_b = kTp["ak"][i % 2]; bkt, bkt_b = kTp["bk"][i % 2]
                        avt, avt_b = pg["av"][i % 4]; bvt, bvt_b = pg["bv"][i % 4]
                        return 128, (lambda q4: akt[:, q4, :]), akt_b, (lambda q4: bkt[:, q4, :]), bkt_b, avt, avt_b, bvt, bvt_b
                    return NS, (lambda q4: akT_s[:, q4, :]), akT_s_b, (lambda q4: bkT_s[:, q4, :]), bkT_s_b, av_s, av_s_b, bv_s, bv_s_b

                def s3_Sc(i):
                    b, j = steps3[i]
                    nkp, akl, akt_b, bkl, bkt_b, avt, avt_b, bvt, bvt_b = s3_ops(i)
                    sl = i % 2
                    if b < NB:
                        Pp(lambda e: e.matmul(out=pS[0:nkp, 0, :], lhsT=ones1[0:1, 0:nkp], rhs=bselt[0:1, b * 512:(b + 1) * 512], start=True, stop=False), r=[ones1_b, bselt_b], w=[pSb[0]])
                        Pp(lambda e: e.matmul(out=pS[0:nkp, 0, :], lhsT=mbs[:, j * 128:(j + 1) * 128], rhs=i8[:, :], start=False, stop=False), r=[mbs_b, i8_b], w=[pSb[0]])
                    else:
                        Pp(lambda e: e.matmul(out=pS[0:nkp, 0, :], lhsT=nbqh[:, 1, :], rhs=i8[:, :], start=True, stop=False), r=[nbqh_b, i8_b], w=[pSb[0]])
                    for q4 in range(4):
                        Pp(lambda e, q4=q4: e.matmul(out=pS[0:nkp, 0, q4 * 128:(q4 + 1) * 128], lhsT=akl(q4), rhs=Qbd_a[:, q4, :, :].rearrange("p a r -> p (a r)"), start=False, stop=(q4 == 3)),
                           r=[akt_b, Qbd_a_b], w=[pSb[0]])
                    pts, pts_b = PTs[sl]
                    Aa(lambda e: e.activation(out=pts[0:nkp, :], in_=pS[0:nkp, 0, :], func=AF.Exp, scale=0.125), r=[pSb[0]], w=[pts_b])
                    if b < NB:
                        Pp(lambda e: e.matmul(out=pS[0:nkp, 1, :], lhsT=ones1[0:1, 0:nkp], rhs=bselt[0:1, b * 512:(b + 1) * 512], start=True, stop=False), r=[ones1_b, bselt_b], w=[pSb[1]])
                    else:
                        Pp(lambda e: e.matmul(out=pS[0:nkp, 1, :], lhsT=nbqh[:, 0, :], rhs=i8[:, :], start=True, stop=False), r=[nbqh_b, i8_b], w=[pSb[1]])
                    for q4 in range(4):
                        Pp(lambda e, q4=q4: e.matmul(out=pS[0:nkp, 1, q4 * 128:(q4 + 1) * 128], lhsT=bkl(q4), rhs=Qbd_b[:, q4, :, :].rearrange("p a r -> p (a r)"), start=False, stop=(q4 == 3)),
                           r=[bkt_b, Qbd_b_b], w=[pSb[1]])
                    ptd, ptd_b = PTd[sl]
                    Aa(lambda e: e.activation(out=ptd[0:nkp, :], in_=pS[0:nkp, 1, :], func=AF.Exp, scale=0.125), r=[pSb[1]], w=[ptd_b])

                def s3_V(i):
                    nkp, akl, akt_b, bkl, bkt_b, avt, avt_b, bvt, bvt_b = s3_ops(i)
                    sl = i % 2
                    first = (i == 0); last = (i == nsteps - 1)
                    pts, pts_b = PTs[sl]; ptd, ptd_b = PTd[sl]
                    if first:
                        for bk_ in range(3):
                            Pp(lambda e, bk_=bk_: e.matmul(out=pO[0:NS, bk_, :], lhsT=zrow[0:1, 0:NS], rhs=zrow[0:1, 0:512], start=True, stop=False), r=[zrow_b], w=[pOb[bk_]])
                    for h in range(8):
                        Pp(lambda e, h=h: e.matmul(out=oA[:, h * 64:(h + 1) * 64], lhsT=pts[0:nkp, h * 64:(h + 1) * 64], rhs=avt[0:nkp, h * 64:(h + 1) * 64], start=False, stop=last),
                           r=[pts_b, avt_b], w=[pOb[0]])
                    Pp(lambda e: e.matmul(out=zA, lhsT=ones1[0:nkp, 0:1], rhs=pts[0:nkp, :], start=first, stop=last), r=[pts_b, ones1_b], w=[pPb[0]])
                    for g in range(8):
                        h_ = g // 2
                        Pp(lambda e, g=g, h_=h_: e.matmul(out=oB[:, g // 4, (g % 4) * 128:(g % 4 + 1) * 128], lhsT=ptd[0:nkp, g * 64:(g + 1) * 64], rhs=bvt[0:nkp, h_ * 128:(h_ + 1) * 128], start=False, stop=last),
                           r=[ptd_b, bvt_b], w=[pOb[1 + g // 4]])
                    Pp(lambda e: e.matmul(out=zB, lhsT=ones1[0:nkp, 0:1], rhs=ptd[0:nkp, :], start=first, stop=last), r=[ptd_b, ones1_b], w=[pPb[0]])

                for n_ in range(nsteps + 3):
                    if n_ < nsteps:
                        s3_L(n_)
                    if 1 <= n_ <= nsteps:
                        s3_T(n_ - 1)
                    if 2 <= n_ <= nsteps + 1:
                        s3_Sc(n_ - 2)
                    if n_ >= 3:
                        s3_V(n_ - 3)
                Vv(lambda e: e.tensor_copy(out=zr[:, 0, :], in_=zA), r=[pPb[0]], w=[zr_b])
                Vv(lambda e: e.tensor_copy(out=zr[:, 1, :], in_=zB), r=[pPb[0]], w=[zr_b])
                for a in range(2):
                    for g in range(8):
                        Pp(lambda e, a=a, g=g: e.matmul(out=pS[0:NS, 0, a * 8 + g:a * 8 + g + 1], lhsT=zr[0:1, a, g * 64:(g + 1) * 64], rhs=identf[0:1, 0:1], start=True, stop=True), r=[zr_b, identf_b], w=[pSb[0]])
                Vv(lambda e: e.reciprocal(out=zc[:, :], in_=pS[0:NS, 0, 0:16]), r=[pSb[0]], w=[zc_b])
                Vv(lambda e: e.tensor_tensor(out=oa_s[:], in0=oA.rearrange("p (h d) -> p h d", h=8), in1=zc[:, 0:8].unsqueeze(2).to_broadcast([NS, 8, 64]), op=ALU.mult), r=[pOb[0], zc_b], w=[oa_s_b])
                Vv(lambda e: e.tensor_copy(out=mixed_s[:, 0:512], in_=oa_s[:].rearrange("p h d -> p (h d)")), r=[oa_s_b], w=[mixed_s_b])
                for h_ in range(4):
                    for c_ in range(2):
                        g = h_ * 2 + c_
                        Vv(lambda e, g=g, c_=c_: e.tensor_scalar(out=bo_s[:, c_, :], in0=oB[:, g // 4, (g % 4) * 128:(g % 4 + 1) * 128], scalar1=zc[:, 8 + g:9 + g], scalar2=None, op0=ALU.mult), r=[pOb[1 + g // 4], zc_b], w=[bo_s_b])
                    diff_finish(bo_s, bo_s_b, mixed_s, mixed_s_b, h_, NS, osm_s, osm_s_b)
                S.dma("sp", lambda e: e.dma_start(out=mbuf[T:T + NS, :], in_=mixed_s[:, :]), r=[mixed_s_b], w=[mbuf_b])
                if debug:
                    S.dma("pool", lambda e: e.dma_start(out=dbg_mixed[:, :], in_=mixed_s[:, :]), r=[mixed_s_b], is_output=True)
                    S.dma("sp", lambda e: e.dma_start(out=dbg_scs[:, :], in_=scs[:, :]), r=[scs_b], is_output=True)
                    S.dma("pool", lambda e: e.dma_start(out=dbg_mbs[:, :], in_=mbs[:, :]), r=[mbs_b], is_output=True)
                    S.dma("sp", lambda e: e.dma_start(out=dbg_z[:, :], in_=zc[:, :]), r=[zc_b], is_output=True)

        S.barrier()
        if "B" in phases:
            with contextlib.ExitStack() as pb_:
                NTT = NT + 1
                xn2T, xn2T_b = sbt(pb_, "xn2T", [128, 8, NTT * 128], BF16)
                gates, gates_b = sbt(pb_, "gates", [128, NTT, 32], F32)
                facc, _ = sbt(pb_, "facc", [128, NTT, D], F32)
                faccb = [Buf("facc%d" % t) for t in range(NTT)]
                wr_sb, wr_b = sbt(pb_, "wr_sb", [128, 8, 36], BF16)
                wout_sb, wout_b = sbt(pb_, "wout_sb", [128, 8, D], BF16)
                gffn_t, gffn_b = sbt(pb_, "gffn_t", [128, D], F32)
                mxl = [sbt(pb_, "mxl%d" % k, [128, D], BF16) for k in range(2)]
                xrl = [sbt(pb_, "xrl%d" % k, [128, D], F32) for k in range(2)]
                mixT, mixT_b = sbt(pb_, "mixT", [128, 8, 128], BF16)
                xn2, xn2_b = sbt(pb_, "xn2", [128, D], BF16)
                rt, rt_b = sbt(pb_, "rt", [128, 160], F32)
                wgu = [sbt(pb_, "wgu%d" % k, [128, 8, 512], BF16) for k in range(2)]
                wdn = [sbt(pb_, "wdn%d" % k, [128, 2, D], BF16) for k in range(2)]
                sil = [sbt(pb_, "sil%d" % k, [128, 256], F32) for k in range(2)]
                actt = [sbt(pb_, "actt%d" % k, [128, 256], BF16) for k in range(2)]
                actT = [sbt(pb_, "actT%d" % k, [128, 2, 128], BF16) for k in range(2)]
                S.dma("pool", lambda e: e.dma_start(out=wout_sb[:], in_=w_out.rearrange("(c p) n -> p c n", p=128)), w=[wout_b])
                S.dma("sp", lambda e: e.dma_start(out=gffn_t[:], in_=g_ffn.partition_broadcast(128)), w=[gffn_b])
                Vv(lambda e: e.memset(gates[:], 0.0), w=[gates_b])
                S.dma("pool", lambda e: e.dma_start(out=wr_sb[:], in_=w_r.rearrange("(c p) n -> p c n", p=128)), w=[wr_b])
                Vv(lambda e: e.memset(xn2T[:, :, NT * 128 + NS:NTT * 128], 0.0), w=[xn2T_b])

                def rows(t):
                    return (t * 128, 128) if t < NT else (T, NS)

                for t in range(NTT):
                    r0, n = rows(t)
                    mx_t, mx_b = mxl[t % 2]
                    xr_t, xr_b = xrl[t % 2]
                    x_src = xp[r0:r0 + n, :] if t < NT else xs[:, :]
                    S.dma("sp", lambda e, mx_t=mx_t, r0=r0, n=n: e.dma_start(out=mx_t[0:n, :], in_=mbuf[r0:r0 + n, :]), r=[mbuf_b], w=[mx_b])
                    S.dma("sp", lambda e, xr_t=xr_t, x_src=x_src, n=n: e.dma_start(out=xr_t[0:n, :], in_=x_src), w=[xr_b])
                    for c in range(8):
                        Pp(lambda e, c=c, n=n, mx_t=mx_t: e.transpose(out=pT[:, c * 128:c * 128 + n], in_=mx_t[0:n, c * 128:(c + 1) * 128], identity=ident[0:n, 0:n]), r=[mx_b, ident_b], w=[pT_b])
                    Aa(lambda e, n=n: e.copy(out=mixT[:, :, 0:n], in_=pT[:, :].rearrange("p (c t) -> p c t", c=8)[:, :, 0:n]), r=[pT_b], w=[mixT_b])
                    h_t = facc[:, t, :]
                    h_b = faccb[t]
                    for half in range(2):
                        for c in range(8):
                            Pp(lambda e, c=c, half=half, n=n: e.matmul(out=pO[0:n, half, :], lhsT=mixT[:, c, 0:n], rhs=wout_sb[:, c, half * 512:(half + 1) * 512], start=(c == 0), stop=(c == 7)),
                               r=[mixT_b, wout_b], w=[pOb[half]])
                        Vv(lambda e, half=half, n=n, h_t=h_t, xr_t=xr_t: e.tensor_tensor(out=h_t[0:n, half * 512:(half + 1) * 512], in0=pO[0:n, half, :], in1=xr_t[0:n, half * 512:(half + 1) * 512], op=ALU.add),
                           r=[pOb[half], xr_b], w=[h_b])
                    Aa(lambda e, h_t=h_t, n=n: e.activation(out=xn2[0:n, :], in_=h_t[0:n, :], func=AF.Square, accum_out=rt[0:n, 0:1]), r=[h_b], w=[xn2_b, rt_b])
                    rstd_from_ssq(rt[0:n, 0:1], 1.0 / D, [rt_b])
                    Vv(lambda e, h_t=h_t, n=n: e.scalar_tensor_tensor(out=xn2[0:n, :], in0=h_t[0:n, :], scalar=rt[0:n, 0:1], in1=gffn_t[0:n, :], op0=ALU.mult, op1=ALU.mult), r=[h_b, rt_b, gffn_b], w=[xn2_b])
                    for c in range(8):
                        Pp(lambda e, c=c, n=n: e.transpose(out=pT[:, c * 128:c * 128 + n], in_=xn2[0:n, c * 128:(c + 1) * 128], identity=ident[0:n, 0:n]), r=[xn2_b, ident_b], w=[pT_b])
                    Aa(lambda e, t=t, n=n: e.copy(out=xn2T[:, :, t * 128:t * 128 + n], in_=pT[:, :].rearrange("p (c t) -> p c t", c=8)[:, :, 0:n]), r=[pT_b], w=[xn2T_b])
                    for c in range(8):
                        Pp(lambda e, c=c, t=t, n=n: e.matmul(out=pS[0:n, 0, 0:36], lhsT=xn2T[:, c, t * 128:t * 128 + n], rhs=wr_sb[:, c, :], start=(c == 0), stop=(c == 7)), r=[xn2T_b, wr_b], w=[pSb[0]])
                    L = rt[0:n, 8:44]
                    Vv(lambda e, n=n, L=L: e.tensor_tensor(out=L, in0=pS[0:n, 0, 0:36], in1=br_t[0:n, :], op=ALU.add), r=[pSb[0], br_b], w=[rt_b])
                    gl = rt[0:n, 8:12]; el = rt[0:n, 12:44]
                    Vv(lambda e, n=n, gl=gl: e.tensor_reduce(out=rt[0:n, 1:2], in_=gl, axis=AX.X, op=ALU.max), r=[rt_b], w=[rt_b])
                    Vv(lambda e, n=n, gl=gl: e.tensor_scalar(out=rt[0:n, 44:48], in0=gl, scalar1=rt[0:n, 1:2], scalar2=None, op0=ALU.is_ge), r=[rt_b], w=[rt_b])
                    Vv(lambda e, n=n, gl=gl: e.tensor_scalar(out=rt[0:n, 48:52], in0=gl, scalar1=rt[0:n, 1:2], scalar2=None, op0=ALU.subtract), r=[rt_b], w=[rt_b])
                    Aa(lambda e, n=n: e.activation(out=rt[0:n, 48:52], in_=rt[0:n, 48:52], func=AF.Exp, accum_out=rt[0:n, 2:3]), r=[rt_b], w=[rt_b])
                    Vv(lambda e, n=n: e.reciprocal(out=rt[0:n, 2:3], in_=rt[0:n, 2:3]), r=[rt_b], w=[rt_b])
                    Vv(lambda e, n=n: e.tensor_scalar(out=rt[0:n, 44:48], in0=rt[0:n, 44:48], scalar1=-1.0, scalar2=1.0e9, op0=ALU.add, op1=ALU.mult), r=[rt_b], w=[rt_b])
                    M = rt[0:n, 52:84]
                    Vv(lambda e, n=n, M=M, el=el: e.tensor_tensor(out=M.rearrange("p (g x) -> p g x", g=4), in0=el.rearrange("p (g x) -> p g x", g=4), in1=rt[0:n, 44:48].unsqueeze(2).to_broadcast([n, 4, 8]), op=ALU.add), r=[rt_b], w=[rt_b])
                    Vv(lambda e, n=n, M=M: e.tensor_reduce(out=rt[0:n, 3:4], in_=M, axis=AX.X, op=ALU.max), r=[rt_b], w=[rt_b])
                    O1 = rt[0:n, 84:116]
                    Vv(lambda e, n=n, M=M, O1=O1: e.tensor_scalar(out=O1, in0=M, scalar1=rt[0:n, 3:4], scalar2=None, op0=ALU.is_ge), r=[rt_b], w=[rt_b])
                    Vv(lambda e, n=n, M=M, O1=O1: e.scalar_tensor_tensor(out=M, in0=O1, scalar=-1.0e9, in1=M, op0=ALU.mult, op1=ALU.add), r=[rt_b], w=[rt_b])
                    Vv(lambda e, n=n, M=M: e.tensor_reduce(out=rt[0:n, 4:5], in_=M, axis=AX.X, op=ALU.max), r=[rt_b], w=[rt_b])
                    O2 = rt[0:n, 116:148]
                    Vv(lambda e, n=n, M=M, O2=O2: e.tensor_scalar(out=O2, in0=M, scalar1=rt[0:n, 4:5], scalar2=None, op0=ALU.is_ge), r=[rt_b], w=[rt_b])
                    Vv(lambda e, n=n: e.tensor_tensor(out=rt[0:n, 5:6], in0=rt[0:n, 4:5], in1=rt[0:n, 3:4], op=ALU.subtract), r=[rt_b], w=[rt_b])
                    Aa(lambda e, n=n: e.activation(out=rt[0:n, 5:6], in_=rt[0:n, 5:6], func=AF.Exp), r=[rt_b], w=[rt_b])
                    Vv(lambda e, n=n: e.tensor_scalar(out=rt[0:n, 6:7], in0=rt[0:n, 5:6], scalar1=1.0, scalar2=None, op0=ALU.add), r=[rt_b], w=[rt_b])
                    Vv(lambda e, n=n: e.reciprocal(out=rt[0:n, 6:7], in_=rt[0:n, 6:7]), r=[rt_b], w=[rt_b])
                    Vv(lambda e, n=n: e.tensor_tensor(out=rt[0:n, 6:7], in0=rt[0:n, 6:7], in1=rt[0:n, 2:3], op=ALU.mult), r=[rt_b], w=[rt_b])
                    Vv(lambda e, n=n: e.tensor_tensor(out=rt[0:n, 7:8], in0=rt[0:n, 6:7], in1=rt[0:n, 5:6], op=ALU.mult), r=[rt_b], w=[rt_b])
                    Vv(lambda e, n=n, t=t, O1=O1: e.tensor_scalar(out=gates[0:n, t, :], in0=O1, scalar1=rt[0:n, 6:7], scalar2=None, op0=ALU.mult), r=[rt_b], w=[gates_b])
                    Vv(lambda e, n=n, t=t, O2=O2: e.scalar_tensor_tensor(out=gates[0:n, t, :], in0=O2, scalar=rt[0:n, 7:8], in1=gates[0:n, t, :], op0=ALU.mult, op1=ALU.add), r=[rt_b, gates_b], w=[gates_b])

                items = [(ex, t) for ex in range(NEXP) for t in range(NTT)]
                NI = len(items)

                def load_w(ex):
                    wg_t, wg_b = wgu[ex % 2]
                    wd_t, wd_b = wdn[ex % 2]
                    S.dma("pool", lambda e: e.dma_start(out=wg_t[:, :, 0:256], in_=w_g[ex].rearrange("(c p) n -> p c n", p=128)), w=[wg_b])
                    S.dma("pool", lambda e: e.dma_start(out=wg_t[:, :, 256:512], in_=w_u[ex].rearrange("(c p) n -> p c n", p=128)), w=[wg_b])
                    S.dma("pool", lambda e: e.dma_start(out=wd_t[:, :, :], in_=w_d[ex].rearrange("(c p) n -> p c n", p=128)), w=[wd_b])

                def stage1(i):
                    ex, t = items[i]
                    k2 = i % 2
                    wg_t, wg_b = wgu[ex % 2]
                    for c in range(8):
                        Pp(lambda e, c=c: e.matmul(out=pP[:, k2, :], lhsT=xn2T[:, c, t * 128:(t + 1) * 128], rhs=wg_t[:, c, :], start=(c == 0), stop=(c == 7)),
                           r=[xn2T_b, wg_b], w=[pPb[k2]])
                    s_t, s_b = sil[k2]
                    a_t, a_b = actt[k2]
                    Aa(lambda e: e.activation(out=s_t[:, :], in_=pP[:, k2, 0:256], func=AF.Silu), r=[pPb[k2]], w=[s_b])
                    Vv(lambda e: e.scalar_tensor_tensor(out=a_t[:, :], in0=pP[:, k2, 256:512], scalar=gates[:, t, ex:ex + 1], in1=s_t[:, :], op0=ALU.mult, op1=ALU.mult),
                       r=[pPb[k2], s_b, gates_b], w=[a_b])


                pT_alt = pO[:, 2, :].bitcast(BF16)
                tbanks = [(pT, pT_b), (pT_alt, pOb[2])]

                def stage2(i):
                    k2 = i % 2
                    a_t, a_b = actt[k2]
                    aT_t, aT_b = actT[k2]
                    tb, tb_b = tbanks[k2]
                    for c2 in range(2):
                        Pp(lambda e, c2=c2: e.transpose(out=tb[:, c2 * 128:(c2 + 1) * 128], in_=a_t[:, c2 * 128:(c2 + 1) * 128], identity=ident[:]), r=[a_b, ident_b], w=[tb_b])
                    Aa(lambda e: e.copy(out=aT_t[:, :, :], in_=tb[:, 0:256].rearrange("p (c t) -> p c t", c=2)), r=[tb_b], w=[aT_b])

                def stage3(i):
                    ex, t = items[i]
                    k2 = i % 2
                    aT_t, aT_b = actT[k2]
                    wd_t, wd_b = wdn[ex % 2]
                    for half in range(2):
                        if k2 == 0:
                            oap = pS[:, half, :]; obuf = pSb[half]
                        else:
                            oap = pO[:, half, :]; obuf = pOb[half]
                        for c2 in range(2):
                            Pp(lambda e, c2=c2, half=half, oap=oap: e.matmul(out=oap, lhsT=aT_t[:, c2, :], rhs=wd_t[:, c2, half * 512:(half + 1) * 512], start=(c2 == 0), stop=(c2 == 1)),
                               r=[aT_b, wd_b], w=[obuf])
                        fa = facc[:, t, half * 512:(half + 1) * 512]
                        Vv(lambda e, fa=fa, oap=oap: e.tensor_tensor(out=fa, in0=oap, in1=fa, op=ALU.add), r=[obuf, faccb[t]], w=[faccb[t]])

                load_w(0)
                load_w(1)
                for i in range(NI + 2):
                    if i < NI:
                        stage1(i)
                    if 1 <= i <= NI:
                        stage2(i - 1)
                    if 2 <= i:
                        stage3(i - 2)
                        exd, td = items[i - 2]
                        if td == NTT - 1 and exd + 2 < NEXP:
                            load_w(exd + 2)
                for t in range(NTT):
                    r0, n = rows(t)
                    if t < NT:
                        S.dma("sp", lambda e, r0=r0, n=n, t=t: e.dma_start(out=y_p[r0:r0 + n, :], in_=facc[0:n, t, :]), r=[faccb[t]], is_output=True)
                    else:
                        S.dma("sp", lambda e, n=n, t=t: e.dma_start(out=y_s[0:n, :], in_=facc[0:n, t, :]), r=[faccb[t]], is_output=True)
        S.finish()
    return nc


def _consts():
    half = 32
    inv = (10000.0 ** (-np.arange(half, dtype=np.float32) / half)).astype(np.float32)

    def table(pos):
        ang = pos.astype(np.float32)[:, None] * inv[None, :]
        c = np.cos(ang).astype(np.float32); s = np.sin(ang).astype(np.float32)
        return np.concatenate([c, c, -s, s], axis=1).astype(np.float32)

    rope_p = table(np.arange(T))
    rope_s = table(np.tile(2048 + np.arange(4), NB))
    q = np.arange(128)[:, None]; k = np.arange(128)[None, :]
    cm = np.where(k <= q, 0.0, NEG).astype(np.float32)
    cm30 = np.where(k <= q, 0.0, -BIG).astype(np.float32)
    r = np.arange(NS)
    same = (r[:, None] // 4) == (r[None, :] // 4)
    caus = (r[None, :] % 4) <= (r[:, None] % 4)
    nbq = np.where(same & caus, 0.0, NEG).astype(np.float32)
    nb30 = np.where(np.arange(4)[None, :] <= (r[:, None] % 4), 0.0, -BIG).astype(np.float32)
    bdiag = same.astype(np.float32)
    bsel = np.full((NB, 8, NS), NEG, np.float32)
    for b in range(NB):
        bsel[b, :, b * 4:(b + 1) * 4] = 0.0
    i8 = np.tile(np.eye(64, dtype=np.float32), (1, 8))
    return dict(rope_p=rope_p, rope_s=rope_s, c_ident=np.eye(128, dtype=np.float32), c_cm=cm, c_cm30=cm30,
                c_nbq=nbq, c_nb30=nb30, c_bdiag=bdiag, c_bsel=bsel.reshape(1, NB * 512), c_i8=i8)


def make_in_maps(inp, cores):
    f = lambda a: np.ascontiguousarray(np.asarray(a, dtype=np.float32))
    n_pool = inp["cache_a_k"].shape[1]
    cons = _consts()
    shared = dict(
        cak=f(inp["cache_a_k"]).reshape(n_pool * 128, 512), cav=f(inp["cache_a_v"]).reshape(n_pool * 128, 512),
        cbk=f(inp["cache_b_k"]).reshape(n_pool * 128, 512), cbv=f(inp["cache_b_v"]).reshape(n_pool * 128, 512),
        cik=f(inp["cache_idx_k"]).reshape(n_pool * 128, 64),
        g_mix=f(inp["g_mix"]).reshape(1, D), w_in=f(inp["w_in"]).reshape(D, INW),
        gains=np.concatenate([f(inp[k]).reshape(1, 64) for k in ("q_norm_a", "k_norm_a", "q_norm_b", "k_norm_b")], axis=1),
        lams=np.concatenate([f(inp[k]).reshape(1, 64) for k in ("lambda_q1", "lambda_k1", "lambda_q2", "lambda_k2")], axis=1),
        subln=f(inp["subln_b"]).reshape(1, 128), w_out=f(inp["w_out"]).reshape(D, D), g_ffn=f(inp["g_ffn"]).reshape(1, D),
        w_r=np.concatenate([f(inp["w_router_group"]).reshape(D, 4), f(inp["w_router_expert"]).reshape(D, 32)], axis=1),
        b_r=np.concatenate([f(inp["b_router_group"]).reshape(1, 4), f(inp["b_router_expert"]).reshape(1, 32)], axis=1),
        w_g=f(inp["w_exp_gate"]).reshape(NEXP, D, DE), w_u=f(inp["w_exp_up"]).reshape(NEXP, D, DE), w_d=f(inp["w_exp_down"]).reshape(NEXP, DE, D),
        **cons)
    xp = f(inp["x_prompt"]); xs = f(inp["x_sample"])
    pt = np.ascontiguousarray(np.asarray(inp["page_table"], dtype=np.int32))
    maps = []
    for c in cores:
        m = dict(shared)
        m["xp"] = xp[c]
        m["xs"] = xs[c * NB:(c + 1) * NB].reshape(NS, D)
        m["ptab"] = pt[c * NB:(c + 1) * NB].reshape(1, NB * NPG)
        m["ptabT"] = np.ascontiguousarray(pt[c * NB:(c + 1) * NB].T).reshape(1, NPG * NB)
        maps.append(m)
    return maps, n_pool


def assemble(results, n_cores=8):
    def cat(name, shp):
        return np.stack([np.asarray(r[name], dtype=np.float32) for r in results], axis=0).reshape(shp)
    B = n_cores
    y_p = cat("y_p", (B, T, D)); y_s = cat("y_s", (B * NB, 4, D))
    return (y_p, y_s,
            cat("oak_p", (1, B, T, 8, 64)), cat("oav_p", (1, B, T, 8, 64)), cat("oik_p", (1, B, T, 64)),
            cat("obk_p", (1, B, T, 4, 2, 64)), cat("obv_p", (1, B, T, 4, 128)),
            cat("oak_s", (1, B * NB, 4, 8, 64)), cat("oav_s", (1, B * NB, 4, 8, 64)), cat("oik_s", (1, B * NB, 4, 64)),
            cat("obk_s", (1, B * NB, 4, 4, 2, 64)), cat("obv_s", (1, B * NB, 4, 4, 128)))


def kernel(**inputs):
    maps, n_pool = make_in_maps(inputs, list(range(8)))
    nc = build(n_pool)
    res = run_bass_kernel_spmd(nc, maps, core_ids=list(range(8)))
    return assemble(res.results, 8)
```

```python
import contextlib
import numpy as np
import concourse.bass as bass
import concourse.mybir as mybir
from concourse.bass_utils import run_bass_kernel_spmd

F32 = mybir.dt.float32
BF16 = mybir.dt.bfloat16
I32 = mybir.dt.int32
ALU = mybir.AluOpType
AF = mybir.ActivationFunctionType
AX = mybir.AxisListType

D = 1024
T = 2048
NT = 16
NS = 64
NB = 16
NPG = 16
INW = 3656
NEXP = 32
DE = 256
NEG = -30000.0
BIG = 1.0e30
EPS = 1e-6
TOPK = 256
NBIS = 14


class Buf:
    __slots__ = ("name", "last_w", "reads")

    def __init__(self, name):
        self.name = name
        self.last_w = None
        self.reads = []


class Sched:
    ENG = ("pe", "dve", "act", "pool", "sp")

    def __init__(self, nc, es, n_dma_sems=32, same_engine_sync=("pool", "dve", "act")):
        self.nc = nc
        self.count = {e: 0 for e in self.ENG}
        self.waited = {e: {} for e in self.ENG}
        self.n_dma_sems = n_dma_sems
        self.dma_rr = 0
        self.dma_cnt = [0] * n_dma_sems
        self.same_engine_sync = set(same_engine_sync)
        self.out_tokens = []
        self.sem = {}
        for e in self.ENG:
            self.sem["e_" + e] = es.enter_context(nc.semaphore("se_" + e))
        for i in range(n_dma_sems):
            self.sem["d%d" % i] = es.enter_context(nc.semaphore("sd%d" % i))
        self.eobj = {"pe": nc.tensor, "dve": nc.vector, "act": nc.scalar,
                     "pool": nc.gpsimd, "sp": nc.sync}

    def _deps(self, eng, reads, writes):
        deps = []
        for b in list(reads) + list(writes):
            if b.last_w is not None:
                deps.append(b.last_w)
        for b in writes:
            deps.extend(b.reads)
        waits = []
        for (sk, val, src_eng) in deps:
            if src_eng == eng and eng not in self.same_engine_sync and not sk.startswith("d"):
                continue
            if self.waited[eng].get(sk, 0) >= val:
                continue
            self.waited[eng][sk] = val
            waits.append((sk, val))
        return waits

    def _commit(self, tok, reads, writes):
        for b in reads:
            b.reads.append(tok)
            if len(b.reads) > 64:
                best = {}
                for t in b.reads:
                    if t[0] not in best or best[t[0]][1] < t[1]:
                        best[t[0]] = t
                b.reads = list(best.values())
        for b in writes:
            b.last_w = tok
            b.reads = []

    def _emit(self, eng, waits, fn, inc):
        e = self.eobj[eng]
        for (wk, wv) in waits:
            e.wait_ge(self.sem[wk], wv)
        fn(e).then_inc(self.sem[inc[0]], inc[1])

    def op(self, eng, fn, r=(), w=()):
        waits = self._deps(eng, r, w)
        self.count[eng] += 1
        tok = ("e_" + eng, self.count[eng], eng)
        self._emit(eng, waits, fn, ("e_" + eng, 1))
        self._commit(tok, r, w)
        return tok

    def dma(self, eng, fn, r=(), w=(), is_output=False):
        waits = self._deps(eng, r, w)
        i = self.dma_rr
        self.dma_rr = (self.dma_rr + 1) % self.n_dma_sems
        sk = "d%d" % i
        prev = self.dma_cnt[i]
        if prev > 0 and self.waited[eng].get(sk, 0) < prev:
            self.waited[eng][sk] = prev
            waits.append((sk, prev))
        self.dma_cnt[i] += 16
        tok = (sk, self.dma_cnt[i], eng)
        self._emit(eng, waits, fn, (sk, 16))
        self._commit(tok, r, w)
        if is_output:
            self.out_tokens.append(tok)
        return tok

    def barrier(self):
        for eng in self.ENG:
            e = self.eobj[eng]
            for f in self.ENG:
                if f == eng or self.count[f] == 0:
                    continue
                sk = "e_" + f
                if self.waited[eng].get(sk, 0) < self.count[f]:
                    self.waited[eng][sk] = self.count[f]
                    e.wait_ge(self.sem[sk], self.count[f])
            for i in range(self.n_dma_sems):
                sk = "d%d" % i
                if self.dma_cnt[i] > 0 and self.waited[eng].get(sk, 0) < self.dma_cnt[i]:
                    self.waited[eng][sk] = self.dma_cnt[i]
                    e.wait_ge(self.sem[sk], self.dma_cnt[i])

    def finish(self):
        seen = {}
        for (sk, val, _e) in self.out_tokens:
            seen[sk] = max(seen.get(sk, 0), val)
        for sk, val in seen.items():
            self.eobj["sp"].wait_ge(self.sem[sk], val)


def build(n_pool, phases=("A", "S", "B"), debug=False):
    nc = bass.Bass("TRN2", target_bir_lowering=False)

    def din(name, shape, dt=F32):
        return nc.dram_tensor(name, list(shape), dt, kind="ExternalInput").ap()

    def dout(name, shape, dt=F32):
        return nc.dram_tensor(name, list(shape), dt, kind="ExternalOutput").ap()

    xp = din("xp", [T, D]); xs = din("xs", [NS, D])
    cak = din("cak", [n_pool * 128, 512]); cav = din("cav", [n_pool * 128, 512])
    cbk = din("cbk", [n_pool * 128, 512]); cbv = din("cbv", [n_pool * 128, 512])
    cik = din("cik", [n_pool * 128, 64])
    ptab = din("ptab", [1, NB * NPG], I32)
    ptabT = din("ptabT", [1, NPG * NB], I32)
    g_mix = din("g_mix", [1, D]); w_in = din("w_in", [D, INW])
    gains = din("gains", [1, 4 * 64])
    lams = din("lams", [1, 4 * 64])
    subln = din("subln", [1, 128]); w_out = din("w_out", [D, D]); g_ffn = din("g_ffn", [1, D])
    w_r = din("w_r", [D, 36]); b_r = din("b_r", [1, 36])
    w_g = din("w_g", [NEXP, D, DE]); w_u = din("w_u", [NEXP, D, DE]); w_d = din("w_d", [NEXP, DE, D])
    rope_p = din("rope_p", [T, 128]); rope_s = din("rope_s", [NS, 128])
    c_ident = din("c_ident", [128, 128]); c_cm = din("c_cm", [128, 128]); c_cm30 = din("c_cm30", [128, 128])
    c_nbq = din("c_nbq", [64, 64]); c_nb30 = din("c_nb30", [64, 4]); c_bdiag = din("c_bdiag", [64, 64])
    c_bsel = din("c_bsel", [1, NB * 512]); c_i8 = din("c_i8", [64, 512])

    y_p = dout("y_p", [T, D]); y_s = dout("y_s", [NS, D])
    o_p = {k: dout("o%s_p" % k, [T, n]) for k, n in (("ak", 512), ("av", 512), ("ik", 64), ("bk", 512), ("bv", 512))}
    o_s = {k: dout("o%s_s" % k, [NS, n]) for k, n in (("ak", 512), ("av", 512), ("ik", 64), ("bk", 512), ("bv", 512))}
    mbuf = nc.dram_tensor("mbuf", [T + NS, D], BF16, kind="Internal").ap()
    if debug:
        dbg_mixed = dout("dbg_mixed", [NS, D])
        dbg_scs = dout("dbg_scs", [NS, 2052])
        dbg_mbs = dout("dbg_mbs", [NS, 2052])
        dbg_z = dout("dbg_z", [NS, 16])
    sgnbuf = nc.dram_tensor("sgnbuf", [NS, 8], F32, kind="Internal").ap()

    with contextlib.ExitStack() as es:
        S = Sched(nc, es)

        def sbt(stack, name, shape, dt):
            t = stack.enter_context(nc.sbuf_tensor(name, list(shape), dt))
            return t, Buf(name)

        def pst(stack, name, shape, dt):
            t = stack.enter_context(nc.psum_tensor(name, list(shape), dt))
            return t, Buf(name)

        def Vv(fn, r=(), w=()): return S.op("dve", fn, r, w)
        def Aa(fn, r=(), w=()): return S.op("act", fn, r, w)
        def Gg(fn, r=(), w=()): return S.op("pool", fn, r, w)
        def Pp(fn, r=(), w=()): return S.op("pe", fn, r, w)

        pP, pP_b = pst(es, "pP", [128, 2, 512], F32)
        pPb = [Buf("pP0"), Buf("pP1")]
        pT, pT_b = pst(es, "pT", [128, 1024], BF16)
        pS, _ = pst(es, "pS", [128, 2, 512], F32)
        pSb = [Buf("pS0"), Buf("pS1")]
        pO, _ = pst(es, "pO", [128, 3, 512], F32)
        pOb = [Buf("pO0"), Buf("pO1"), Buf("pO2")]

        identf, identf_b = sbt(es, "identf", [128, 128], F32)
        ident, ident_b = sbt(es, "ident", [128, 128], BF16)
        cm, cm_b = sbt(es, "cm", [128, 128], BF16)
        cm30, cm30_b = sbt(es, "cm30", [128, 128], F32)
        gmix_t, gmix_b = sbt(es, "gmix_t", [128, D], F32)
        Gt, Gt_b = sbt(es, "Gt", [128, 5, 64], F32)
        Gs, Gs_b = sbt(es, "Gs", [128, 5, 64], F32)
        subln_t, subln_b = sbt(es, "subln_t", [128, 128], F32)
        br_t, br_b = sbt(es, "br_t", [128, 36], F32)
        lam_t, lam_b = sbt(es, "lam_t", [128, 4, 64], F32)
        lamv, lamv_b = sbt(es, "lamv", [128, 4], F32)
        half_t, half_b = sbt(es, "half_t", [128, 1], F32)

        S.dma("sp", lambda e: e.dma_start(out=identf[:], in_=c_ident[:, :]), w=[identf_b])
        Vv(lambda e: e.tensor_copy(out=ident[:], in_=identf[:]), r=[identf_b], w=[ident_b])
        S.dma("pool", lambda e: e.dma_start(out=cm[:], in_=c_cm[:, :]), w=[cm_b])
        S.dma("sp", lambda e: e.dma_start(out=cm30[:], in_=c_cm30[:, :]), w=[cm30_b])
        S.dma("sp", lambda e: e.dma_start(out=gmix_t[:], in_=g_mix.partition_broadcast(128)), w=[gmix_b])
        S.dma("sp", lambda e: e.dma_start(out=subln_t[:], in_=subln.partition_broadcast(128)), w=[subln_b])
        S.dma("sp", lambda e: e.dma_start(out=br_t[:], in_=b_r.partition_broadcast(128)), w=[br_b])
        S.dma("sp", lambda e: e.dma_start(out=lam_t[:].rearrange("p a b -> p (a b)"), in_=lams.partition_broadcast(128)), w=[lam_b])
        Vv(lambda e: e.memset(Gt[:], 1.0), w=[Gt_b])
        Vv(lambda e: e.memset(half_t[:], 0.5), w=[half_b])
        S.dma("sp", lambda e: e.dma_start(out=Gt[:, 0:4, :].rearrange("p a b -> p (a b)"), in_=gains.partition_broadcast(128)), w=[Gt_b])
        Vv(lambda e: e.tensor_copy(out=Gs[:, :, 0:32], in_=Gt[:, :, 32:64]), r=[Gt_b], w=[Gs_b])
        Vv(lambda e: e.tensor_copy(out=Gs[:, :, 32:64], in_=Gt[:, :, 0:32]), r=[Gt_b], w=[Gs_b])
        Vv(lambda e: e.tensor_tensor(out=lam_t[:, 0, :], in0=lam_t[:, 0, :], in1=lam_t[:, 1, :], op=ALU.mult), r=[lam_b], w=[lam_b])
        Vv(lambda e: e.tensor_tensor(out=lam_t[:, 2, :], in0=lam_t[:, 2, :], in1=lam_t[:, 3, :], op=ALU.mult), r=[lam_b], w=[lam_b])
        Vv(lambda e: e.tensor_reduce(out=lamv[:, 0:1], in_=lam_t[:, 0, :], axis=AX.X, op=ALU.add), r=[lam_b], w=[lamv_b])
        Vv(lambda e: e.tensor_reduce(out=lamv[:, 1:2], in_=lam_t[:, 2, :], axis=AX.X, op=ALU.add), r=[lam_b], w=[lamv_b])
        Aa(lambda e: e.activation(out=lamv[:, 0:2], in_=lamv[:, 0:2], func=AF.Exp), r=[lamv_b], w=[lamv_b])
        Vv(lambda e: e.tensor_tensor(out=lamv[:, 2:3], in0=lamv[:, 1:2], in1=lamv[:, 0:1], op=ALU.subtract), r=[lamv_b], w=[lamv_b])
        Vv(lambda e: e.tensor_scalar(out=lamv[:, 3:4], in0=lamv[:, 2:3], scalar1=-0.2, scalar2=None, op0=ALU.add), r=[lamv_b], w=[lamv_b])
        neglam = lamv[:, 3:4]

        def rstd_from_ssq(t_ap, n_inv, bufs):
            Vv(lambda e: e.tensor_scalar(out=t_ap, in0=t_ap, scalar1=n_inv, scalar2=EPS, op0=ALU.mult, op1=ALU.add), r=bufs, w=bufs)
            Aa(lambda e: e.activation(out=t_ap, in_=t_ap, func=AF.Ln), r=bufs, w=bufs)
            Aa(lambda e: e.activation(out=t_ap, in_=t_ap, func=AF.Exp, scale=-0.5), r=bufs, w=bufs)

        mixed_s, mixed_s_b = sbt(es, "mixed_s", [NS, D], BF16)

        hbuf_b = Buf("hbuf")
        mbuf_b = Buf("mbuf")

        sA = es.enter_context(contextlib.ExitStack())
        sq_s = {}
        for nm, shp, dt in (("aqT_s", [128, 4, NS], BF16), ("bqT_s", [128, 4, NS], BF16), ("iqT_s8", [64, 8, NS], BF16),
                            ("akT_s", [128, 4, NS], BF16), ("bkT_s", [128, 4, NS], BF16), ("ikT_s", [128, NS], BF16),
                            ("av_s", [NS, 512], BF16), ("bv_s", [NS, 512], BF16), ("wsg_s", [NS, 8], F32)):
            sq_s[nm] = sbt(es, nm, shp, dt)

        with contextlib.ExitStack() as pa:
            win_sb, win_b = sbt(pa, "win_sb", [128, 8, INW], BF16)
            akT, _ = sbt(pa, "akT", [128, 4, T], BF16)
            bkT, _ = sbt(pa, "bkT", [128, 4, T], BF16)
            ikT2, _ = sbt(pa, "ikT2", [128, T], BF16)
            av_sb, _ = sbt(pa, "av_sb", [128, NT, 8, 65], BF16)
            bv_sb, _ = sbt(pa, "bv_sb", [128, NT, 4, 129], BF16)
            kvb = [Buf("kv%d" % i) for i in range(NT)]
            xt, xt_b = sbt(pa, "xt", [128, D], F32)
            xn, xn_b = sbt(pa, "xn", [128, D], BF16)
            xnT, xnT_b = sbt(pa, "xnT", [128, 8, 128], BF16)
            hs, _ = sbt(pa, "hs", [128, 2, 512], F32)
            hsb = [Buf("hs0"), Buf("hs1")]
            kvout, kvout_b = sbt(pa, "kvout", [128, 1088], F32)
            qtm, qtm_b = sbt(pa, "qtm", [128, 5 * 512 + 128], BF16)
            qTs = [sbt(pa, "qT%d" % k, [128, 3, 4, 128], BF16) for k in range(2)]
            rp, rp_b = sbt(pa, "rp", [128, 128], F32)
            Ct, Ct_b = sbt(pa, "Ct", [128, 5, 64], F32)
            St, St_b = sbt(pa, "St", [128, 5, 64], F32)
            st8, st8_b = sbt(pa, "st8", [128, 16], F32)
            wsg, wsg_b = sbt(pa, "wsg", [128, 3, 8], F32)
            t2, t2_b = sbt(pa, "t2", [128, 512], F32)
            sc, sc_b = sbt(pa, "sc", [128, T], F32)
            rr, _ = sbt(pa, "rr", [128, 3, 512], BF16)
            rrb = [Buf("rr0"), Buf("rr1"), Buf("rr2")]
            dg, dg_b = sbt(pa, "dg", [128, 8, 128], BF16)
            mbs2 = [sbt(pa, "mb%d" % k, [128, T], BF16) for k in range(2)]
            bis, bis_b = sbt(pa, "bis", [128, 8], F32)
            PT, _ = sbt(pa, "PT", [128, 3, 512], BF16)
            PTb = [Buf("PT0"), Buf("PT1"), Buf("PT2")]
            mixed, mixed_b = sbt(pa, "mixed", [128, D], BF16)
            osm, osm_b = sbt(pa, "osm", [128, 8], F32)
            bo, bo_b = sbt(pa, "bo", [128, 2, 128], F32)

            iqs, iqs_b = sbt(pa, "iqs", [128, 512], F32)
            print("phaseA sbuf remaining", nc.sbuf_bytes_remaining)
            S.dma("pool", lambda e: e.dma_start(out=win_sb[:, :, 0:2048], in_=w_in.rearrange("(c p) n -> p c n", p=128)[:, :, 0:2048]), w=[win_b])
            S.dma("pool", lambda e: e.dma_start(out=win_sb[:, :, 2048:INW], in_=w_in.rearrange("(c p) n -> p c n", p=128)[:, :, 2048:INW]), w=[win_b])
            Vv(lambda e: e.memset(av_sb[:, :, :, 64:65], 1.0), w=kvb)
            Vv(lambda e: e.memset(bv_sb[:, :, :, 128:129], 1.0), w=kvb)

            def project_tile(x_src, rope_src, n, outs, row0, ti):
                S.dma("sp", lambda e: e.dma_start(out=xt[0:n, :], in_=x_src), w=[xt_b])
                S.dma("sp", lambda e: e.dma_start(out=rp[0:n, :], in_=rope_src), w=[rp_b])
                Aa(lambda e: e.activation(out=xn[0:n, :], in_=xt[0:n, :], func=AF.Square, accum_out=st8[0:n, 0:1]), r=[xt_b], w=[xn_b, st8_b])
                rstd_from_ssq(st8[0:n, 0:1], 1.0 / D, [st8_b])
                Vv(lambda e: e.scalar_tensor_tensor(out=xn[0:n, :], in0=xt[0:n, :], scalar=st8[0:n, 0:1], in1=gmix_t[0:n, :], op0=ALU.mult, op1=ALU.mult),
                   r=[xt_b, st8_b, gmix_b], w=[xn_b])
                for c in range(8):
                    Pp(lambda e, c=c: e.transpose(out=pT[:, c * 128:c * 128 + n], in_=xn[0:n, c * 128:(c + 1) * 128], identity=ident[0:n, 0:n]),
                       r=[xn_b, ident_b], w=[pT_b])
                Aa(lambda e: e.copy(out=xnT[:, :, 0:n], in_=pT[:, :].rearrange("p (c t) -> p c t", c=8)[:, :, 0:n]), r=[pT_b], w=[xnT_b])
                Vv(lambda e: e.tensor_tensor(out=Ct[0:n], in0=Gt[0:n], in1=rp[0:n, 0:64].unsqueeze(1).to_broadcast([n, 5, 64]), op=ALU.mult), r=[Gt_b, rp_b], w=[Ct_b])
                Vv(lambda e: e.tensor_tensor(out=St[0:n], in0=Gs[0:n], in1=rp[0:n, 64:128].unsqueeze(1).to_broadcast([n, 5, 64]), op=ALU.mult), r=[Gs_b, rp_b], w=[St_b])

                chunks = [("aq", 0, 512), ("ak", 512, 512), ("av", 1024, 512), ("bq", 1536, 512), ("bk", 2048, 512),
                          ("bv", 2560, 512), ("iq", 3072, 512), ("ikw", 3584, 72)]
                kvo = {"ak": 0, "ik": 512, "bk": 576}
                for ci, (nm, c0, cw) in enumerate(chunks):
                    pb = ci % 2
                    for c in range(8):
                        Pp(lambda e, c=c, pb=pb, c0=c0, cw=cw: e.matmul(out=pP[0:n, pb, 0:cw], lhsT=xnT[:, c, 0:n], rhs=win_sb[:, c, c0:c0 + cw], start=(c == 0), stop=(c == 7)),
                           r=[xnT_b, win_b], w=[pPb[pb]])
                    hsl = hs[0:n, pb, 0:cw]
                    if ci % 2 == 0:
                        Aa(lambda e, pb=pb, cw=cw, hsl=hsl: e.copy(out=hsl, in_=pP[0:n, pb, 0:cw]), r=[pPb[pb]], w=[hsb[pb]])
                    else:
                        Vv(lambda e, pb=pb, cw=cw, hsl=hsl: e.tensor_copy(out=hsl, in_=pP[0:n, pb, 0:cw]), r=[pPb[pb]], w=[hsb[pb]])
                    hb = hsb[pb]
                    if nm in ("av", "bv"):
                        S.dma("sp", lambda e, hsl=hsl, nm=nm: e.dma_start(out=outs[nm][row0:row0 + n, :], in_=hsl), r=[hb], is_output=True)
                        if ti is not None:
                            if nm == "av":
                                Vv(lambda e, hsl=hsl: e.tensor_copy(out=av_sb[:, ti, :, 0:64], in_=hsl.rearrange("p (h d) -> p h d", h=8)), r=[hb], w=[kvb[ti]])
                            else:
                                Vv(lambda e, hsl=hsl: e.tensor_copy(out=bv_sb[:, ti, :, 0:128], in_=hsl.rearrange("p (h d) -> p h d", h=4)), r=[hb], w=[kvb[ti]])
                        else:
                            dst = sq_s["av_s"] if nm == "av" else sq_s["bv_s"]
                            Vv(lambda e, hsl=hsl, dst=dst: e.tensor_copy(out=dst[0][:, :], in_=hsl), r=[hb], w=[dst[1]])
                        continue
                    if nm == "ikw":
                        Vv(lambda e: e.tensor_scalar(out=wsg[0:n, 0, :], in0=hs[0:n, pb, 64:72], scalar1=float(8 ** -0.5 * 64 ** -0.5), scalar2=None, op0=ALU.mult), r=[hb], w=[wsg_b])
                        Vv(lambda e: e.tensor_scalar(out=wsg[0:n, 2, :], in0=wsg[0:n, 0, :], scalar1=0.0, scalar2=2.0, op0=ALU.is_ge, op1=ALU.mult), r=[wsg_b], w=[wsg_b])
                        Vv(lambda e: e.tensor_scalar(out=wsg[0:n, 2, :], in0=wsg[0:n, 2, :], scalar1=-1.0, scalar2=None, op0=ALU.add), r=[wsg_b], w=[wsg_b])
                        Vv(lambda e: e.tensor_tensor(out=wsg[0:n, 1, :], in0=wsg[0:n, 0, :], in1=wsg[0:n, 2, :], op=ALU.mult), r=[wsg_b], w=[wsg_b])
                    sec = {"aq": 0, "ak": 1, "bq": 2, "bk": 3, "iq": 4, "ikw": 4}[nm]
                    nh = 1 if nm == "ikw" else 8
                    w_ = 64 * nh
                    src = hs[0:n, pb, 0:w_]
                    src3 = src.rearrange("p (h d) -> p h d", h=nh)
                    if sec < 4:
                        Aa(lambda e, src=src, w_=w_: e.activation(out=t2[0:n, 0:w_], in_=src, func=AF.Square), r=[hb], w=[t2_b])
                        Vv(lambda e, w_=w_, nh=nh: e.tensor_reduce(out=st8[0:n, 8:8 + nh], in_=t2[0:n, 0:w_].rearrange("p (h d) -> p h d", h=nh), axis=AX.X, op=ALU.add), r=[t2_b], w=[st8_b])
                        rstd_from_ssq(st8[0:n, 8:8 + nh], 1.0 / 64, [st8_b])
                        Vv(lambda e, src3=src3, nh=nh: e.tensor_tensor(out=src3, in0=src3, in1=st8[0:n, 8:8 + nh].unsqueeze(2).to_broadcast([n, nh, 64]), op=ALU.mult), r=[hb, st8_b], w=[hb])
                    t23 = t2[0:n, 0:w_].rearrange("p (h d) -> p h d", h=nh)
                    Gg(lambda e, src3=src3, t23=t23, nh=nh, sec=sec: e.tensor_tensor(out=t23[:, :, 0:32], in0=src3[:, :, 32:64], in1=St[0:n, sec, 0:32].unsqueeze(1).to_broadcast([n, nh, 32]), op=ALU.mult), r=[hb, St_b], w=[t2_b])
                    Gg(lambda e, src3=src3, t23=t23, nh=nh, sec=sec: e.tensor_tensor(out=t23[:, :, 32:64], in0=src3[:, :, 0:32], in1=St[0:n, sec, 32:64].unsqueeze(1).to_broadcast([n, nh, 32]), op=ALU.mult), r=[hb, St_b], w=[t2_b])
                    Vv(lambda e, src3=src3, nh=nh, sec=sec: e.tensor_tensor(out=src3, in0=src3, in1=Ct[0:n, sec, :].unsqueeze(1).to_broadcast([n, nh, 64]), op=ALU.mult), r=[hb, Ct_b], w=[hb])
                    if nm == "iq":
                        Vv(lambda e, src=src, w_=w_: e.tensor_tensor(out=iqs[0:n, 0:512], in0=src, in1=t2[0:n, 0:w_], op=ALU.add), r=[hb, t2_b], w=[iqs_b])
                        continue
                    if nm in ("ak", "bk"):
                        Vv(lambda e, src=src, nm=nm: e.tensor_tensor(out=kvout[0:n, kvo[nm]:kvo[nm] + 512], in0=src, in1=t2[0:n, 0:512], op=ALU.add), r=[hb, t2_b], w=[kvout_b])
                        q0 = 512 if nm == "ak" else 1536
                        Aa(lambda e, nm=nm, q0=q0: e.copy(out=qtm[0:n, q0:q0 + 512], in_=kvout[0:n, kvo[nm]:kvo[nm] + 512]), r=[kvout_b], w=[qtm_b])
                    elif nm == "ikw":
                        Vv(lambda e, src=src: e.tensor_tensor(out=kvout[0:n, 512:576], in0=src, in1=t2[0:n, 0:64], op=ALU.add), r=[hb, t2_b], w=[kvout_b])
                        Aa(lambda e: e.copy(out=qtm[0:n, 2560:2624], in_=kvout[0:n, 512:576]), r=[kvout_b], w=[qtm_b])
                        Aa(lambda e: e.copy(out=qtm[0:n, 2624:2688], in_=kvout[0:n, 512:576]), r=[kvout_b], w=[qtm_b])
                        Vv(lambda e: e.tensor_tensor(out=qtm[0:n, 2048:2560].rearrange("p (h d) -> p h d", h=8), in0=iqs[0:n, 0:512].rearrange("p (h d) -> p h d", h=8),
                                                      in1=wsg[0:n, 1, :].unsqueeze(2).to_broadcast([n, 8, 64]), op=ALU.mult), r=[iqs_b, wsg_b], w=[qtm_b])
                    else:
                        q0 = 0 if nm == "aq" else 1024
                        Vv(lambda e, src=src, q0=q0: e.tensor_tensor(out=qtm[0:n, q0:q0 + 512], in0=src, in1=t2[0:n, 0:512], op=ALU.add), r=[hb, t2_b], w=[qtm_b])
                for k, (o0, ow) in (("ak", (0, 512)), ("ik", (512, 64)), ("bk", (576, 512))):
                    S.dma("sp", lambda e, k=k, o0=o0, ow=ow: e.dma_start(out=outs[k][row0:row0 + n, :], in_=kvout[0:n, o0:o0 + ow]), r=[kvout_b], is_output=True)

            def transposes_tile(n, ti):
                qT, qT_b = qTs[(ti or 0) % 2]
                for gi, (q0, dst) in enumerate(((0, 0), (1024, 1), (2048, 2))):
                    for j in range(4):
                        Pp(lambda e, j=j, q0=q0: e.transpose(out=pT[:, j * 128:j * 128 + n], in_=qtm[0:n, q0 + j * 128:q0 + (j + 1) * 128], identity=ident[0:n, 0:n]), r=[qtm_b, ident_b], w=[pT_b])
                    if ti is not None:
                        Vv(lambda e, dst=dst: e.tensor_copy(out=qT[:, dst, :, :], in_=pT[:, 0:512].rearrange("p (j t) -> p j t", j=4)), r=[pT_b], w=[qT_b])
                    else:
                        if dst < 2:
                            d_, db_ = sq_s["aqT_s" if dst == 0 else "bqT_s"]
                            Vv(lambda e, d_=d_: e.tensor_copy(out=d_[:, :, :], in_=pT[:, 0:512].rearrange("p (j t) -> p j t", j=4)[:, :, 0:n]), r=[pT_b], w=[db_])
                for gi, q0 in enumerate((512, 1536)):
                    for j in range(4):
                        Pp(lambda e, j=j, q0=q0: e.transpose(out=pT[:, j * 128:j * 128 + n], in_=qtm[0:n, q0 + j * 128:q0 + (j + 1) * 128], identity=ident[0:n, 0:n]), r=[qtm_b, ident_b], w=[pT_b])
                    if ti is not None:
                        d_ = akT if gi == 0 else bkT
                        Aa(lambda e, d_=d_: e.copy(out=d_[:, :, ti * 128:(ti + 1) * 128], in_=pT[:, 0:512].rearrange("p (j t) -> p j t", j=4)), r=[pT_b], w=[kvb[ti]])
                    else:
                        d_, db_ = sq_s["akT_s" if gi == 0 else "bkT_s"]
                        Aa(lambda e, d_=d_: e.copy(out=d_[:, :, :], in_=pT[:, 0:512].rearrange("p (j t) -> p j t", j=4)[:, :, 0:n]), r=[pT_b], w=[db_])
                Pp(lambda e: e.transpose(out=pT[:, 0:n], in_=qtm[0:n, 2560:2688], identity=ident[0:n, 0:n]), r=[qtm_b, ident_b], w=[pT_b])
                if ti is not None:
                    Vv(lambda e: e.tensor_copy(out=ikT2[:, ti * 128:(ti + 1) * 128], in_=pT[:, 0:128]), r=[pT_b], w=[kvb[ti]])
                else:
                    Vv(lambda e: e.tensor_copy(out=sq_s["ikT_s"][0][:, :], in_=pT[:, 0:n]), r=[pT_b], w=[sq_s["ikT_s"][1]])
                    for h in range(8):
                        Pp(lambda e, h=h: e.transpose(out=pT[0:64, h * 64:h * 64 + n], in_=qtm[0:n, 2048 + h * 64:2048 + (h + 1) * 64], identity=ident[0:n, 0:n]), r=[qtm_b, ident_b], w=[pT_b])
                    Vv(lambda e: e.tensor_copy(out=sq_s["iqT_s8"][0][:, :, :], in_=pT[0:64, 0:512].rearrange("p (h t) -> p h t", h=8)), r=[pT_b], w=[sq_s["iqT_s8"][1]])
                    Vv(lambda e: e.tensor_copy(out=sq_s["wsg_s"][0][:, :], in_=wsg[0:n, 2, :]), r=[wsg_b], w=[sq_s["wsg_s"][1]])

            def index_tile(i):
                nk = 128 * (i + 1)
                use_topk = i >= 2
                qT, qT_b = qTs[i % 2]
                mb, mb_b = mbs2[i % 2]
                if use_topk:
                    for h in range(8):
                        Vv(lambda e, h=h: e.tensor_scalar(out=dg[:, h, :], in0=identf[:], scalar1=wsg[:, 2, h:h + 1], scalar2=None, op0=ALU.mult), r=[identf_b, wsg_b], w=[dg_b])
                    nch = (nk + 511) // 512
                    ibanks = [(pP[:, 0, :], pPb[0]), (pP[:, 1, :], pPb[1]), (pO[:, 0, :], pOb[0])]
                    iunits = [(kc, h) for kc in range(nch) for h in range(8)]

                    def idx_S(u):
                        kc, h = iunits[u]
                        k0 = kc * 512; kw = min(512, nk - k0)
                        kread = [kvb[t_] for t_ in range(k0 // 128, (k0 + kw) // 128)]
                        hp = (h % 2) * 64
                        bank, bankb = ibanks[u % 3]
                        Pp(lambda e: e.matmul(out=bank[:, 0:kw], lhsT=qT[hp:hp + 64, 2, h // 2, :], rhs=ikT2[hp:hp + 64, k0:k0 + kw], start=True, stop=True), r=[qT_b] + kread, w=[bankb])
                        Aa(lambda e: e.activation(out=rr[:, u % 3, 0:kw], in_=bank[:, 0:kw], func=AF.Relu), r=[bankb], w=[rrb[u % 3]])

                    def idx_D(u):
                        kc, h = iunits[u]
                        k0 = kc * 512; kw = min(512, nk - k0)
                        Pp(lambda e: e.matmul(out=pS[:, 0, 0:kw], lhsT=dg[:, h, :], rhs=rr[:, u % 3, 0:kw], start=(h == 0), stop=(h == 7)), r=[dg_b, rrb[u % 3]], w=[pSb[0]])
                        if h == 7:
                            Vv(lambda e: e.tensor_copy(out=sc[:, k0:k0 + kw], in_=pS[:, 0, 0:kw]), r=[pSb[0]], w=[sc_b])

                    for n_ in range(len(iunits) + 2):
                        if n_ < len(iunits):
                            idx_S(n_)
                        if n_ >= 2:
                            idx_D(n_ - 2)
                    steps = []

                    def st_init():
                        Vv(lambda e: e.tensor_reduce(out=bis[:, 0:1], in_=sc[:, 0:nk], axis=AX.X, op=ALU.min), r=[sc_b], w=[bis_b])
                        Vv(lambda e: e.tensor_tensor(out=sc[:, nk - 128:nk], in0=sc[:, nk - 128:nk], in1=cm30[:], op=ALU.add), r=[sc_b, cm30_b], w=[sc_b])
                        Vv(lambda e: e.tensor_reduce(out=bis[:, 1:2], in_=sc[:, 0:nk], axis=AX.X, op=ALU.max), r=[sc_b], w=[bis_b])
                        Vv(lambda e: e.tensor_tensor(out=bis[:, 2:3], in0=bis[:, 1:2], in1=bis[:, 0:1], op=ALU.subtract), r=[bis_b], w=[bis_b])
                    steps.append(st_init)

                    def mk_it(it):
                        f = 0.5 ** (it + 1)

                        def st_it():
                            Vv(lambda e: e.scalar_tensor_tensor(out=bis[:, 3:4], in0=bis[:, 2:3], scalar=f, in1=bis[:, 0:1], op0=ALU.mult, op1=ALU.add), r=[bis_b], w=[bis_b])
                            Vv(lambda e: e.tensor_scalar(out=mb[:, 0:nk], in0=sc[:, 0:nk], scalar1=bis[:, 3:4], scalar2=None, op0=ALU.is_ge, op1=ALU.add, accum_out=bis[:, 4:5]),
                               r=[sc_b, bis_b], w=[mb_b, bis_b])
                            Vv(lambda e: e.tensor_scalar(out=bis[:, 5:6], in0=bis[:, 4:5], scalar1=float(TOPK), scalar2=f, op0=ALU.is_ge, op1=ALU.mult), r=[bis_b], w=[bis_b])
                            Vv(lambda e: e.scalar_tensor_tensor(out=bis[:, 0:1], in0=bis[:, 5:6], scalar=bis[:, 2:3], in1=bis[:, 0:1], op0=ALU.mult, op1=ALU.add), r=[bis_b], w=[bis_b])
                        return st_it
                    for it in range(NBIS):
                        steps.append(mk_it(it))

                    def st_fin():
                        Vv(lambda e: e.tensor_scalar(out=mb[:, 0:nk], in0=sc[:, 0:nk], scalar1=bis[:, 0:1], scalar2=NEG, op0=ALU.is_lt, op1=ALU.mult), r=[sc_b, bis_b], w=[mb_b])
                    steps.append(st_fin)
                    return steps
                return []

            def attend_tile(i, extra_steps=()):
                extra_steps = list(extra_steps)
                nk = 128 * (i + 1)
                use_topk = i >= 2
                qT, qT_b = qTs[i % 2]
                mb, mb_b = mbs2[i % 2]
                sbanks = [(pS[:, 0, :], pSb[0]), (pS[:, 1, :], pSb[1]), (pO[:, 2, :], pOb[2])]
                units = []
                for g in range(8):
                    kgs = list(range(0, i + 1, 4))
                    for kg in kgs:
                        units.append(("a", g, kg, kg == kgs[-1]))
                for g in range(8):
                    kgs = list(range(0, i + 1, 4))
                    for kg in kgs:
                        units.append(("b", g, kg, kg == kgs[-1]))

                def uparams(kind, g):
                    if kind == "a":
                        return dict(hp=(g % 2) * 64, blk=g // 2, kT=akT, qsel=0, ob=pOb[g % 2], oap=pO[:, g % 2, 0:65])
                    h_, c_ = g // 2, g % 2
                    return dict(hp=c_ * 64, blk=h_, kT=bkT, qsel=1, ob=pOb[c_], oap=pO[:, c_, 0:129])

                def att_QK(u):
                    kind, g, kg, _last = units[u]
                    p_ = uparams(kind, g)
                    hp, blk, kT, qsel = p_["hp"], p_["blk"], p_["kT"], p_["qsel"]
                    bank, bankb = sbanks[u % 3]
                    kts = list(range(kg, min(kg + 4, i + 1)))
                    for jj, kt in enumerate(kts):
                        need_mask = (kind == "a" and use_topk) or kt == i
                        Pp(lambda e, jj=jj, kt=kt, need_mask=need_mask: e.matmul(out=bank[:, jj * 128:(jj + 1) * 128], lhsT=kT[hp:hp + 64, blk, kt * 128:(kt + 1) * 128],
                                                                                 rhs=qT[hp:hp + 64, qsel, blk, :], start=True, stop=not need_mask),
                           r=[kvb[kt], qT_b], w=[bankb])
                        if need_mask:
                            if kind == "a" and use_topk:
                                Pp(lambda e, jj=jj, kt=kt: e.matmul(out=bank[:, jj * 128:(jj + 1) * 128], lhsT=mb[:, kt * 128:(kt + 1) * 128], rhs=ident[:], start=False, stop=True),
                                   r=[mb_b, ident_b], w=[bankb])
                            else:
                                Pp(lambda e, jj=jj: e.matmul(out=bank[:, jj * 128:(jj + 1) * 128], lhsT=cm[:], rhs=ident[:], start=False, stop=True),
                                   r=[cm_b, ident_b], w=[bankb])
                    wd = 128 * len(kts)
                    Aa(lambda e: e.activation(out=PT[:, u % 3, 0:wd], in_=bank[:, 0:wd], func=AF.Exp, scale=0.125), r=[bankb], w=[PTb[u % 3]])

                def att_PV(u):
                    kind, g, kg, last = units[u]
                    p_ = uparams(kind, g)
                    ob, oap = p_["ob"], p_["oap"]
                    kts = list(range(kg, min(kg + 4, i + 1)))
                    for jj, kt in enumerate(kts):
                        rhs = av_sb[:, kt, g, :] if kind == "a" else bv_sb[:, kt, g // 2, :]
                        Pp(lambda e, jj=jj, kt=kt, rhs=rhs: e.matmul(out=oap, lhsT=PT[:, u % 3, jj * 128:(jj + 1) * 128], rhs=rhs, start=(kt == 0), stop=(kt == i)),
                           r=[PTb[u % 3], kvb[kt]], w=[ob])
                    if not last:
                        return
                    if kind == "a":
                        Vv(lambda e: e.reciprocal(out=osm[:, g:g + 1], in_=oap[:, 64:65]), r=[ob], w=[osm_b])
                        Vv(lambda e: e.tensor_scalar(out=mixed[:, g * 64:(g + 1) * 64], in0=oap[:, 0:64], scalar1=osm[:, g:g + 1], scalar2=None, op0=ALU.mult), r=[ob, osm_b], w=[mixed_b])
                    else:
                        h_, c_ = g // 2, g % 2
                        Vv(lambda e: e.reciprocal(out=osm[:, c_:c_ + 1], in_=oap[:, 128:129]), r=[ob], w=[osm_b])
                        Vv(lambda e: e.tensor_scalar(out=bo[:, c_, :], in0=oap[:, 0:128], scalar1=osm[:, c_:c_ + 1], scalar2=None, op0=ALU.mult), r=[ob, osm_b], w=[bo_b])
                        if c_ == 1:
                            diff_finish(bo, bo_b, mixed, mixed_b, h_, 128, osm, osm_b)

                LOOK = 2
                n_total = len(units) + LOOK
                done_steps = 0
                for n_ in range(n_total):
                    if n_ < len(units):
                        att_QK(n_)
                    if n_ >= LOOK:
                        att_PV(n_ - LOOK)
                    want = (len(extra_steps) * (n_ + 1)) // n_total
                    while done_steps < want:
                        extra_steps[done_steps]()
                        done_steps += 1
                while done_steps < len(extra_steps):
                    extra_steps[done_steps]()
                    done_steps += 1

            def diff_finish(bo_, bo_b_, mixed_, mixed_b_, h_, n, osm_, osm_b_):
                Vv(lambda e: e.scalar_tensor_tensor(out=bo_[0:n, 0, :], in0=bo_[0:n, 1, :], scalar=neglam[0:n, :], in1=bo_[0:n, 0, :], op0=ALU.mult, op1=ALU.add), r=[bo_b_, lamv_b], w=[bo_b_])
                Aa(lambda e: e.activation(out=bo_[0:n, 1, :], in_=bo_[0:n, 0, :], func=AF.Square, accum_out=osm_[0:n, 2:3]), r=[bo_b_], w=[bo_b_, osm_b_])
                rstd_from_ssq(osm_[0:n, 2:3], 1.0 / 128, [osm_b_])
                Vv(lambda e: e.tensor_scalar(out=osm_[0:n, 2:3], in0=osm_[0:n, 2:3], scalar1=0.8, scalar2=None, op0=ALU.mult), r=[osm_b_], w=[osm_b_])
                Vv(lambda e: e.scalar_tensor_tensor(out=mixed_[0:n, 512 + h_ * 128:512 + (h_ + 1) * 128], in0=bo_[0:n, 0, :], scalar=osm_[0:n, 2:3], in1=subln_t[0:n, :], op0=ALU.mult, op1=ALU.mult),
                   r=[bo_b_, osm_b_, subln_b], w=[mixed_b_])

            if "A" in phases:
                project_tile(xs[:, :], rope_s[:, :], NS, o_s, 0, None)
                transposes_tile(NS, None)
                S.dma("sp", lambda e: e.dma_start(out=sgnbuf[:, :], in_=sq_s["wsg_s"][0][:, :]), r=[sq_s["wsg_s"][1]], w=[hbuf_b])
                npt = NT
                def stage_A(i):
                    project_tile(xp[i * 128:(i + 1) * 128, :], rope_p[i * 128:(i + 1) * 128, :], 128, o_p, i * 128, i)
                    transposes_tile(128, i)
                    return index_tile(i)
                for st_ in stage_A(0):
                    st_()
                for i in range(npt):
                    steps_next = stage_A(i + 1) if i + 1 < npt else []
                    attend_tile(i, steps_next)
                    S.dma("sp", lambda e, i=i: e.dma_start(out=mbuf[i * 128:(i + 1) * 128, :], in_=mixed[:, :]), r=[mixed_b], w=[mbuf_b])

        S.barrier()
        if "S" in phases:
            with contextlib.ExitStack() as ps_:
                aqT_s, aqT_s_b = sq_s["aqT_s"]; bqT_s, bqT_s_b = sq_s["bqT_s"]; iqT_s8, iqT_s8_b = sq_s["iqT_s8"]
                akT_s, akT_s_b = sq_s["akT_s"]; bkT_s, bkT_s_b = sq_s["bkT_s"]; ikT_s, ikT_s_b = sq_s["ikT_s"]
                av_s, av_s_b = sq_s["av_s"]; bv_s, bv_s_b = sq_s["bv_s"]; wsg_s, wsg_s_b = sq_s["wsg_s"]
                pti, pti_b = sbt(ps_, "pti", [128, NB * NPG], I32)
                idx, idx_b = sbt(ps_, "idx", [128, NB * NPG], I32)
                iot, iot_b = sbt(ps_, "iot", [128, 1], I32)
                signB, signB_b = sbt(ps_, "signB", [128, NB, 8, 4], F32)
                sgn_tmp, sgn_tmp_b = sbt(ps_, "sgn_tmp", [128, 512], F32)
                bselt, bselt_b = sbt(ps_, "bselt", [1, NB * 512], BF16)
                ones1, ones1_b = sbt(ps_, "ones1", [128, 128], BF16)
                zrow, zrow_b = sbt(ps_, "zrow", [1, 512], BF16)
                i8, i8_b = sbt(ps_, "i8", [64, 512], BF16)
                nbq, nbq_b = sbt(ps_, "nbq", [64, 64], F32)
                nbqh, nbqh_b = sbt(ps_, "nbqh", [64, 2, 64], BF16)
                nb30, nb30_b = sbt(ps_, "nb30", [64, 4], F32)
                bdiag, bdiag_b = sbt(ps_, "bdiag", [64, 64], F32)
                Qbd_a, Qbd_a_b = sbt(ps_, "Qbd_a", [128, 4, 2, NS], BF16)
                Qbd_b, Qbd_b_b = sbt(ps_, "Qbd_b", [128, 4, 2, NS], BF16)
                ipg = [sbt(ps_, "ipg%d" % k, [128, NPG, 64], BF16) for k in range(2)]
                pti2, pti2_b = sbt(ps_, "pti2", [128, NB], I32)
                idx2, idx2_b = sbt(ps_, "idx2", [128, NB], I32)
                iot7, iot7_b = sbt(ps_, "iot7", [128, 1], I32)
                ikTp, ikTp_b = sbt(ps_, "ikTp", [64, NPG, 128], BF16)
                Rb, Rb_b = sbt(ps_, "Rb", [128, NPG, 8, 4], F32)
                STall, STall_b = sbt(ps_, "STall", [128, NPG, NS], F32)
                scs, scs_b = sbt(ps_, "scs", [NS, 2052], F32)
                mbs, mbs_b = sbt(ps_, "mbs", [NS, 2052], BF16)
                bis2, bis2_b = sbt(ps_, "bis2", [NS, 8], F32)
                snew, snew_b = sbt(ps_, "snew", [NS, 8, 64], F32)
                sacc, sacc_b = sbt(ps_, "sacc", [NS, 64], F32)
                pg = {k: [sbt(ps_, "pg%s%d" % (k, j), [128, 512], BF16) for j in range(4)] for k in ("ak", "av", "bk", "bv")}
                kTp = {k: [sbt(ps_, "kTp%s%d" % (k, j), [128, 4, 128], BF16) for j in range(2)] for k in ("ak", "bk")}
                PTs = [sbt(ps_, "PTs%d" % j, [128, 512], BF16) for j in range(2)]
                PTd = [sbt(ps_, "PTd%d" % j, [128, 512], BF16) for j in range(2)]
                zr, zr_b = sbt(ps_, "zr", [1, 2, 512], F32)
                zc, zc_b = sbt(ps_, "zc", [NS, 16], F32)
                oa_s, oa_s_b = sbt(ps_, "oa_s", [NS, 8, 64], F32)
                bo_s, bo_s_b = sbt(ps_, "bo_s", [NS, 2, 128], F32)
                osm_s, osm_s_b = sbt(ps_, "osm_s", [NS, 8], F32)

                S.dma("sp", lambda e: e.dma_start(out=pti[:], in_=ptab.partition_broadcast(128)), w=[pti_b])
                Gg(lambda e: e.iota(out=iot[:], pattern=[[0, 1]], base=0, channel_multiplier=1), w=[iot_b])
                Gg(lambda e: e.tensor_scalar(out=idx[:], in0=pti[:], scalar1=128, scalar2=None, op0=ALU.mult), r=[pti_b], w=[idx_b])
                Gg(lambda e: e.tensor_tensor(out=idx[:], in0=idx[:], in1=iot[:].to_broadcast([128, NB * NPG]), op=ALU.add), r=[idx_b, iot_b], w=[idx_b])
                for j in range(NPG):
                    S.dma("sp", lambda e, j=j: e.dma_start(out=pti2[j * 8:(j + 1) * 8, :], in_=ptabT[:, j * NB:(j + 1) * NB].partition_broadcast(8)), w=[pti2_b])
                Vv(lambda e: e.tensor_single_scalar(out=iot7[:], in_=iot[:], scalar=7, op=ALU.bitwise_and), r=[iot_b], w=[iot7_b])
                Gg(lambda e: e.tensor_scalar(out=idx2[:], in0=pti2[:], scalar1=8, scalar2=None, op0=ALU.mult), r=[pti2_b], w=[idx2_b])
                Gg(lambda e: e.tensor_tensor(out=idx2[:], in0=idx2[:], in1=iot7[:].to_broadcast([128, NB]), op=ALU.add), r=[idx2_b, iot7_b], w=[idx2_b])
                cik2 = cik.rearrange("(r t) d -> r (t d)", t=16)
                S.dma("pool", lambda e: e.dma_start(out=bselt[:], in_=c_bsel[:, :]), w=[bselt_b])
                S.dma("pool", lambda e: e.dma_start(out=i8[:], in_=c_i8[:, :]), w=[i8_b])
                S.dma("sp", lambda e: e.dma_start(out=nbq[:], in_=c_nbq[:, :]), w=[nbq_b])
                S.dma("sp", lambda e: e.dma_start(out=nb30[:], in_=c_nb30[:, :]), w=[nb30_b])
                S.dma("sp", lambda e: e.dma_start(out=bdiag[:], in_=c_bdiag[:, :]), w=[bdiag_b])
                Vv(lambda e: e.memset(ones1[:], 1.0), w=[ones1_b])
                Vv(lambda e: e.memset(zrow[:], 0.0), w=[zrow_b])
                S.dma("sp", lambda e: e.dma_start(out=sgn_tmp[:, :], in_=sgnbuf.rearrange("(o r) h -> o (r h)", o=1).partition_broadcast(128)), r=[hbuf_b], w=[sgn_tmp_b])
                Vv(lambda e: e.tensor_copy(out=signB[:], in_=sgn_tmp[:, :].rearrange("p (b q h) -> p b h q", b=NB, q=4)), r=[sgn_tmp_b], w=[signB_b])
                Vv(lambda e: e.memset(Qbd_a[:], 0.0), w=[Qbd_a_b])
                Vv(lambda e: e.memset(Qbd_b[:], 0.0), w=[Qbd_b_b])
                for (Qd, Qd_b, src, src_b) in ((Qbd_a, Qbd_a_b, aqT_s, aqT_s_b), (Qbd_b, Qbd_b_b, bqT_s, bqT_s_b)):
                    Vv(lambda e, Qd=Qd, src=src: e.tensor_copy(out=Qd[0:64, :, 0, :], in_=src[0:64, :, :]), r=[src_b], w=[Qd_b])
                    Vv(lambda e, Qd=Qd, src=src: e.tensor_copy(out=Qd[64:128, :, 1, :], in_=src[64:128, :, :]), r=[src_b], w=[Qd_b])

                for b in range(NB):
                    ip, ip_b = ipg[b % 2]
                    S.dma("pool", lambda e, b=b, ip=ip: e.indirect_dma_start(out=ip[:, :, :].rearrange("p t d -> p (t d)"), out_offset=None, in_=cik2,
                                                                              in_offset=bass.IndirectOffsetOnAxis(ap=idx2[:, b:b + 1], axis=0)), r=[idx2_b], w=[ip_b])
                    for half in range(2):
                        for j in range(8):
                            jj = half * 8 + j
                            Pp(lambda e, j=j, jj=jj, ip=ip: e.transpose(out=pT[0:64, j * 128:(j + 1) * 128], in_=ip[:, jj, :], identity=ident[:]), r=[ip_b, ident_b], w=[pT_b])
                        Vv(lambda e, half=half: e.tensor_copy(out=ikTp[:, half * 8:(half + 1) * 8, :], in_=pT[0:64, :].rearrange("p (j t) -> p j t", j=8)), r=[pT_b], w=[ikTp_b])
                    pb = b % 2
                    for j in range(NPG):
                        Pp(lambda e, j=j, b=b, pb=pb: e.matmul(out=pP[:, pb, j * 32:(j + 1) * 32].rearrange("p (h q) -> p h q", h=8), lhsT=ikTp[:, j, :], rhs=iqT_s8[:, :, b * 4:(b + 1) * 4], start=True, stop=True),
                           r=[ikTp_b, iqT_s8_b], w=[pPb[pb]])
                    Aa(lambda e, pb=pb: e.activation(out=Rb[:].rearrange("p j h q -> p (j h q)"), in_=pP[:, pb, :], func=AF.Relu), r=[pPb[pb]], w=[Rb_b])
                    Vv(lambda e, b=b: e.tensor_tensor(out=Rb[:], in0=Rb[:], in1=signB[:, b, :, :].unsqueeze(1).to_broadcast([128, NPG, 8, 4]), op=ALU.mult), r=[Rb_b, signB_b], w=[Rb_b])
                    Vv(lambda e, b=b: e.tensor_reduce(out=STall[:, :, b * 4:(b + 1) * 4], in_=Rb[:].rearrange("p j h q -> p j q h"), axis=AX.X, op=ALU.add), r=[Rb_b], w=[STall_b])
                for j in range(NPG):
                    pb = j % 2
                    Pp(lambda e, j=j, pb=pb: e.transpose(out=pP[0:NS, pb, 0:128], in_=STall[:, j, :], identity=identf[:]), r=[STall_b, identf_b], w=[pPb[pb]])
                    Vv(lambda e, j=j, pb=pb: e.tensor_copy(out=scs[:, 0:2048].rearrange("r (g t) -> r g t", t=16)[:, :, j], in_=pP[0:NS, pb, 0:128]), r=[pPb[pb]], w=[scs_b])
                for h in range(8):
                    Pp(lambda e, h=h: e.matmul(out=pS[0:NS, 0, h * 64:(h + 1) * 64], lhsT=iqT_s8[:, h, :], rhs=ikT_s[0:64, :], start=True, stop=True), r=[iqT_s8_b, ikT_s_b], w=[pSb[0]])
                Aa(lambda e: e.activation(out=snew[:].rearrange("p h r -> p (h r)"), in_=pS[0:NS, 0, :], func=AF.Relu), r=[pSb[0]], w=[snew_b])
                Vv(lambda e: e.tensor_scalar(out=sacc[:], in0=snew[:, 0, :], scalar1=wsg_s[:, 0:1], scalar2=None, op0=ALU.mult), r=[snew_b, wsg_s_b], w=[sacc_b])
                for h in range(1, 8):
                    Vv(lambda e, h=h: e.scalar_tensor_tensor(out=sacc[:], in0=snew[:, h, :], scalar=wsg_s[:, h:h + 1], in1=sacc[:], op0=ALU.mult, op1=ALU.add), r=[snew_b, wsg_s_b, sacc_b], w=[sacc_b])
                Vv(lambda e: e.tensor_tensor(out=sacc[:], in0=sacc[:], in1=bdiag[:], op=ALU.mult), r=[sacc_b, bdiag_b], w=[sacc_b])
                Vv(lambda e: e.tensor_reduce(out=scs[:, 2048:2052], in_=sacc[:].rearrange("p (b j) -> p j b", j=4), axis=AX.X, op=ALU.add), r=[sacc_b], w=[scs_b])
                Vv(lambda e: e.tensor_reduce(out=bis2[:, 0:1], in_=scs[:, :], axis=AX.X, op=ALU.min), r=[scs_b], w=[bis2_b])
                Vv(lambda e: e.tensor_tensor(out=scs[:, 2048:2052], in0=scs[:, 2048:2052], in1=nb30[:], op=ALU.add), r=[scs_b, nb30_b], w=[scs_b])
                Vv(lambda e: e.tensor_reduce(out=bis2[:, 1:2], in_=scs[:, :], axis=AX.X, op=ALU.max), r=[scs_b], w=[bis2_b])
                Vv(lambda e: e.tensor_tensor(out=bis2[:, 2:3], in0=bis2[:, 1:2], in1=bis2[:, 0:1], op=ALU.subtract), r=[bis2_b], w=[bis2_b])
                for it in range(NBIS):
                    f = 0.5 ** (it + 1)
                    Vv(lambda e, f=f: e.scalar_tensor_tensor(out=bis2[:, 3:4], in0=bis2[:, 2:3], scalar=f, in1=bis2[:, 0:1], op0=ALU.mult, op1=ALU.add), r=[bis2_b], w=[bis2_b])
                    Vv(lambda e: e.tensor_scalar(out=mbs[:, :], in0=scs[:, :], scalar1=bis2[:, 3:4], scalar2=None, op0=ALU.is_ge, op1=ALU.add, accum_out=bis2[:, 4:5]), r=[scs_b, bis2_b], w=[mbs_b, bis2_b])
                    Vv(lambda e, f=f: e.tensor_scalar(out=bis2[:, 5:6], in0=bis2[:, 4:5], scalar1=float(TOPK), scalar2=f, op0=ALU.is_ge, op1=ALU.mult), r=[bis2_b], w=[bis2_b])
                    Vv(lambda e: e.scalar_tensor_tensor(out=bis2[:, 0:1], in0=bis2[:, 5:6], scalar=bis2[:, 2:3], in1=bis2[:, 0:1], op0=ALU.mult, op1=ALU.add), r=[bis2_b], w=[bis2_b])
                Vv(lambda e: e.tensor_scalar(out=mbs[:, :], in0=scs[:, :], scalar1=bis2[:, 0:1], scalar2=NEG, op0=ALU.is_lt, op1=ALU.mult), r=[scs_b, bis2_b], w=[mbs_b])
                Vv(lambda e: e.tensor_copy(out=nbqh[:, 0, :], in_=nbq[:]), r=[nbq_b], w=[nbqh_b])
                Vv(lambda e: e.tensor_tensor(out=nbqh[:, 1, :].rearrange("p (b j) -> p b j", j=4), in0=nbq[:].rearrange("p (b j) -> p b j", j=4),
                                              in1=mbs[:, 2048:2052].unsqueeze(1).to_broadcast([NS, NB, 4]), op=ALU.add), r=[nbq_b, mbs_b], w=[nbqh_b])

                oA = pO[0:NS, 0, :]
                oB = pO[0:NS, 1:3, :]
                zA = pP[0:1, 0, :]
                zB = pP[32:33, 0, :]
                pT2 = pP[:, 1, :].bitcast(BF16)
                steps3 = [(b, j) for b in range(NB) for j in range(NPG)] + [(NB, 0)]
                nsteps = len(steps3)

                def s3_L(i):
                    b, j = steps3[i]
                    if b == NB:
                        return
                    col = b * NPG + j
                    for k, src in (("ak", cak), ("av", cav), ("bk", cbk), ("bv", cbv)):
                        tt, tb = pg[k][i % 4]
                        S.dma("pool", lambda e, tt=tt, src=src: e.indirect_dma_start(out=tt[:, :], out_offset=None, in_=src[:, :],
                                                                                    in_offset=bass.IndirectOffsetOnAxis(ap=idx[:, col:col + 1], axis=0)), r=[idx_b], w=[tb])

                def s3_T(i):
                    b, j = steps3[i]
                    if b == NB:
                        return
                    for k, (tbank, tbank_b) in (("ak", (pT, pT_b)), ("bk", (pT2, pPb[1]))):
                        tt, tb = pg[k][i % 4]
                        dd, ddb = kTp[k][i % 2]
                        for q4 in range(4):
                            Pp(lambda e, q4=q4: e.transpose(out=tbank[:, q4 * 128:(q4 + 1) * 128], in_=tt[:, q4 * 128:(q4 + 1) * 128], identity=ident[:]), r=[tb, ident_b], w=[tbank_b])
                        if k == "ak":
                            Vv(lambda e: e.tensor_copy(out=dd[:, :, :], in_=tbank[:, 0:512].rearrange("p (j t) -> p j t", j=4)), r=[tbank_b], w=[ddb])
                        else:
                            Aa(lambda e: e.copy(out=dd[:, :, :], in_=tbank[:, 0:512].rearrange("p (j t) -> p j t", j=4)), r=[tbank_b], w=[ddb])

                def s3_ops(i):
                    b, j = steps3[i]
                    if b < NB:
                        akt, akt_b = kTp["ak"][i % 2]; bkt, bkt_b = kTp["bk"][i % 2]
                        avt, avt_b = pg["av"][i % 4]; bvt, bvt_b = pg["bv"][i % 4]
                        return 128, (lambda q4: akt[:, q4, :]), akt_b, (lambda q4: bkt[:, q4, :]), bkt_b, avt, avt_b, bvt, bvt_b
                    return NS, (lambda q4: akT_s[:, q4, :]), akT_s_b, (lambda q4: bkT_s[:, q4, :]), bkT_s_b, av_s, av_s_b, bv_s, bv_s_b

                def s3_Sc(i):
                    b, j = steps3[i]
                    nkp, akl, akt_b, bkl, bkt_b, avt, avt_b, bvt, bvt_b = s3_ops(i)
                    sl = i % 2
                    if b < NB:
                        Pp(lambda e: e.matmul(out=pS[0:nkp, 0, :], lhsT=ones1[0:1, 0:nkp], rhs=bselt[0:1, b * 512:(b + 1) * 512], start=True, stop=False), r=[ones1_b, bselt_b], w=[pSb[0]])
                        Pp(lambda e: e.matmul(out=pS[0:nkp, 0, :], lhsT=mbs[:, j * 128:(j + 1) * 128], rhs=i8[:, :], start=False, stop=False), r=[mbs_b, i8_b], w=[pSb[0]])
                    else:
                        Pp(lambda e: e.matmul(out=pS[0:nkp, 0, :], lhsT=nbqh[:, 1, :], rhs=i8[:, :], start=True, stop=False), r=[nbqh_b, i8_b], w=[pSb[0]])
                    for q4 in range(4):
                        Pp(lambda e, q4=q4: e.matmul(out=pS[0:nkp, 0, q4 * 128:(q4 + 1) * 128], lhsT=akl(q4), rhs=Qbd_a[:, q4, :, :].rearrange("p a r -> p (a r)"), start=False, stop=(q4 == 3)),
                           r=[akt_b, Qbd_a_b], w=[pSb[0]])
                    pts, pts_b = PTs[sl]
                    Aa(lambda e: e.activation(out=pts[0:nkp, :], in_=pS[0:nkp, 0, :], func=AF.Exp, scale=0.125), r=[pSb[0]], w=[pts_b])
                    if b < NB:
                        Pp(lambda e: e.matmul(out=pS[0:nkp, 1, :], lhsT=ones1[0:1, 0:nkp], rhs=bselt[0:1, b * 512:(b + 1) * 512], start=True, stop=False), r=[ones1_b, bselt_b], w=[pSb[1]])
                    else:
                        Pp(lambda e: e.matmul(out=pS[0:nkp, 1, :], lhsT=nbqh[:, 0, :], rhs=i8[:, :], start=True, stop=False), r=[nbqh_b, i8_b], w=[pSb[1]])
                    for q4 in range(4):
                        Pp(lambda e, q4=q4: e.matmul(out=pS[0:nkp, 1, q4 * 128:(q4 + 1) * 128], lhsT=bkl(q4), rhs=Qbd_b[:, q4, :, :].rearrange("p a r -> p (a r)"), start=False, stop=(q4 == 3)),
                           r=[bkt_b, Qbd_b_b], w=[pSb[1]])
                    ptd, ptd_b = PTd[sl]
                    Aa(lambda e: e.activation(out=ptd[0:nkp, :], in_=pS[0:nkp, 1, :], func=AF.Exp, scale=0.125), r=[pSb[1]], w=[ptd_b])

                def s3_V(i):
                    nkp, akl, akt_b, bkl, bkt_b, avt, avt_b, bvt, bvt_b = s3_ops(i)
                    sl = i % 2
                    first = (i == 0); last = (i == nsteps - 1)
                    pts, pts_b = PTs[sl]; ptd, ptd_b = PTd[sl]
                    if first:
                        for bk_ in range(3):
                            Pp(lambda e, bk_=bk_: e.matmul(out=pO[0:NS, bk_, :], lhsT=zrow[0:1, 0:NS], rhs=zrow[0:1, 0:512], start=True, stop=False), r=[zrow_b], w=[pOb[bk_]])
                    for h in range(8):
                        Pp(lambda e, h=h: e.matmul(out=oA[:, h * 64:(h + 1) * 64], lhsT=pts[0:nkp, h * 64:(h + 1) * 64], rhs=avt[0:nkp, h * 64:(h + 1) * 64], start=False, stop=last),
                           r=[pts_b, avt_b], w=[pOb[0]])
                    Pp(lambda e: e.matmul(out=zA, lhsT=ones1[0:nkp, 0:1], rhs=pts[0:nkp, :], start=first, stop=last), r=[pts_b, ones1_b], w=[pPb[0]])
                    for g in range(8):
                        h_ = g // 2
                        Pp(lambda e, g=g, h_=h_: e.matmul(out=oB[:, g // 4, (g % 4) * 128:(g % 4 + 1) * 128], lhsT=ptd[0:nkp, g * 64:(g + 1) * 64], rhs=bvt[0:nkp, h_ * 128:(h_ + 1) * 128], start=False, stop=last),
                           r=[ptd_b, bvt_b], w=[pOb[1 + g // 4]])
                    Pp(lambda e: e.matmul(out=zB, lhsT=ones1[0:nkp, 0:1], rhs=ptd[0:nkp, :], start=first, stop=last), r=[ptd_b, ones1_b], w=[pPb[0]])

                for n_ in range(nsteps + 3):
                    if n_ < nsteps:
                        s3_L(n_)
                    if 1 <= n_ <= nsteps:
                        s3_T(n_ - 1)
                    if 2 <= n_ <= nsteps + 1:
                        s3_Sc(n_ - 2)
                    if n_ >= 3:
                        s3_V(n_ - 3)
                Vv(lambda e: e.tensor_copy(out=zr[:, 0, :], in_=zA), r=[pPb[0]], w=[zr_b])
                Vv(lambda e: e.tensor_copy(out=zr[:, 1, :], in_=zB), r=[pPb[0]], w=[zr_b])
                for a in range(2):
                    for g in range(8):
                        Pp(lambda e, a=a, g=g: e.matmul(out=pS[0:NS, 0, a * 8 + g:a * 8 + g + 1], lhsT=zr[0:1, a, g * 64:(g + 1) * 64], rhs=identf[0:1, 0:1], start=True, stop=True), r=[zr_b, identf_b], w=[pSb[0]])
                Vv(lambda e: e.reciprocal(out=zc[:, :], in_=pS[0:NS, 0, 0:16]), r=[pSb[0]], w=[zc_b])
                Vv(lambda e: e.tensor_tensor(out=oa_s[:], in0=oA.rearrange("p (h d) -> p h d", h=8), in1=zc[:, 0:8].unsqueeze(2).to_broadcast([NS, 8, 64]), op=ALU.mult), r=[pOb[0], zc_b], w=[oa_s_b])
                Vv(lambda e: e.tensor_copy(out=mixed_s[:, 0:512], in_=oa_s[:].rearrange("p h d -> p (h d)")), r=[oa_s_b], w=[mixed_s_b])
                for h_ in range(4):
                    for c_ in range(2):
                        g = h_ * 2 + c_
                        Vv(lambda e, g=g, c_=c_: e.tensor_scalar(out=bo_s[:, c_, :], in0=oB[:, g // 4, (g % 4) * 128:(g % 4 + 1) * 128], scalar1=zc[:, 8 + g:9 + g], scalar2=None, op0=ALU.mult), r=[pOb[1 + g // 4], zc_b], w=[bo_s_b])
                    diff_finish(bo_s, bo_s_b, mixed_s, mixed_s_b, h_, NS, osm_s, osm_s_b)
                S.dma("sp", lambda e: e.dma_start(out=mbuf[T:T + NS, :], in_=mixed_s[:, :]), r=[mixed_s_b], w=[mbuf_b])
                if debug:
                    S.dma("pool", lambda e: e.dma_start(out=dbg_mixed[:, :], in_=mixed_s[:, :]), r=[mixed_s_b], is_output=True)
                    S.dma("sp", lambda e: e.dma_start(out=dbg_scs[:, :], in_=scs[:, :]), r=[scs_b], is_output=True)
                    S.dma("pool", lambda e: e.dma_start(out=dbg_mbs[:, :], in_=mbs[:, :]), r=[mbs_b], is_output=True)
                    S.dma("sp", lambda e: e.dma_start(out=dbg_z[:, :], in_=zc[:, :]), r=[zc_b], is_output=True)

        S.barrier()
        if "B" in phases:
            with contextlib.ExitStack() as pb_:
                NTT = NT + 1
                xn2T, xn2T_b = sbt(pb_, "xn2T", [128, 8, NTT * 128], BF16)
                gates, gates_b = sbt(pb_, "gates", [128, NTT, 32], F32)
                facc, _ = sbt(pb_, "facc", [128, NTT, D], F32)
                faccb = [Buf("facc%d" % t) for t in range(NTT)]
                wr_sb, wr_b = sbt(pb_, "wr_sb", [128, 8, 36], BF16)
                wout_sb, wout_b = sbt(pb_, "wout_sb", [128, 8, D], BF16)
                gffn_t, gffn_b = sbt(pb_, "gffn_t", [128, D], F32)
                mxl = [sbt(pb_, "mxl%d" % k, [128, D], BF16) for k in range(2)]
                xrl = [sbt(pb_, "xrl%d" % k, [128, D], F32) for k in range(2)]
                mixT, mixT_b = sbt(pb_, "mixT", [128, 8, 128], BF16)
                xn2, xn2_b = sbt(pb_, "xn2", [128, D], BF16)
                rt, rt_b = sbt(pb_, "rt", [128, 160], F32)
                wgu = [sbt(pb_, "wgu%d" % k, [128, 8, 512], BF16) for k in range(2)]
                wdn = [sbt(pb_, "wdn%d" % k, [128, 2, D], BF16) for k in range(2)]
                sil = [sbt(pb_, "sil%d" % k, [128, 256], F32) for k in range(2)]
                actt = [sbt(pb_, "actt%d" % k, [128, 256], BF16) for k in range(2)]
                actT = [sbt(pb_, "actT%d" % k, [128, 2, 128], BF16) for k in range(2)]
                S.dma("pool", lambda e: e.dma_start(out=wout_sb[:], in_=w_out.rearrange("(c p) n -> p c n", p=128)), w=[wout_b])
                S.dma("sp", lambda e: e.dma_start(out=gffn_t[:], in_=g_ffn.partition_broadcast(128)), w=[gffn_b])
                Vv(lambda e: e.memset(gates[:], 0.0), w=[gates_b])
                S.dma("pool", lambda e: e.dma_start(out=wr_sb[:], in_=w_r.rearrange("(c p) n -> p c n", p=128)), w=[wr_b])
                Vv(lambda e: e.memset(xn2T[:, :, NT * 128 + NS:NTT * 128], 0.0), w=[xn2T_b])

                def rows(t):
                    return (t * 128, 128) if t < NT else (T, NS)

                for t in range(NTT):
                    r0, n = rows(t)
                    mx_t, mx_b = mxl[t % 2]
                    xr_t, xr_b = xrl[t % 2]
                    x_src = xp[r0:r0 + n, :] if t < NT else xs[:, :]
                    S.dma("sp", lambda e, mx_t=mx_t, r0=r0, n=n: e.dma_start(out=mx_t[0:n, :], in_=mbuf[r0:r0 + n, :]), r=[mbuf_b], w=[mx_b])
                    S.dma("sp", lambda e, xr_t=xr_t, x_src=x_src, n=n: e.dma_start(out=xr_t[0:n, :], in_=x_src), w=[xr_b])
                    for c in range(8):
                        Pp(lambda e, c=c, n=n, mx_t=mx_t: e.transpose(out=pT[:, c * 128:c * 128 + n], in_=mx_t[0:n, c * 128:(c + 1) * 128], identity=ident[0:n, 0:n]), r=[mx_b, ident_b], w=[pT_b])
                    Aa(lambda e, n=n: e.copy(out=mixT[:, :, 0:n], in_=pT[:, :].rearrange("p (c t) -> p c t", c=8)[:, :, 0:n]), r=[pT_b], w=[mixT_b])
                    h_t = facc[:, t, :]
                    h_b = faccb[t]
                    for half in range(2):
                        for c in range(8):
                            Pp(lambda e, c=c, half=half, n=n: e.matmul(out=pO[0:n, half, :], lhsT=mixT[:, c, 0:n], rhs=wout_sb[:, c, half * 512:(half + 1) * 512], start=(c == 0), stop=(c == 7)),
                               r=[mixT_b, wout_b], w=[pOb[half]])
                        Vv(lambda e, half=half, n=n, h_t=h_t, xr_t=xr_t: e.tensor_tensor(out=h_t[0:n, half * 512:(half + 1) * 512], in0=pO[0:n, half, :], in1=xr_t[0:n, half * 512:(half + 1) * 512], op=ALU.add),
                           r=[pOb[half], xr_b], w=[h_b])
                    Aa(lambda e, h_t=h_t, n=n: e.activation(out=xn2[0:n, :], in_=h_t[0:n, :], func=AF.Square, accum_out=rt[0:n, 0:1]), r=[h_b], w=[xn2_b, rt_b])
                    rstd_from_ssq(rt[0:n, 0:1], 1.0 / D, [rt_b])
                    Vv(lambda e, h_t=h_t, n=n: e.scalar_tensor_tensor(out=xn2[0:n, :], in0=h_t[0:n, :], scalar=rt[0:n, 0:1], in1=gffn_t[0:n, :], op0=ALU.mult, op1=ALU.mult), r=[h_b, rt_b, gffn_b], w=[xn2_b])
                    for c in range(8):
                        Pp(lambda e, c=c, n=n: e.transpose(out=pT[:, c * 128:c * 128 + n], in_=xn2[0:n, c * 128:(c + 1) * 128], identity=ident[0:n, 0:n]), r=[xn2_b, ident_b], w=[pT_b])
                    Aa(lambda e, t=t, n=n: e.copy(out=xn2T[:, :, t * 128:t * 128 + n], in_=pT[:, :].rearrange("p (c t) -> p c t", c=8)[:, :, 0:n]), r=[pT_b], w=[xn2T_b])
                    for c in range(8):
                        Pp(lambda e, c=c, t=t, n=n: e.matmul(out=pS[0:n, 0, 0:36], lhsT=xn2T[:, c, t * 128:t * 128 + n], rhs=wr_sb[:, c, :], start=(c == 0), stop=(c == 7)), r=[xn2T_b, wr_b], w=[pSb[0]])
                    L = rt[0:n, 8:44]
                    Vv(lambda e, n=n, L=L: e.tensor_tensor(out=L, in0=pS[0:n, 0, 0:36], in1=br_t[0:n, :], op=ALU.add), r=[pSb[0], br_b], w=[rt_b])
                    gl = rt[0:n, 8:12]; el = rt[0:n, 12:44]
                    Vv(lambda e, n=n, gl=gl: e.tensor_reduce(out=rt[0:n, 1:2], in_=gl, axis=AX.X, op=ALU.max), r=[rt_b], w=[rt_b])
                    Vv(lambda e, n=n, gl=gl: e.tensor_scalar(out=rt[0:n, 44:48], in0=gl, scalar1=rt[0:n, 1:2], scalar2=None, op0=ALU.is_ge), r=[rt_b], w=[rt_b])
                    Vv(lambda e, n=n, gl=gl: e.tensor_scalar(out=rt[0:n, 48:52], in0=gl, scalar1=rt[0:n, 1:2], scalar2=None, op0=ALU.subtract), r=[rt_b], w=[rt_b])
                    Aa(lambda e, n=n: e.activation(out=rt[0:n, 48:52], in_=rt[0:n, 48:52], func=AF.Exp, accum_out=rt[0:n, 2:3]), r=[rt_b], w=[rt_b])
                    Vv(lambda e, n=n: e.reciprocal(out=rt[0:n, 2:3], in_=rt[0:n, 2:3]), r=[rt_b], w=[rt_b])
                    Vv(lambda e, n=n: e.tensor_scalar(out=rt[0:n, 44:48], in0=rt[0:n, 44:48], scalar1=-1.0, scalar2=1.0e9, op0=ALU.add, op1=ALU.mult), r=[rt_b], w=[rt_b])
                    M = rt[0:n, 52:84]
                    Vv(lambda e, n=n, M=M, el=el: e.tensor_tensor(out=M.rearrange("p (g x) -> p g x", g=4), in0=el.rearrange("p (g x) -> p g x", g=4), in1=rt[0:n, 44:48].unsqueeze(2).to_broadcast([n, 4, 8]), op=ALU.add), r=[rt_b], w=[rt_b])
                    Vv(lambda e, n=n, M=M: e.tensor_reduce(out=rt[0:n, 3:4], in_=M, axis=AX.X, op=ALU.max), r=[rt_b], w=[rt_b])
                    O1 = rt[0:n, 84:116]
                    Vv(lambda e, n=n, M=M, O1=O1: e.tensor_scalar(out=O1, in0=M, scalar1=rt[0:n, 3:4], scalar2=None, op0=ALU.is_ge), r=[rt_b], w=[rt_b])
                    Vv(lambda e, n=n, M=M, O1=O1: e.scalar_tensor_tensor(out=M, in0=O1, scalar=-1.0e9, in1=M, op0=ALU.mult, op1=ALU.add), r=[rt_b], w=[rt_b])
                    Vv(lambda e, n=n, M=M: e.tensor_reduce(out=rt[0:n, 4:5], in_=M, axis=AX.X, op=ALU.max), r=[rt_b], w=[rt_b])
                    O2 = rt[0:n, 116:148]
                    Vv(lambda e, n=n, M=M, O2=O2: e.tensor_scalar(out=O2, in0=M, scalar1=rt[0:n, 4:5], scalar2=None, op0=ALU.is_ge), r=[rt_b], w=[rt_b])
                    Vv(lambda e, n=n: e.tensor_tensor(out=rt[0:n, 5:6], in0=rt[0:n, 4:5], in1=rt[0:n, 3:4], op=ALU.subtract), r=[rt_b], w=[rt_b])
                    Aa(lambda e, n=n: e.activation(out=rt[0:n, 5:6], in_=rt[0:n, 5:6], func=AF.Exp), r=[rt_b], w=[rt_b])
                    Vv(lambda e, n=n: e.tensor_scalar(out=rt[0:n, 6:7], in0=rt[0:n, 5:6], scalar1=1.0, scalar2=None, op0=ALU.add), r=[rt_b], w=[rt_b])
                    Vv(lambda e, n=n: e.reciprocal(out=rt[0:n, 6:7], in_=rt[0:n, 6:7]), r=[rt_b], w=[rt_b])
                    Vv(lambda e, n=n: e.tensor_tensor(out=rt[0:n, 6:7], in0=rt[0:n, 6:7], in1=rt[0:n, 2:3], op=ALU.mult), r=[rt_b], w=[rt_b])
                    Vv(lambda e, n=n: e.tensor_tensor(out=rt[0:n, 7:8], in0=rt[0:n, 6:7], in1=rt[0:n, 5:6], op=ALU.mult), r=[rt_b], w=[rt_b])
                    Vv(lambda e, n=n, t=t, O1=O1: e.tensor_scalar(out=gates[0:n, t, :], in0=O1, scalar1=rt[0:n, 6:7], scalar2=None, op0=ALU.mult), r=[rt_b], w=[gates_b])
                    Vv(lambda e, n=n, t=t, O2=O2: e.scalar_tensor_tensor(out=gates[0:n, t, :], in0=O2, scalar=rt[0:n, 7:8], in1=gates[0:n, t, :], op0=ALU.mult, op1=ALU.add), r=[rt_b, gates_b], w=[gates_b])

                items = [(ex, t) for ex in range(NEXP) for t in range(NTT)]
                NI = len(items)

                def load_w(ex):
                    wg_t, wg_b = wgu[ex % 2]
                    wd_t, wd_b = wdn[ex % 2]
                    S.dma("pool", lambda e: e.dma_start(out=wg_t[:, :, 0:256], in_=w_g[ex].rearrange("(c p) n -> p c n", p=128)), w=[wg_b])
                    S.dma("pool", lambda e: e.dma_start(out=wg_t[:, :, 256:512], in_=w_u[ex].rearrange("(c p) n -> p c n", p=128)), w=[wg_b])
                    S.dma("pool", lambda e: e.dma_start(out=wd_t[:, :, :], in_=w_d[ex].rearrange("(c p) n -> p c n", p=128)), w=[wd_b])

                def stage1(i):
                    ex, t = items[i]
                    k2 = i % 2
                    wg_t, wg_b = wgu[ex % 2]
                    for c in range(8):
                        Pp(lambda e, c=c: e.matmul(out=pP[:, k2, :], lhsT=xn2T[:, c, t * 128:(t + 1) * 128], rhs=wg_t[:, c, :], start=(c == 0), stop=(c == 7)),
                           r=[xn2T_b, wg_b], w=[pPb[k2]])
                    s_t, s_b = sil[k2]
                    a_t, a_b = actt[k2]
                    Aa(lambda e: e.activation(out=s_t[:, :], in_=pP[:, k2, 0:256], func=AF.Silu), r=[pPb[k2]], w=[s_b])
                    Vv(lambda e: e.scalar_tensor_tensor(out=a_t[:, :], in0=pP[:, k2, 256:512], scalar=gates[:, t, ex:ex + 1], in1=s_t[:, :], op0=ALU.mult, op1=ALU.mult),
                       r=[pPb[k2], s_b, gates_b], w=[a_b])


                pT_alt = pO[:, 2, :].bitcast(BF16)
                tbanks = [(pT, pT_b), (pT_alt, pOb[2])]

                def stage2(i):
                    k2 = i % 2
                    a_t, a_b = actt[k2]
                    aT_t, aT_b = actT[k2]
                    tb, tb_b = tbanks[k2]
                    for c2 in range(2):
                        Pp(lambda e, c2=c2: e.transpose(out=tb[:, c2 * 128:(c2 + 1) * 128], in_=a_t[:, c2 * 128:(c2 + 1) * 128], identity=ident[:]), r=[a_b, ident_b], w=[tb_b])
                    Aa(lambda e: e.copy(out=aT_t[:, :, :], in_=tb[:, 0:256].rearrange("p (c t) -> p c t", c=2)), r=[tb_b], w=[aT_b])

                def stage3(i):
                    ex, t = items[i]
                    k2 = i % 2
                    aT_t, aT_b = actT[k2]
                    wd_t, wd_b = wdn[ex % 2]
                    for half in range(2):
                        if k2 == 0:
                            oap = pS[:, half, :]; obuf = pSb[half]
                        else:
                            oap = pO[:, half, :]; obuf = pOb[half]
                        for c2 in range(2):
                            Pp(lambda e, c2=c2, half=half, oap=oap: e.matmul(out=oap, lhsT=aT_t[:, c2, :], rhs=wd_t[:, c2, half * 512:(half + 1) * 512], start=(c2 == 0), stop=(c2 == 1)),
                               r=[aT_b, wd_b], w=[obuf])
                        fa = facc[:, t, half * 512:(half + 1) * 512]
                        Vv(lambda e, fa=fa, oap=oap: e.tensor_tensor(out=fa, in0=oap, in1=fa, op=ALU.add), r=[obuf, faccb[t]], w=[faccb[t]])

                load_w(0)
                load_w(1)
                for i in range(NI + 2):
                    if i < NI:
                        stage1(i)
                    if 1 <= i <= NI:
                        stage2(i - 1)
                    if 2 <= i:
                        stage3(i - 2)
                        exd, td = items[i - 2]
                        if td == NTT - 1 and exd + 2 < NEXP:
                            load_w(exd + 2)
                for t in range(NTT):
                    r0, n = rows(t)
                    if t < NT:
                        S.dma("sp", lambda e, r0=r0, n=n, t=t: e.dma_start(out=y_p[r0:r0 + n, :], in_=facc[0:n, t, :]), r=[faccb[t]], is_output=True)
                    else:
                        S.dma("sp", lambda e, n=n, t=t: e.dma_start(out=y_s[0:n, :], in_=facc[0:n, t, :]), r=[faccb[t]], is_output=True)
        S.finish()
    return nc


def _consts():
    half = 32
    inv = (10000.0 ** (-np.arange(half, dtype=np.float32) / half)).astype(np.float32)

    def table(pos):
        ang = pos.astype(np.float32)[:, None] * inv[None, :]
        c = np.cos(ang).astype(np.float32); s = np.sin(ang).astype(np.float32)
        return np.concatenate([c, c, -s, s], axis=1).astype(np.float32)

    rope_p = table(np.arange(T))
    rope_s = table(np.tile(2048 + np.arange(4), NB))
    q = np.arange(128)[:, None]; k = np.arange(128)[None, :]
    cm = np.where(k <= q, 0.0, NEG).astype(np.float32)
    cm30 = np.where(k <= q, 0.0, -BIG).astype(np.float32)
    r = np.arange(NS)
    same = (r[:, None] // 4) == (r[None, :] // 4)
    caus = (r[None, :] % 4) <= (r[:, None] % 4)
    nbq = np.where(same & caus, 0.0, NEG).astype(np.float32)
    nb30 = np.where(np.arange(4)[None, :] <= (r[:, None] % 4), 0.0, -BIG).astype(np.float32)
    bdiag = same.astype(np.float32)
    bsel = np.full((NB, 8, NS), NEG, np.float32)
    for b in range(NB):
        bsel[b, :, b * 4:(b + 1) * 4] = 0.0
    i8 = np.tile(np.eye(64, dtype=np.float32), (1, 8))
    return dict(rope_p=rope_p, rope_s=rope_s, c_ident=np.eye(128, dtype=np.float32), c_cm=cm, c_cm30=cm30,
                c_nbq=nbq, c_nb30=nb30, c_bdiag=bdiag, c_bsel=bsel.reshape(1, NB * 512), c_i8=i8)


def make_in_maps(inp, cores):
    f = lambda a: np.ascontiguousarray(np.asarray(a, dtype=np.float32))
    n_pool = inp["cache_a_k"].shape[1]
    cons = _consts()
    shared = dict(
        cak=f(inp["cache_a_k"]).reshape(n_pool * 128, 512), cav=f(inp["cache_a_v"]).reshape(n_pool * 128, 512),
        cbk=f(inp["cache_b_k"]).reshape(n_pool * 128, 512), cbv=f(inp["cache_b_v"]).reshape(n_pool * 128, 512),
        cik=f(inp["cache_idx_k"]).reshape(n_pool * 128, 64),
        g_mix=f(inp["g_mix"]).reshape(1, D), w_in=f(inp["w_in"]).reshape(D, INW),
        gains=np.concatenate([f(inp[k]).reshape(1, 64) for k in ("q_norm_a", "k_norm_a", "q_norm_b", "k_norm_b")], axis=1),
        lams=np.concatenate([f(inp[k]).reshape(1, 64) for k in ("lambda_q1", "lambda_k1", "lambda_q2", "lambda_k2")], axis=1),
        subln=f(inp["subln_b"]).reshape(1, 128), w_out=f(inp["w_out"]).reshape(D, D), g_ffn=f(inp["g_ffn"]).reshape(1, D),
        w_r=np.concatenate([f(inp["w_router_group"]).reshape(D, 4), f(inp["w_router_expert"]).reshape(D, 32)], axis=1),
        b_r=np.concatenate([f(inp["b_router_group"]).reshape(1, 4), f(inp["b_router_expert"]).reshape(1, 32)], axis=1),
        w_g=f(inp["w_exp_gate"]).reshape(NEXP, D, DE), w_u=f(inp["w_exp_up"]).reshape(NEXP, D, DE), w_d=f(inp["w_exp_down"]).reshape(NEXP, DE, D),
        **cons)
    xp = f(inp["x_prompt"]); xs = f(inp["x_sample"])
    pt = np.ascontiguousarray(np.asarray(inp["page_table"], dtype=np.int32))
    maps = []
    for c in cores:
        m = dict(shared)
        m["xp"] = xp[c]
        m["xs"] = xs[c * NB:(c + 1) * NB].reshape(NS, D)
        m["ptab"] = pt[c * NB:(c + 1) * NB].reshape(1, NB * NPG)
        m["ptabT"] = np.ascontiguousarray(pt[c * NB:(c + 1) * NB].T).reshape(1, NPG * NB)
        maps.append(m)
    return maps, n_pool


def assemble(results, n_cores=8):
    def cat(name, shp):
        return np.stack([np.asarray(r[name], dtype=np.float32) for r in results], axis=0).reshape(shp)
    B = n_cores
    y_p = cat("y_p", (B, T, D)); y_s = cat("y_s", (B * NB, 4, D))
    return (y_p, y_s,
            cat("oak_p", (1, B, T, 8, 64)), cat("oav_p", (1, B, T, 8, 64)), cat("oik_p", (1, B, T, 64)),
            cat("obk_p", (1, B, T, 4, 2, 64)), cat("obv_p", (1, B, T, 4, 128)),
            cat("oak_s", (1, B * NB, 4, 8, 64)), cat("oav_s", (1, B * NB, 4, 8, 64)), cat("oik_s", (1, B * NB, 4, 64)),
            cat("obk_s", (1, B * NB, 4, 4, 2, 64)), cat("obv_s", (1, B * NB, 4, 4, 128)))


def kernel(**inputs):
    maps, n_pool = make_in_maps(inputs, list(range(8)))
    nc = build(n_pool)
    res = run_bass_kernel_spmd(nc, maps, core_ids=list(range(8)))
    return assemble(res.results, 8)
```

```python
import contextlib
import numpy as np
import concourse.bass as bass
import concourse.mybir as mybir
from concourse.bass_utils import run_bass_kernel_spmd

F32 = mybir.dt.float32
BF16 = mybir.dt.bfloat16
I32 = mybir.dt.int32
ALU = mybir.AluOpType
AF = mybir.ActivationFunctionType
AX = mybir.AxisListType

D = 1024
T = 2048
NT = 16
NS = 64
NB = 16
NPG = 16
INW = 3656
NEXP = 32
DE = 256
NEG = -30000.0
BIG = 1.0e30
EPS = 1e-6
TOPK = 256
NBIS = 14


class Buf:
    __slots__ = ("name", "last_w", "reads")

    def __init__(self, name):
        self.name = name
        self.last_w = None
        self.reads = []


class Sched:
    ENG = ("pe", "dve", "act", "pool", "sp")

    def __init__(self, nc, es, n_dma_sems=32, same_engine_sync=("pool", "dve", "act")):
        self.nc = nc
        self.count = {e: 0 for e in self.ENG}
        self.waited = {e: {} for e in self.ENG}
        self.n_dma_sems = n_dma_sems
        self.dma_rr = 0
        self.dma_cnt = [0] * n_dma_sems
        self.same_engine_sync = set(same_engine_sync)
        self.out_tokens = []
        self.sem = {}
        for e in self.ENG:
            self.sem["e_" + e] = es.enter_context(nc.semaphore("se_" + e))
        for i in range(n_dma_sems):
            self.sem["d%d" % i] = es.enter_context(nc.semaphore("sd%d" % i))
        self.eobj = {"pe": nc.tensor, "dve": nc.vector, "act": nc.scalar,
                     "pool": nc.gpsimd, "sp": nc.sync}

    def _deps(self, eng, reads, writes):
        deps = []
        for b in list(reads) + list(writes):
            if b.last_w is not None:
                deps.append(b.last_w)
        for b in writes:
            deps.extend(b.reads)
        waits = []
        for (sk, val, src_eng) in deps:
            if src_eng == eng and eng not in self.same_engine_sync and not sk.startswith("d"):
                continue
            if self.waited[eng].get(sk, 0) >= val:
                continue
            self.waited[eng][sk] = val
            waits.append((sk, val))
        return waits

    def _commit(self, tok, reads, writes):
        for b in reads:
            b.reads.append(tok)
            if len(b.reads) > 64:
                best = {}
                for t in b.reads:
                    if t[0] not in best or best[t[0]][1] < t[1]:
                        best[t[0]] = t
                b.reads = list(best.values())
        for b in writes:
            b.last_w = tok
            b.reads = []

    def _emit(self, eng, waits, fn, inc):
        e = self.eobj[eng]
        for (wk, wv) in waits:
            e.wait_ge(self.sem[wk], wv)
        fn(e).then_inc(self.sem[inc[0]], inc[1])

    def op(self, eng, fn, r=(), w=()):
        waits = self._deps(eng, r, w)
        self.count[eng] += 1
        tok = ("e_" + eng, self.count[eng], eng)
        self._emit(eng, waits, fn, ("e_" + eng, 1))
        self._commit(tok, r, w)
        return tok

    def dma(self, eng, fn, r=(), w=(), is_output=False):
        waits = self._deps(eng, r, w)
        i = self.dma_rr
        self.dma_rr = (self.dma_rr + 1) % self.n_dma_sems
        sk = "d%d" % i
        prev = self.dma_cnt[i]
        if prev > 0 and self.waited[eng].get(sk, 0) < prev:
            self.waited[eng][sk] = prev
            waits.append((sk, prev))
        self.dma_cnt[i] += 16
        tok = (sk, self.dma_cnt[i], eng)
        self._emit(eng, waits, fn, (sk, 16))
        self._commit(tok, r, w)
        if is_output:
            self.out_tokens.append(tok)
        return tok

    def barrier(self):
        for eng in self.ENG:
            e = self.eobj[eng]
            for f in self.ENG:
                if f == eng or self.count[f] == 0:
                    continue
                sk = "e_" + f
                if self.waited[eng].get(sk, 0) < self.count[f]:
                    self.waited[eng][sk] = self.count[f]
                    e.wait_ge(self.sem[sk], self.count[f])
            for i in range(self.n_dma_sems):
                sk = "d%d" % i
                if self.dma_cnt[i] > 0 and self.waited[eng].get(sk, 0) < self.dma_cnt[i]:
                    self.waited[eng][sk] = self.dma_cnt[i]
                    e.wait_ge(self.sem[sk], self.dma_cnt[i])

    def finish(self):
        seen = {}
        for (sk, val, _e) in self.out_tokens:
            seen[sk] = max(seen.get(sk, 0), val)
        for sk, val in seen.items():
            self.eobj["sp"].wait_ge(self.sem[sk], val)


def build(n_pool, phases=("A", "S", "B"), debug=False):
    nc = bass.Bass("TRN2", target_bir_lowering=False)

    def din(name, shape, dt=F32):
        return nc.dram_tensor(name, list(shape), dt, kind="ExternalInput").ap()

    def dout(name, shape, dt=F32):
        return nc.dram_tensor(name, list(shape), dt, kind="ExternalOutput").ap()

    xp = din("xp", [T, D]); xs = din("xs", [NS, D])
    cak = din("cak", [n_pool * 128, 512]); cav = din("cav", [n_pool * 128, 512])
    cbk = din("cbk", [n_pool * 128, 512]); cbv = din("cbv", [n_pool * 128, 512])
    cik = din("cik", [n_pool * 128, 64])
    ptab = din("ptab", [1, NB * NPG], I32)
    ptabT = din("ptabT", [1, NPG * NB], I32)
    g_mix = din("g_mix", [1, D]); w_in = din("w_in", [D, INW])
    gains = din("gains", [1, 4 * 64])
    lams = din("lams", [1, 4 * 64])
    subln = din("subln", [1, 128]); w_out = din("w_out", [D, D]); g_ffn = din("g_ffn", [1, D])
    w_r = din("w_r", [D, 36]); b_r = din("b_r", [1, 36])
    w_g = din("w_g", [NEXP, D, DE]); w_u = din("w_u", [NEXP, D, DE]); w_d = din("w_d", [NEXP, DE, D])
    rope_p = din("rope_p", [T, 128]); rope_s = din("rope_s", [NS, 128])
    c_ident = din("c_ident", [128, 128]); c_cm = din("c_cm", [128, 128]); c_cm30 = din("c_cm30", [128, 128])
    c_nbq = din("c_nbq", [64, 64]); c_nb30 = din("c_nb30", [64, 4]); c_bdiag = din("c_bdiag", [64, 64])
    c_bsel = din("c_bsel", [1, NB * 512]); c_i8 = din("c_i8", [64, 512])

    y_p = dout("y_p", [T, D]); y_s = dout("y_s", [NS, D])
    o_p = {k: dout("o%s_p" % k, [T, n]) for k, n in (("ak", 512), ("av", 512), ("ik", 64), ("bk", 512), ("bv", 512))}
    o_s = {k: dout("o%s_s" % k, [NS, n]) for k, n in (("ak", 512), ("av", 512), ("ik", 64), ("bk", 512), ("bv", 512))}
    mbuf = nc.dram_tensor("mbuf", [T + NS, D], BF16, kind="Internal").ap()
    if debug:
        dbg_mixed = dout("dbg_mixed", [NS, D])
        dbg_scs = dout("dbg_scs", [NS, 2052])
        dbg_mbs = dout("dbg_mbs", [NS, 2052])
        dbg_z = dout("dbg_z", [NS, 16])
    sgnbuf = nc.dram_tensor("sgnbuf", [NS, 8], F32, kind="Internal").ap()

    with contextlib.ExitStack() as es:
        S = Sched(nc, es)

        def sbt(stack, name, shape, dt):
            t = stack.enter_context(nc.sbuf_tensor(name, list(shape), dt))
            return t, Buf(name)

        def pst(stack, name, shape, dt):
            t = stack.enter_context(nc.psum_tensor(name, list(shape), dt))
            return t, Buf(name)

        def Vv(fn, r=(), w=()): return S.op("dve", fn, r, w)
        def Aa(fn, r=(), w=()): return S.op("act", fn, r, w)
        def Gg(fn, r=(), w=()): return S.op("pool", fn, r, w)
        def Pp(fn, r=(), w=()): return S.op("pe", fn, r, w)

        pP, pP_b = pst(es, "pP", [128, 2, 512], F32)
        pPb = [Buf("pP0"), Buf("pP1")]
        pT, pT_b = pst(es, "pT", [128, 1024], BF16)
        pS, _ = pst(es, "pS", [128, 2, 512], F32)
        pSb = [Buf("pS0"), Buf("pS1")]
        pO, _ = pst(es, "pO", [128, 3, 512], F32)
        pOb = [Buf("pO0"), Buf("pO1"), Buf("pO2")]

        identf, identf_b = sbt(es, "identf", [128, 128], F32)
        ident, ident_b = sbt(es, "ident", [128, 128], BF16)
        cm, cm_b = sbt(es, "cm", [128, 128], BF16)
        cm30, cm30_b = sbt(es, "cm30", [128, 128], F32)
        gmix_t, gmix_b = sbt(es, "gmix_t", [128, D], F32)
        Gt, Gt_b = sbt(es, "Gt", [128, 5, 64], F32)
        Gs, Gs_b = sbt(es, "Gs", [128, 5, 64], F32)
        subln_t, subln_b = sbt(es, "subln_t", [128, 128], F32)
        br_t, br_b = sbt(es, "br_t", [128, 36], F32)
        lam_t, lam_b = sbt(es, "lam_t", [128, 4, 64], F32)
        lamv, lamv_b = sbt(es, "lamv", [128, 4], F32)
        half_t, half_b = sbt(es, "half_t", [128, 1], F32)

        S.dma("sp", lambda e: e.dma_start(out=identf[:], in_=c_ident[:, :]), w=[identf_b])
        Vv(lambda e: e.tensor_copy(out=ident[:], in_=identf[:]), r=[identf_b], w=[ident_b])
        S.dma("pool", lambda e: e.dma_start(out=cm[:], in_=c_cm[:, :]), w=[cm_b])
        S.dma("sp", lambda e: e.dma_start(out=cm30[:], in_=c_cm30[:, :]), w=[cm30_b])
        S.dma("sp", lambda e: e.dma_start(out=gmix_t[:], in_=g_mix.partition_broadcast(128)), w=[gmix_b])
        S.dma("sp", lambda e: e.dma_start(out=subln_t[:], in_=subln.partition_broadcast(128)), w=[subln_b])
        S.dma("sp", lambda e: e.dma_start(out=br_t[:], in_=b_r.partition_broadcast(128)), w=[br_b])
        S.dma("sp", lambda e: e.dma_start(out=lam_t[:].rearrange("p a b -> p (a b)"), in_=lams.partition_broadcast(128)), w=[lam_b])
        Vv(lambda e: e.memset(Gt[:], 1.0), w=[Gt_b])
        Vv(lambda e: e.memset(half_t[:], 0.5), w=[half_b])
        S.dma("sp", lambda e: e.dma_start(out=Gt[:, 0:4, :].rearrange("p a b -> p (a b)"), in_=gains.partition_broadcast(128)), w=[Gt_b])
        Vv(lambda e: e.tensor_copy(out=Gs[:, :, 0:32], in_=Gt[:, :, 32:64]), r=[Gt_b], w=[Gs_b])
        Vv(lambda e: e.tensor_copy(out=Gs[:, :, 32:64], in_=Gt[:, :, 0:32]), r=[Gt_b], w=[Gs_b])
        Vv(lambda e: e.tensor_tensor(out=lam_t[:, 0, :], in0=lam_t[:, 0, :], in1=lam_t[:, 1, :], op=ALU.mult), r=[lam_b], w=[lam_b])
        Vv(lambda e: e.tensor_tensor(out=lam_t[:, 2, :], in0=lam_t[:, 2, :], in1=lam_t[:, 3, :], op=ALU.mult), r=[lam_b], w=[lam_b])
        Vv(lambda e: e.tensor_reduce(out=lamv[:, 0:1], in_=lam_t[:, 0, :], axis=AX.X, op=ALU.add), r=[lam_b], w=[lamv_b])
        Vv(lambda e: e.tensor_reduce(out=lamv[:, 1:2], in_=lam_t[:, 2, :], axis=AX.X, op=ALU.add), r=[lam_b], w=[lamv_b])
        Aa(lambda e: e.activation(out=lamv[:, 0:2], in_=lamv[:, 0:2], func=AF.Exp), r=[lamv_b], w=[lamv_b])
        Vv(lambda e: e.tensor_tensor(out=lamv[:, 2:3], in0=lamv[:, 1:2], in1=lamv[:, 0:1], op=ALU.subtract), r=[lamv_b], w=[lamv_b])
        Vv(lambda e: e.tensor_scalar(out=lamv[:, 3:4], in0=lamv[:, 2:3], scalar1=-0.2, scalar2=None, op0=ALU.add), r=[lamv_b], w=[lamv_b])
        neglam = lamv[:, 3:4]

        def rstd_from_ssq(t_ap, n_inv, bufs):
            Vv(lambda e: e.tensor_scalar(out=t_ap, in0=t_ap, scalar1=n_inv, scalar2=EPS, op0=ALU.mult, op1=ALU.add), r=bufs, w=bufs)
            Aa(lambda e: e.activation(out=t_ap, in_=t_ap, func=AF.Ln), r=bufs, w=bufs)
            Aa(lambda e: e.activation(out=t_ap, in_=t_ap, func=AF.Exp, scale=-0.5), r=bufs, w=bufs)

        mixed_s, mixed_s_b = sbt(es, "mixed_s", [NS, D], BF16)

        hbuf_b = Buf("hbuf")
        mbuf_b = Buf("mbuf")

        sA = es.enter_context(contextlib.ExitStack())
        sq_s = {}
        for nm, shp, dt in (("aqT_s", [128, 4, NS], BF16), ("bqT_s", [128, 4, NS], BF16), ("iqT_s8", [64, 8, NS], BF16),
                            ("akT_s", [128, 4, NS], BF16), ("bkT_s", [128, 4, NS], BF16), ("ikT_s", [128, NS], BF16),
                            ("av_s", [NS, 512], BF16), ("bv_s", [NS, 512], BF16), ("wsg_s", [NS, 8], F32)):
            sq_s[nm] = sbt(es, nm, shp, dt)

        with contextlib.ExitStack() as pa:
            win_sb, win_b = sbt(pa, "win_sb", [128, 8, INW], BF16)
            akT, _ = sbt(pa, "akT", [128, 4, T], BF16)
            bkT, _ = sbt(pa, "bkT", [128, 4, T], BF16)
            ikT2, _ = sbt(pa, "ikT2", [128, T], BF16)
            av_sb, _ = sbt(pa, "av_sb", [128, NT, 8, 65], BF16)
            bv_sb, _ = sbt(pa, "bv_sb", [128, NT, 4, 129], BF16)
            kvb = [Buf("kv%d" % i) for i in range(NT)]
            xt, xt_b = sbt(pa, "xt", [128, D], F32)
            xn, xn_b = sbt(pa, "xn", [128, D], BF16)
            xnT, xnT_b = sbt(pa, "xnT", [128, 8, 128], BF16)
            hs, _ = sbt(pa, "hs", [128, 2, 512], F32)
            hsb = [Buf("hs0"), Buf("hs1")]
            kvout, kvout_b = sbt(pa, "kvout", [128, 1088], F32)
            qtm, qtm_b = sbt(pa, "qtm", [128, 5 * 512 + 128], BF16)
            qTs = [sbt(pa, "qT%d" % k, [128, 3, 4, 128], BF16) for k in range(2)]
            rp, rp_b = sbt(pa, "rp", [128, 128], F32)
            Ct, Ct_b = sbt(pa, "Ct", [128, 5, 64], F32)
            St, St_b = sbt(pa, "St", [128, 5, 64], F32)
            st8, st8_b = sbt(pa, "st8", [128, 16], F32)
            wsg, wsg_b = sbt(pa, "wsg", [128, 3, 8], F32)
            t2, t2_b = sbt(pa, "t2", [128, 512], F32)
            sc, sc_b = sbt(pa, "sc", [128, T], F32)
            rr, _ = sbt(pa, "rr", [128, 3, 512], BF16)
            rrb = [Buf("rr0"), Buf("rr1"), Buf("rr2")]
            dg, dg_b = sbt(pa, "dg", [128, 8, 128], BF16)
            mbs2 = [sbt(pa, "mb%d" % k, [128, T], BF16) for k in range(2)]
            bis, bis_b = sbt(pa, "bis", [128, 8], F32)
            PT, _ = sbt(pa, "PT", [128, 3, 512], BF16)
            PTb = [Buf("PT0"), Buf("PT1"), Buf("PT2")]
            mixed, mixed_b = sbt(pa, "mixed", [128, D], BF16)
            osm, osm_b = sbt(pa, "osm", [128, 8], F32)
            bo, bo_b = sbt(pa, "bo", [128, 2, 128], F32)

            iqs, iqs_b = sbt(pa, "iqs", [128, 512], F32)
            print("phaseA sbuf remaining", nc.sbuf_bytes_remaining)
            S.dma("pool", lambda e: e.dma_start(out=win_sb[:, :, 0:2048], in_=w_in.rearrange("(c p) n -> p c n", p=128)[:, :, 0:2048]), w=[win_b])
            S.dma("pool", lambda e: e.dma_start(out=win_sb[:, :, 2048:INW], in_=w_in.rearrange("(c p) n -> p c n", p=128)[:, :, 2048:INW]), w=[win_b])
            Vv(lambda e: e.memset(av_sb[:, :, :, 64:65], 1.0), w=kvb)
            Vv(lambda e: e.memset(bv_sb[:, :, :, 128:129], 1.0), w=kvb)

            def project_tile(x_src, rope_src, n, outs, row0, ti):
                S.dma("sp", lambda e: e.dma_start(out=xt[0:n, :], in_=x_src), w=[xt_b])
                S.dma("sp", lambda e: e.dma_start(out=rp[0:n, :], in_=rope_src), w=[rp_b])
                Aa(lambda e: e.activation(out=xn[0:n, :], in_=xt[0:n, :], func=AF.Square, accum_out=st8[0:n, 0:1]), r=[xt_b], w=[xn_b, st8_b])
                rstd_from_ssq(st8[0:n, 0:1], 1.0 / D, [st8_b])
                Vv(lambda e: e.scalar_tensor_tensor(out=xn[0:n, :], in0=xt[0:n, :], scalar=st8[0:n, 0:1], in1=gmix_t[0:n, :], op0=ALU.mult, op1=ALU.mult),
                   r=[xt_b, st8_b, gmix_b], w=[xn_b])
                for c in range(8):
                    Pp(lambda e, c=c: e.transpose(out=pT[:, c * 128:c * 128 + n], in_=xn[0:n, c * 128:(c + 1) * 128], identity=ident[0:n, 0:n]),
                       r=[xn_b, ident_b], w=[pT_b])
                Aa(lambda e: e.copy(out=xnT[:, :, 0:n], in_=pT[:, :].rearrange("p (c t) -> p c t", c=8)[:, :, 0:n]), r=[pT_b], w=[xnT_b])
                Vv(lambda e: e.tensor_tensor(out=Ct[0:n], in0=Gt[0:n], in1=rp[0:n, 0:64].unsqueeze(1).to_broadcast([n, 5, 64]), op=ALU.mult), r=[Gt_b, rp_b], w=[Ct_b])
                Vv(lambda e: e.tensor_tensor(out=St[0:n], in0=Gs[0:n], in1=rp[0:n, 64:128].unsqueeze(1).to_broadcast([n, 5, 64]), op=ALU.mult), r=[Gs_b, rp_b], w=[St_b])

                chunks = [("aq", 0, 512), ("ak", 512, 512), ("av", 1024, 512), ("bq", 1536, 512), ("bk", 2048, 512),
                          ("bv", 2560, 512), ("iq", 3072, 512), ("ikw", 3584, 72)]
                kvo = {"ak": 0, "ik": 512, "bk": 576}
                for ci, (nm, c0, cw) in enumerate(chunks):
                    pb = ci % 2
                    for c in range(8):
                        Pp(lambda e, c=c, pb=pb, c0=c0, cw=cw: e.matmul(out=pP[0:n, pb, 0:cw], lhsT=xnT[:, c, 0:n], rhs=win_sb[:, c, c0:c0 + cw], start=(c == 0), stop=(c == 7)),
                           r=[xnT_b, win_b], w=[pPb[pb]])
                    hsl = hs[0:n, pb, 0:cw]
                    if ci % 2 == 0:
                        Aa(lambda e, pb=pb, cw=cw, hsl=hsl: e.copy(out=hsl, in_=pP[0:n, pb, 0:cw]), r=[pPb[pb]], w=[hsb[pb]])
                    else:
                        Vv(lambda e, pb=pb, cw=cw, hsl=hsl: e.tensor_copy(out=hsl, in_=pP[0:n, pb, 0:cw]), r=[pPb[pb]], w=[hsb[pb]])
                    hb = hsb[pb]
                    if nm in ("av", "bv"):
                        S.dma("sp", lambda e, hsl=hsl, nm=nm: e.dma_start(out=outs[nm][row0:row0 + n, :], in_=hsl), r=[hb], is_output=True)
                        if ti is not None:
                            if nm == "av":
                                Vv(lambda e, hsl=hsl: e.tensor_copy(out=av_sb[:, ti, :, 0:64], in_=hsl.rearrange("p (h d) -> p h d", h=8)), r=[hb], w=[kvb[ti]])
                            else:
                                Vv(lambda e, hsl=hsl: e.tensor_copy(out=bv_sb[:, ti, :, 0:128], in_=hsl.rearrange("p (h d) -> p h d", h=4)), r=[hb], w=[kvb[ti]])
                        else:
                            dst = sq_s["av_s"] if nm == "av" else sq_s["bv_s"]
                            Vv(lambda e, hsl=hsl, dst=dst: e.tensor_copy(out=dst[0][:, :], in_=hsl), r=[hb], w=[dst[1]])
                        continue
                    if nm == "ikw":
                        Vv(lambda e: e.tensor_scalar(out=wsg[0:n, 0, :], in0=hs[0:n, pb, 64:72], scalar1=float(8 ** -0.5 * 64 ** -0.5), scalar2=None, op0=ALU.mult), r=[hb], w=[wsg_b])
                        Vv(lambda e: e.tensor_scalar(out=wsg[0:n, 2, :], in0=wsg[0:n, 0, :], scalar1=0.0, scalar2=2.0, op0=ALU.is_ge, op1=ALU.mult), r=[wsg_b], w=[wsg_b])
                        Vv(lambda e: e.tensor_scalar(out=wsg[0:n, 2, :], in0=wsg[0:n, 2, :], scalar1=-1.0, scalar2=None, op0=ALU.add), r=[wsg_b], w=[wsg_b])
                        Vv(lambda e: e.tensor_tensor(out=wsg[0:n, 1, :], in0=wsg[0:n, 0, :], in1=wsg[0:n, 2, :], op=ALU.mult), r=[wsg_b], w=[wsg_b])
                    sec = {"aq": 0, "ak": 1, "bq": 2, "bk": 3, "iq": 4, "ikw": 4}[nm]
                    nh = 1 if nm == "ikw" else 8
                    w_ = 64 * nh
                    src = hs[0:n, pb, 0:w_]
                    src3 = src.rearrange("p (h d) -> p h d", h=nh)
                    if sec < 4:
                        Aa(lambda e, src=src, w_=w_: e.activation(out=t2[0:n, 0:w_], in_=src, func=AF.Square), r=[hb], w=[t2_b])
                        Vv(lambda e, w_=w_, nh=nh: e.tensor_reduce(out=st8[0:n, 8:8 + nh], in_=t2[0:n, 0:w_].rearrange("p (h d) -> p h d", h=nh), axis=AX.X, op=ALU.add), r=[t2_b], w=[st8_b])
                        rstd_from_ssq(st8[0:n, 8:8 + nh], 1.0 / 64, [st8_b])
                        Vv(lambda e, src3=src3, nh=nh: e.tensor_tensor(out=src3, in0=src3, in1=st8[0:n, 8:8 + nh].unsqueeze(2).to_broadcast([n, nh, 64]), op=ALU.mult), r=[hb, st8_b], w=[hb])
                    t23 = t2[0:n, 0:w_].rearrange("p (h d) -> p h d", h=nh)
                    Gg(lambda e, src3=src3, t23=t23, nh=nh, sec=sec: e.tensor_tensor(out=t23[:, :, 0:32], in0=src3[:, :, 32:64], in1=St[0:n, sec, 0:32].unsqueeze(1).to_broadcast([n, nh, 32]), op=ALU.mult), r=[hb, St_b], w=[t2_b])
                    Gg(lambda e, src3=src3, t23=t23, nh=nh, sec=sec: e.tensor_tensor(out=t23[:, :, 32:64], in0=src3[:, :, 0:32], in1=St[0:n, sec, 32:64].unsqueeze(1).to_broadcast([n, nh, 32]), op=ALU.mult), r=[hb, St_b], w=[t2_b])
                    Vv(lambda e, src3=src3, nh=nh, sec=sec: e.tensor_tensor(out=src3, in0=src3, in1=Ct[0:n, sec, :].unsqueeze(1).to_broadcast([n, nh, 64]), op=ALU.mult), r=[hb, Ct_b], w=[hb])
                    if nm == "iq":
                        Vv(lambda e, src=src, w_=w_: e.tensor_tensor(out=iqs[0:n, 0:512], in0=src, in1=t2[0:n, 0:w_], op=ALU.add), r=[hb, t2_b], w=[iqs_b])
                        continue
                    if nm in ("ak", "bk"):
                        Vv(lambda e, src=src, nm=nm: e.tensor_tensor(out=kvout[0:n, kvo[nm]:kvo[nm] + 512], in0=src, in1=t2[0:n, 0:512], op=ALU.add), r=[hb, t2_b], w=[kvout_b])
                        q0 = 512 if nm == "ak" else 1536
                        Aa(lambda e, nm=nm, q0=q0: e.copy(out=qtm[0:n, q0:q0 + 512], in_=kvout[0:n, kvo[nm]:kvo[nm] + 512]), r=[kvout_b], w=[qtm_b])
                    elif nm == "ikw":
                        Vv(lambda e, src=src: e.tensor_tensor(out=kvout[0:n, 512:576], in0=src, in1=t2[0:n, 0:64], op=ALU.add), r=[hb, t2_b], w=[kvout_b])
                        Aa(lambda e: e.copy(out=qtm[0:n, 2560:2624], in_=kvout[0:n, 512:576]), r=[kvout_b], w=[qtm_b])
                        Aa(lambda e: e.copy(out=qtm[0:n, 2624:2688], in_=kvout[0:n, 512:576]), r=[kvout_b], w=[qtm_b])
                        Vv(lambda e: e.tensor_tensor(out=qtm[0:n, 2048:2560].rearrange("p (h d) -> p h d", h=8), in0=iqs[0:n, 0:512].rearrange("p (h d) -> p h d", h=8),
                                                      in1=wsg[0:n, 1, :].unsqueeze(2).to_broadcast([n, 8, 64]), op=ALU.mult), r=[iqs_b, wsg_b], w=[qtm_b])
                    else:
                        q0 = 0 if nm == "aq" else 1024
                        Vv(lambda e, src=src, q0=q0: e.tensor_tensor(out=qtm[0:n, q0:q0 + 512], in0=src, in1=t2[0:n, 0:512], op=ALU.add), r=[hb, t2_b], w=[qtm_b])
                for k, (o0, ow) in (("ak", (0, 512)), ("ik", (512, 64)), ("bk", (576, 512))):
                    S.dma("sp", lambda e, k=k, o0=o0, ow=ow: e.dma_start(out=outs[k][row0:row0 + n, :], in_=kvout[0:n, o0:o0 + ow]), r=[kvout_b], is_output=True)

            def transposes_tile(n, ti):
                qT, qT_b = qTs[(ti or 0) % 2]
                for gi, (q0, dst) in enumerate(((0, 0), (1024, 1), (2048, 2))):
                    for j in range(4):
                        Pp(lambda e, j=j, q0=q0: e.transpose(out=pT[:, j * 128:j * 128 + n], in_=qtm[0:n, q0 + j * 128:q0 + (j + 1) * 128], identity=ident[0:n, 0:n]), r=[qtm_b, ident_b], w=[pT_b])
                    if ti is not None:
                        Vv(lambda e, dst=dst: e.tensor_copy(out=qT[:, dst, :, :], in_=pT[:, 0:512].rearrange("p (j t) -> p j t", j=4)), r=[pT_b], w=[qT_b])
                    else:
                        if dst < 2:
                            d_, db_ = sq_s["aqT_s" if dst == 0 else "bqT_s"]
                            Vv(lambda e, d_=d_: e.tensor_copy(out=d_[:, :, :], in_=pT[:, 0:512].rearrange("p (j t) -> p j t", j=4)[:, :, 0:n]), r=[pT_b], w=[db_])
                for gi, q0 in enumerate((512, 1536)):
                    for j in range(4):
                        Pp(lambda e, j=j, q0=q0: e.transpose(out=pT[:, j * 128:j * 128 + n], in_=qtm[0:n, q0 + j * 128:q0 + (j + 1) * 128], identity=ident[0:n, 0:n]), r=[qtm_b, ident_b], w=[pT_b])
                    if ti is not None:
                        d_ = akT if gi == 0 else bkT
                        Aa(lambda e, d_=d_: e.copy(out=d_[:, :, ti * 128:(ti + 1) * 128], in_=pT[:, 0:512].rearrange("p (j t) -> p j t", j=4)), r=[pT_b], w=[kvb[ti]])
                    else:
                        d_, db_ = sq_s["akT_s" if gi == 0 else "bkT_s"]
                        Aa(lambda e, d_=d_: e.copy(out=d_[:, :, :], in_=pT[:, 0:512].rearrange("p (j t) -> p j t", j=4)[:, :, 0:n]), r=[pT_b], w=[db_])
                Pp(lambda e: e.transpose(out=pT[:, 0:n], in_=qtm[0:n, 2560:2688], identity=ident[0:n, 0:n]), r=[qtm_b, ident_b], w=[pT_b])
                if ti is not None:
                    Vv(lambda e: e.tensor_copy(out=ikT2[:, ti * 128:(ti + 1) * 128], in_=pT[:, 0:128]), r=[pT_b], w=[kvb[ti]])
                else:
                    Vv(lambda e: e.tensor_copy(out=sq_s["ikT_s"][0][:, :], in_=pT[:, 0:n]), r=[pT_b], w=[sq_s["ikT_s"][1]])
                    for h in range(8):
                        Pp(lambda e, h=h: e.transpose(out=pT[0:64, h * 64:h * 64 + n], in_=qtm[0:n, 2048 + h * 64:2048 + (h + 1) * 64], identity=ident[0:n, 0:n]), r=[qtm_b, ident_b], w=[pT_b])
                    Vv(lambda e: e.tensor_copy(out=sq_s["iqT_s8"][0][:, :, :], in_=pT[0:64, 0:512].rearrange("p (h t) -> p h t", h=8)), r=[pT_b], w=[sq_s["iqT_s8"][1]])
                    Vv(lambda e: e.tensor_copy(out=sq_s["wsg_s"][0][:, :], in_=wsg[0:n, 2, :]), r=[wsg_b], w=[sq_s["wsg_s"][1]])

            def index_tile(i):
                nk = 128 * (i + 1)
                use_topk = i >= 2
                qT, qT_b = qTs[i % 2]
                mb, mb_b = mbs2[i % 2]
                if use_topk:
                    for h in range(8):
                        Vv(lambda e, h=h: e.tensor_scalar(out=dg[:, h, :], in0=identf[:], scalar1=wsg[:, 2, h:h + 1], scalar2=None, op0=ALU.mult), r=[identf_b, wsg_b], w=[dg_b])
                    nch = (nk + 511) // 512
                    ibanks = [(pP[:, 0, :], pPb[0]), (pP[:, 1, :], pPb[1]), (pO[:, 0, :], pOb[0])]
                    iunits = [(kc, h) for kc in range(nch) for h in range(8)]

                    def idx_S(u):
                        kc, h = iunits[u]
                        k0 = kc * 512; kw = min(512, nk - k0)
                        kread = [kvb[t_] for t_ in range(k0 // 128, (k0 + kw) // 128)]
                        hp = (h % 2) * 64
                        bank, bankb = ibanks[u % 3]
                        Pp(lambda e: e.matmul(out=bank[:, 0:kw], lhsT=qT[hp:hp + 64, 2, h // 2, :], rhs=ikT2[hp:hp + 64, k0:k0 + kw], start=True, stop=True), r=[qT_b] + kread, w=[bankb])
                        Aa(lambda e: e.activation(out=rr[:, u % 3, 0:kw], in_=bank[:, 0:kw], func=AF.Relu), r=[bankb], w=[rrb[u % 3]])

                    def idx_D(u):
                        kc, h = iunits[u]
                        k0 = kc * 512; kw = min(512, nk - k0)
                        Pp(lambda e: e.matmul(out=pS[:, 0, 0:kw], lhsT=dg[:, h, :], rhs=rr[:, u % 3, 0:kw], start=(h == 0), stop=(h == 7)), r=[dg_b, rrb[u % 3]], w=[pSb[0]])
                        if h == 7:
                            Vv(lambda e: e.tensor_copy(out=sc[:, k0:k0 + kw], in_=pS[:, 0, 0:kw]), r=[pSb[0]], w=[sc_b])

                    for n_ in range(len(iunits) + 2):
                        if n_ < len(iunits):
                            idx_S(n_)
                        if n_ >= 2:
                            idx_D(n_ - 2)
                    steps = []

                    def st_init():
                        Vv(lambda e: e.tensor_reduce(out=bis[:, 0:1], in_=sc[:, 0:nk], axis=AX.X, op=ALU.min), r=[sc_b], w=[bis_b])
                        Vv(lambda e: e.tensor_tensor(out=sc[:, nk - 128:nk], in0=sc[:, nk - 128:nk], in1=cm30[:], op=ALU.add), r=[sc_b, cm30_b], w=[sc_b])
                        Vv(lambda e: e.tensor_reduce(out=bis[:, 1:2], in_=sc[:, 0:nk], axis=AX.X, op=ALU.max), r=[sc_b], w=[bis_b])
                        Vv(lambda e: e.tensor_tensor(out=bis[:, 2:3], in0=bis[:, 1:2], in1=bis[:, 0:1], op=ALU.subtract), r=[bis_b], w=[bis_b])
                    steps.append(st_init)

                    def mk_it(it):
                        f = 0.5 ** (it + 1)

                        def st_it():
                            Vv(lambda e: e.scalar_tensor_tensor(out=bis[:, 3:4], in0=bis[:, 2:3], scalar=f, in1=bis[:, 0:1], op0=ALU.mult, op1=ALU.add), r=[bis_b], w=[bis_b])
                            Vv(lambda e: e.tensor_scalar(out=mb[:, 0:nk], in0=sc[:, 0:nk], scalar1=bis[:, 3:4], scalar2=None, op0=ALU.is_ge, op1=ALU.add, accum_out=bis[:, 4:5]),
                               r=[sc_b, bis_b], w=[mb_b, bis_b])
                            Vv(lambda e: e.tensor_scalar(out=bis[:, 5:6], in0=bis[:, 4:5], scalar1=float(TOPK), scalar2=f, op0=ALU.is_ge, op1=ALU.mult), r=[bis_b], w=[bis_b])
                            Vv(lambda e: e.scalar_tensor_tensor(out=bis[:, 0:1], in0=bis[:, 5:6], scalar=bis[:, 2:3], in1=bis[:, 0:1], op0=ALU.mult, op1=ALU.add), r=[bis_b], w=[bis_b])
                        return st_it
                    for it in range(NBIS):
                        steps.append(mk_it(it))

                    def st_fin():
                        Vv(lambda e: e.tensor_scalar(out=mb[:, 0:nk], in0=sc[:, 0:nk], scalar1=bis[:, 0:1], scalar2=NEG, op0=ALU.is_lt, op1=ALU.mult), r=[sc_b, bis_b], w=[mb_b])
                    steps.append(st_fin)
                    return steps
                return []

            def attend_tile(i, extra_steps=()):
                extra_steps = list(extra_steps)
                nk = 128 * (i + 1)
                use_topk = i >= 2
                qT, qT_b = qTs[i % 2]
                mb, mb_b = mbs2[i % 2]
                sbanks = [(pS[:, 0, :], pSb[0]), (pS[:, 1, :], pSb[1]), (pO[:, 2, :], pOb[2])]
                units = []
                for g in range(8):
                    kgs = list(range(0, i + 1, 4))
                    for kg in kgs:
                        units.append(("a", g, kg, kg == kgs[-1]))
                for g in range(8):
                    kgs = list(range(0, i + 1, 4))
                    for kg in kgs:
                        units.append(("b", g, kg, kg == kgs[-1]))

                def uparams(kind, g):
                    if kind == "a":
                        return dict(hp=(g % 2) * 64, blk=g // 2, kT=akT, qsel=0, ob=pOb[g % 2], oap=pO[:, g % 2, 0:65])
                    h_, c_ = g // 2, g % 2
                    return dict(hp=c_ * 64, blk=h_, kT=bkT, qsel=1, ob=pOb[c_], oap=pO[:, c_, 0:129])

                def att_QK(u):
                    kind, g, kg, _last = units[u]
                    p_ = uparams(kind, g)
                    hp, blk, kT, qsel = p_["hp"], p_["blk"], p_["kT"], p_["qsel"]
                    bank, bankb = sbanks[u % 3]
                    kts = list(range(kg, min(kg + 4, i + 1)))
                    for jj, kt in enumerate(kts):
                        need_mask = (kind == "a" and use_topk) or kt == i
                        Pp(lambda e, jj=jj, kt=kt, need_mask=need_mask: e.matmul(out=bank[:, jj * 128:(jj + 1) * 128], lhsT=kT[hp:hp + 64, blk, kt * 128:(kt + 1) * 128],
                                                                                 rhs=qT[hp:hp + 64, qsel, blk, :], start=True, stop=not need_mask),
                           r=[kvb[kt], qT_b], w=[bankb])
                        if need_mask:
                            if kind == "a" and use_topk:
                                Pp(lambda e, jj=jj, kt=kt: e.matmul(out=bank[:, jj * 128:(jj + 1) * 128], lhsT=mb[:, kt * 128:(kt + 1) * 128], rhs=ident[:], start=False, stop=True),
                                   r=[mb_b, ident_b], w=[bankb])
                            else:
                                Pp(lambda e, jj=jj: e.matmul(out=bank[:, jj * 128:(jj + 1) * 128], lhsT=cm[:], rhs=ident[:], start=False, stop=True),
                                   r=[cm_b, ident_b], w=[bankb])
                    wd = 128 * len(kts)
                    Aa(lambda e: e.activation(out=PT[:, u % 3, 0:wd], in_=bank[:, 0:wd], func=AF.Exp, scale=0.125), r=[bankb], w=[PTb[u % 3]])

                def att_PV(u):
                    kind, g, kg, last = units[u]
                    p_ = uparams(kind, g)
                    ob, oap = p_["ob"], p_["oap"]
                    kts = list(range(kg, min(kg + 4, i + 1)))
                    for jj, kt in enumerate(kts):
                        rhs = av_sb[:, kt, g, :] if kind == "a" else bv_sb[:, kt, g // 2, :]
                        Pp(lambda e, jj=jj, kt=kt, rhs=rhs: e.matmul(out=oap, lhsT=PT[:, u % 3, jj * 128:(jj + 1) * 128], rhs=rhs, start=(kt == 0), stop=(kt == i)),
                           r=[PTb[u % 3], kvb[kt]], w=[ob])
                    if not last:
                        return
                    if kind == "a":
                        Vv(lambda e: e.reciprocal(out=osm[:, g:g + 1], in_=oap[:, 64:65]), r=[ob], w=[osm_b])
                        Vv(lambda e: e.tensor_scalar(out=mixed[:, g * 64:(g + 1) * 64], in0=oap[:, 0:64], scalar1=osm[:, g:g + 1], scalar2=None, op0=ALU.mult), r=[ob, osm_b], w=[mixed_b])
                    else:
                        h_, c_ = g // 2, g % 2
                        Vv(lambda e: e.reciprocal(out=osm[:, c_:c_ + 1], in_=oap[:, 128:129]), r=[ob], w=[osm_b])
                        Vv(lambda e: e.tensor_scalar(out=bo[:, c_, :], in0=oap[:, 0:128], scalar1=osm[:, c_:c_ + 1], scalar2=None, op0=ALU.mult), r=[ob, osm_b], w=[bo_b])
                        if c_ == 1:
                            diff_finish(bo, bo_b, mixed, mixed_b, h_, 128, osm, osm_b)

                LOOK = 2
                n_total = len(units) + LOOK
                done_steps = 0
                for n_ in range(n_total):
                    if n_ < len(units):
                        att_QK(n_)
                    if n_ >= LOOK:
                        att_PV(n_ - LOOK)
                    want = (len(extra_steps) * (n_ + 1)) // n_total
                    while done_steps < want:
                        extra_steps[done_steps]()
                        done_steps += 1
                while done_steps < len(extra_steps):
                    extra_steps[done_steps]()
                    done_steps += 1

            def diff_finish(bo_, bo_b_, mixed_, mixed_b_, h_, n, osm_, osm_b_):
                Vv(lambda e: e.scalar_tensor_tensor(out=bo_[0:n, 0, :], in0=bo_[0:n, 1, :], scalar=neglam[0:n, :], in1=bo_[0:n, 0, :], op0=ALU.mult, op1=ALU.add), r=[bo_b_, lamv_b], w=[bo_b_])
                Aa(lambda e: e.activation(out=bo_[0:n, 1, :], in_=bo_[0:n, 0, :], func=AF.Square, accum_out=osm_[0:n, 2:3]), r=[bo_b_], w=[bo_b_, osm_b_])
                rstd_from_ssq(osm_[0:n, 2:3], 1.0 / 128, [osm_b_])
                Vv(lambda e: e.tensor_scalar(out=osm_[0:n, 2:3], in0=osm_[0:n, 2:3], scalar1=0.8, scalar2=None, op0=ALU.mult), r=[osm_b_], w=[osm_b_])
                Vv(lambda e: e.scalar_tensor_tensor(out=mixed_[0:n, 512 + h_ * 128:512 + (h_ + 1) * 128], in0=bo_[0:n, 0, :], scalar=osm_[0:n, 2:3], in1=subln_t[0:n, :], op0=ALU.mult, op1=ALU.mult),
                   r=[bo_b_, osm_b_, subln_b], w=[mixed_b_])

            if "A" in phases:
                project_tile(xs[:, :], rope_s[:, :], NS, o_s, 0, None)
                transposes_tile(NS, None)
                S.dma("sp", lambda e: e.dma_start(out=sgnbuf[:, :], in_=sq_s["wsg_s"][0][:, :]), r=[sq_s["wsg_s"][1]], w=[hbuf_b])
                npt = NT
                def stage_A(i):
                    project_tile(xp[i * 128:(i + 1) * 128, :], rope_p[i * 128:(i + 1) * 128, :], 128, o_p, i * 128, i)
                    transposes_tile(128, i)
                    return index_tile(i)
                for st_ in stage_A(0):
                    st_()
                for i in range(npt):
                    steps_next = stage_A(i + 1) if i + 1 < npt else []
                    attend_tile(i, steps_next)
                    S.dma("sp", lambda e, i=i: e.dma_start(out=mbuf[i * 128:(i + 1) * 128, :], in_=mixed[:, :]), r=[mixed_b], w=[mbuf_b])

        S.barrier()
        if "S" in phases:
            with contextlib.ExitStack() as ps_:
                aqT_s, aqT_s_b = sq_s["aqT_s"]; bqT_s, bqT_s_b = sq_s["bqT_s"]; iqT_s8, iqT_s8_b = sq_s["iqT_s8"]
                akT_s, akT_s_b = sq_s["akT_s"]; bkT_s, bkT_s_b = sq_s["bkT_s"]; ikT_s, ikT_s_b = sq_s["ikT_s"]
                av_s, av_s_b = sq_s["av_s"]; bv_s, bv_s_b = sq_s["bv_s"]; wsg_s, wsg_s_b = sq_s["wsg_s"]
                pti, pti_b = sbt(ps_, "pti", [128, NB * NPG], I32)
                idx, idx_b = sbt(ps_, "idx", [128, NB * NPG], I32)
                iot, iot_b = sbt(ps_, "iot", [128, 1], I32)
                signB, signB_b = sbt(ps_, "signB", [128, NB, 8, 4], F32)
                sgn_tmp, sgn_tmp_b = sbt(ps_, "sgn_tmp", [128, 512], F32)
                bselt, bselt_b = sbt(ps_, "bselt", [1, NB * 512], BF16)
                ones1, ones1_b = sbt(ps_, "ones1", [128, 128], BF16)
                zrow, zrow_b = sbt(ps_, "zrow", [1, 512], BF16)
                i8, i8_b = sbt(ps_, "i8", [64, 512], BF16)
                nbq, nbq_b = sbt(ps_, "nbq", [64, 64], F32)
                nbqh, nbqh_b = sbt(ps_, "nbqh", [64, 2, 64], BF16)
                nb30, nb30_b = sbt(ps_, "nb30", [64, 4], F32)
                bdiag, bdiag_b = sbt(ps_, "bdiag", [64, 64], F32)
                Qbd_a, Qbd_a_b = sbt(ps_, "Qbd_a", [128, 4, 2, NS], BF16)
                Qbd_b, Qbd_b_b = sbt(ps_, "Qbd_b", [128, 4, 2, NS], BF16)
                ipg = [sbt(ps_, "ipg%d" % k, [128, NPG, 64], BF16) for k in range(2)]
                pti2, pti2_b = sbt(ps_, "pti2", [128, NB], I32)
                idx2, idx2_b = sbt(ps_, "idx2", [128, NB], I32)
                iot7, iot7_b = sbt(ps_, "iot7", [128, 1], I32)
                ikTp, ikTp_b = sbt(ps_, "ikTp", [64, NPG, 128], BF16)
                Rb, Rb_b = sbt(ps_, "Rb", [128, NPG, 8, 4], F32)
                STall, STall_b = sbt(ps_, "STall", [128, NPG, NS], F32)
                scs, scs_b = sbt(ps_, "scs", [NS, 2052], F32)
                mbs, mbs_b = sbt(ps_, "mbs", [NS, 2052], BF16)
                bis2, bis2_b = sbt(ps_, "bis2", [NS, 8], F32)
                snew, snew_b = sbt(ps_, "snew", [NS, 8, 64], F32)
                sacc, sacc_b = sbt(ps_, "sacc", [NS, 64], F32)
                pg = {k: [sbt(ps_, "pg%s%d" % (k, j), [128, 512], BF16) for j in range(4)] for k in ("ak", "av", "bk", "bv")}
                kTp = {k: [sbt(ps_, "kTp%s%d" % (k, j), [128, 4, 128], BF16) for j in range(2)] for k in ("ak", "bk")}
                PTs = [sbt(ps_, "PTs%d" % j, [128, 512], BF16) for j in range(2)]
                PTd = [sbt(ps_, "PTd%d" % j, [128, 512], BF16) for j in range(2)]
                zr, zr_b = sbt(ps_, "zr", [1, 2, 512], F32)
                zc, zc_b = sbt(ps_, "zc", [NS, 16], F32)
                oa_s, oa_s_b = sbt(ps_, "oa_s", [NS, 8, 64], F32)
                bo_s, bo_s_b = sbt(ps_, "bo_s", [NS, 2, 128], F32)
                osm_s, osm_s_b = sbt(ps_, "osm_s", [NS, 8], F32)

                S.dma("sp", lambda e: e.dma_start(out=pti[:], in_=ptab.partition_broadcast(128)), w=[pti_b])
                Gg(lambda e: e.iota(out=iot[:], pattern=[[0, 1]], base=0, channel_multiplier=1), w=[iot_b])
                Gg(lambda e: e.tensor_scalar(out=idx[:], in0=pti[:], scalar1=128, scalar2=None, op0=ALU.mult), r=[pti_b], w=[idx_b])
                Gg(lambda e: e.tensor_tensor(out=idx[:], in0=idx[:], in1=iot[:].to_broadcast([128, NB * NPG]), op=ALU.add), r=[idx_b, iot_b], w=[idx_b])
                for j in range(NPG):
                    S.dma("sp", lambda e, j=j: e.dma_start(out=pti2[j * 8:(j + 1) * 8, :], in_=ptabT[:, j * NB:(j + 1) * NB].partition_broadcast(8)), w=[pti2_b])
                Vv(lambda e: e.tensor_single_scalar(out=iot7[:], in_=iot[:], scalar=7, op=ALU.bitwise_and), r=[iot_b], w=[iot7_b])
                Gg(lambda e: e.tensor_scalar(out=idx2[:], in0=pti2[:], scalar1=8, scalar2=None, op0=ALU.mult), r=[pti2_b], w=[idx2_b])
                Gg(lambda e: e.tensor_tensor(out=idx2[:], in0=idx2[:], in1=iot7[:].to_broadcast([128, NB]), op=ALU.add), r=[idx2_b, iot7_b], w=[idx2_b])
                cik2 = cik.rearrange("(r t) d -> r (t d)", t=16)
                S.dma("pool", lambda e: e.dma_start(out=bselt[:], in_=c_bsel[:, :]), w=[bselt_b])
                S.dma("pool", lambda e: e.dma_start(out=i8[:], in_=c_i8[:, :]), w=[i8_b])
                S.dma("sp", lambda e: e.dma_start(out=nbq[:], in_=c_nbq[:, :]), w=[nbq_b])
                S.dma("sp", lambda e: e.dma_start(out=nb30[:], in_=c_nb30[:, :]), w=[nb30_b])
                S.dma("sp", lambda e: e.dma_start(out=bdiag[:], in_=c_bdiag[:, :]), w=[bdiag_b])
                Vv(lambda e: e.memset(ones1[:], 1.0), w=[ones1_b])
                Vv(lambda e: e.memset(zrow[:], 0.0), w=[zrow_b])
                S.dma("sp", lambda e: e.dma_start(out=sgn_tmp[:, :], in_=sgnbuf.rearrange("(o r) h -> o (r h)", o=1).partition_broadcast(128)), r=[hbuf_b], w=[sgn_tmp_b])
                Vv(lambda e: e.tensor_copy(out=signB[:], in_=sgn_tmp[:, :].rearrange("p (b q h) -> p b h q", b=NB, q=4)), r=[sgn_tmp_b], w=[signB_b])
                Vv(lambda e: e.memset(Qbd_a[:], 0.0), w=[Qbd_a_b])
                Vv(lambda e: e.memset(Qbd_b[:], 0.0), w=[Qbd_b_b])
                for (Qd, Qd_b, src, src_b) in ((Qbd_a, Qbd_a_b, aqT_s, aqT_s_b), (Qbd_b, Qbd_b_b, bqT_s, bqT_s_b)):
                    Vv(lambda e, Qd=Qd, src=src: e.tensor_copy(out=Qd[0:64, :, 0, :], in_=src[0:64, :, :]), r=[src_b], w=[Qd_b])
                    Vv(lambda e, Qd=Qd, src=src: e.tensor_copy(out=Qd[64:128, :, 1, :], in_=src[64:128, :, :]), r=[src_b], w=[Qd_b])

                for b in range(NB):
                    ip, ip_b = ipg[b % 2]
                    S.dma("pool", lambda e, b=b, ip=ip: e.indirect_dma_start(out=ip[:, :, :].rearrange("p t d -> p (t d)"), out_offset=None, in_=cik2,
                                                                              in_offset=bass.IndirectOffsetOnAxis(ap=idx2[:, b:b + 1], axis=0)), r=[idx2_b], w=[ip_b])
                    for half in range(2):
                        for j in range(8):
                            jj = half * 8 + j
                            Pp(lambda e, j=j, jj=jj, ip=ip: e.transpose(out=pT[0:64, j * 128:(j + 1) * 128], in_=ip[:, jj, :], identity=ident[:]), r=[ip_b, ident_b], w=[pT_b])
                        Vv(lambda e, half=half: e.tensor_copy(out=ikTp[:, half * 8:(half + 1) * 8, :], in_=pT[0:64, :].rearrange("p (j t) -> p j t", j=8)), r=[pT_b], w=[ikTp_b])
                    pb = b % 2
                    for j in range(NPG):
                        Pp(lambda e, j=j, b=b, pb=pb: e.matmul(out=pP[:, pb, j * 32:(j + 1) * 32].rearrange("p (h q) -> p h q", h=8), lhsT=ikTp[:, j, :], rhs=iqT_s8[:, :, b * 4:(b + 1) * 4], start=True, stop=True),
                           r=[ikTp_b, iqT_s8_b], w=[pPb[pb]])
                    Aa(lambda e, pb=pb: e.activation(out=Rb[:].rearrange("p j h q -> p (j h q)"), in_=pP[:, pb, :], func=AF.Relu), r=[pPb[pb]], w=[Rb_b])
                    Vv(lambda e, b=b: e.tensor_tensor(out=Rb[:], in0=Rb[:], in1=signB[:, b, :, :].unsqueeze(1).to_broadcast([128, NPG, 8, 4]), op=ALU.mult), r=[Rb_b, signB_b], w=[Rb_b])
                    Vv(lambda e, b=b: e.tensor_reduce(out=STall[:, :, b * 4:(b + 1) * 4], in_=Rb[:].rearrange("p j h q -> p j q h"), axis=AX.X, op=ALU.add), r=[Rb_b], w=[STall_b])
                for j in range(NPG):
                    pb = j % 2
                    Pp(lambda e, j=j, pb=pb: e.transpose(out=pP[0:NS, pb, 0:128], in_=STall[:, j, :], identity=identf[:]), r=[STall_b, identf_b], w=[pPb[pb]])
                    Vv(lambda e, j=j, pb=pb: e.tensor_copy(out=scs[:, 0:2048].rearrange("r (g t) -> r g t", t=16)[:, :, j], in_=pP[0:NS, pb, 0:128]), r=[pPb[pb]], w=[scs_b])
                for h in range(8):
                    Pp(lambda e, h=h: e.matmul(out=pS[0:NS, 0, h * 64:(h + 1) * 64], lhsT=iqT_s8[:, h, :], rhs=ikT_s[0:64, :], start=True, stop=True), r=[iqT_s8_b, ikT_s_b], w=[pSb[0]])
                Aa(lambda e: e.activation(out=snew[:].rearrange("p h r -> p (h r)"), in_=pS[0:NS, 0, :], func=AF.Relu), r=[pSb[0]], w=[snew_b])
                Vv(lambda e: e.tensor_scalar(out=sacc[:], in0=snew[:, 0, :], scalar1=wsg_s[:, 0:1], scalar2=None, op0=ALU.mult), r=[snew_b, wsg_s_b], w=[sacc_b])
                for h in range(1, 8):
                    Vv(lambda e, h=h: e.scalar_tensor_tensor(out=sacc[:], in0=snew[:, h, :], scalar=wsg_s[:, h:h + 1], in1=sacc[:], op0=ALU.mult, op1=ALU.add), r=[snew_b, wsg_s_b, sacc_b], w=[sacc_b])
                Vv(lambda e: e.tensor_tensor(out=sacc[:], in0=sacc[:], in1=bdiag[:], op=ALU.mult), r=[sacc_b, bdiag_b], w=[sacc_b])
                Vv(lambda e: e.tensor_reduce(out=scs[:, 2048:2052], in_=sacc[:].rearrange("p (b j) -> p j b", j=4), axis=AX.X, op=ALU.add), r=[sacc_b], w=[scs_b])
                Vv(lambda e: e.tensor_reduce(out=bis2[:, 0:1], in_=scs[:, :], axis=AX.X, op=ALU.min), r=[scs_b], w=[bis2_b])
                Vv(lambda e: e.tensor_tensor(out=scs[:, 2048:2052], in0=scs[:, 2048:2052], in1=nb30[:], op=ALU.add), r=[scs_b, nb30_b], w=[scs_b])
                Vv(lambda e: e.tensor_reduce(out=bis2[:, 1:2], in_=scs[:, :], axis=AX.X, op=ALU.max), r=[scs_b], w=[bis2_b])
                Vv(lambda e: e.tensor_tensor(out=bis2[:, 2:3], in0=bis2[:, 1:2], in1=bis2[:, 0:1], op=ALU.subtract), r=[bis2_b], w=[bis2_b])
                for it in range(NBIS):
                    f = 0.5 ** (it + 1)
                    Vv(lambda e, f=f: e.scalar_tensor_tensor(out=bis2[:, 3:4], in0=bis2[:, 2:3], scalar=f, in1=bis2[:, 0:1], op0=ALU.mult, op1=ALU.add), r=[bis2_b], w=[bis2_b])
                    Vv(lambda e: e.tensor_scalar(out=mbs[:, :], in0=scs[:, :], scalar1=bis2[:, 3:4], scalar2=None, op0=ALU.is_ge, op1=ALU.add, accum_out=bis2[:, 4:5]), r=[scs_b, bis2_b], w=[mbs_b, bis2_b])
                    Vv(lambda e, f=f: e.tensor_scalar(out=bis2[:, 5:6], in0=bis2[:, 4:5], scalar1=float(TOPK), scalar2=f, op0=ALU.is_ge, op1=ALU.mult), r=[bis2_b], w=[bis2_b])
                    Vv(lambda e: e.scalar_tensor_tensor(out=bis2[:, 0:1], in0=bis2[:, 5:6], scalar=bis2[:, 2:3], in1=bis2[:, 0:1], op0=ALU.mult, op1=ALU.add), r=[bis2_b], w=[bis2_b])
                Vv(lambda e: e.tensor_scalar(out=mbs[:, :], in0=scs[:, :], scalar1=bis2[:, 0:1], scalar2=NEG, op0=ALU.is_lt, op1=ALU.mult), r=[scs_b, bis2_b], w=[mbs_b])
                Vv(lambda e: e.tensor_copy(out=nbqh[:, 0, :], in_=nbq[:]), r=[nbq_b], w=[nbqh_b])
                Vv(lambda e: e.tensor_tensor(out=nbqh[:, 1, :].rearrange("p (b j) -> p b j", j=4), in0=nbq[:].rearrange("p (b j) -> p b j", j=4),
                                              in1=mbs[:, 2048:2052].unsqueeze(1).to_broadcast([NS, NB, 4]), op=ALU.add), r=[nbq_b, mbs_b], w=[nbqh_b])

                oA = pO[0:NS, 0, :]
                oB = pO[0:NS, 1:3, :]
                zA = pP[0:1, 0, :]
                zB = pP[32:33, 0, :]
                pT2 = pP[:, 1, :].bitcast(BF16)
                steps3 = [(b, j) for b in range(NB) for j in range(NPG)] + [(NB, 0)]
                nsteps = len(steps3)

                def s3_L(i):
                    b, j = steps3[i]
                    if b == NB:
                        return
                    col = b * NPG + j
                    for k, src in (("ak", cak), ("av", cav), ("bk", cbk), ("bv", cbv)):
                        tt, tb = pg[k][i % 4]
                        S.dma("pool", lambda e, tt=tt, src=src: e.indirect_dma_start(out=tt[:, :], out_offset=None, in_=src[:, :],
                                                                                    in_offset=bass.IndirectOffsetOnAxis(ap=idx[:, col:col + 1], axis=0)), r=[idx_b], w=[tb])

                def s3_T(i):
                    b, j = steps3[i]
                    if b == NB:
                        return
                    for k, (tbank, tbank_b) in (("ak", (pT, pT_b)), ("bk", (pT2, pPb[1]))):
                        tt, tb = pg[k][i % 4]
                        dd, ddb = kTp[k][i % 2]
                        for q4 in range(4):
                            Pp(lambda e, q4=q4: e.transpose(out=tbank[:, q4 * 128:(q4 + 1) * 128], in_=tt[:, q4 * 128:(q4 + 1) * 128], identity=ident[:]), r=[tb, ident_b], w=[tbank_b])
                        if k == "ak":
                            Vv(lambda e: e.tensor_copy(out=dd[:, :, :], in_=tbank[:, 0:512].rearrange("p (j t) -> p j t", j=4)), r=[tbank_b], w=[ddb])
                        else:
                            Aa(lambda e: e.copy(out=dd[:, :, :], in_=tbank[:, 0:512].rearrange("p (j t) -> p j t", j=4)), r=[tbank_b], w=[ddb])

                def s3_ops(i):
                    b, j = steps3[i]
                    if b < NB:
                        akt, akt_b = kTp["ak"][i % 2]; bkt, bkt_b = kTp["bk"][i % 2]
                        avt, avt_b = pg["av"][i % 4]; bvt, bvt_b = pg["bv"][i % 4]
                        return 128, (lambda q4: akt[:, q4, :]), akt_b, (lambda q4: bkt[:, q4, :]), bkt_b, avt, avt_b, bvt, bvt_b
                    return NS, (lambda q4: akT_s[:, q4, :]), akT_s_b, (lambda q4: bkT_s[:, q4, :]), bkT_s_b, av_s, av_s_b, bv_s, bv_s_b

                def s3_Sc(i):
                    b, j = steps3[i]
                    nkp, akl, akt_b, bkl, bkt_b, avt, avt_b, bvt, bvt_b = s3_ops(i)
                    sl = i % 2
                    if b < NB:
                        Pp(lambda e: e.matmul(out=pS[0:nkp, 0, :], lhsT=ones1[0:1, 0:nkp], rhs=bselt[0:1, b * 512:(b + 1) * 512], start=True, stop=False), r=[ones1_b, bselt_b], w=[pSb[0]])
                        Pp(lambda e: e.matmul(out=pS[0:nkp, 0, :], lhsT=mbs[:, j * 128:(j + 1) * 128], rhs=i8[:, :], start=False, stop=False), r=[mbs_b, i8_b], w=[pSb[0]])
                    else:
                        Pp(lambda e: e.matmul(out=pS[0:nkp, 0, :], lhsT=nbqh[:, 1, :], rhs=i8[:, :], start=True, stop=False), r=[nbqh_b, i8_b], w=[pSb[0]])
                    for q4 in range(4):
                        Pp(lambda e, q4=q4: e.matmul(out=pS[0:nkp, 0, q4 * 128:(q4 + 1) * 128], lhsT=akl(q4), rhs=Qbd_a[:, q4, :, :].rearrange("p a r -> p (a r)"), start=False, stop=(q4 == 3)),
                           r=[akt_b, Qbd_a_b], w=[pSb[0]])
                    pts, pts_b = PTs[sl]
                    Aa(lambda e: e.activation(out=pts[0:nkp, :], in_=pS[0:nkp, 0, :], func=AF.Exp, scale=0.125), r=[pSb[0]], w=[pts_b])
                    if b < NB:
                        Pp(lambda e: e.matmul(out=pS[0:nkp, 1, :], lhsT=ones1[0:1, 0:nkp], rhs=bselt[0:1, b * 512:(b + 1) * 512], start=True, stop=False), r=[ones1_b, bselt_b], w=[pSb[1]])
                    else:
                        Pp(lambda e: e.matmul(out=pS[0:nkp, 1, :], lhsT=nbqh[:, 0, :], rhs=i8[:, :], start=True, stop=False), r=[nbqh_b, i8_b], w=[pSb[1]])
                    for q4 in range(4):
                        Pp(lambda e, q4=q4: e.matmul(out=pS[0:nkp, 1, q4 * 128:(q4 + 1) * 128], lhsT=bkl(q4), rhs=Qbd_b[:, q4, :, :].rearrange("p a r -> p (a r)"), start=False, stop=(q4 == 3)),
                           r=[bkt_b, Qbd_b_b], w=[pSb[1]])
                    ptd, ptd_b = PTd[sl]
                    Aa(lambda e: e.activation(out=ptd[0:nkp, :], in_=pS[0:nkp, 1, :], func=AF.Exp, scale=0.125), r=[pSb[1]], w=[ptd_b])

                def s3_V(i):
                    nkp, akl, akt_b, bkl, bkt_b, avt, avt_b, bvt, bvt_b = s3_ops(i)
                    sl = i % 2
                    first = (i == 0); last = (i == nsteps - 1)
                    pts, pts_b = PTs[sl]; ptd, ptd_b = PTd[sl]
                    if first:
                        for bk_ in range(3):
                            Pp(lambda e, bk_=bk_: e.matmul(out=pO[0:NS, bk_, :], lhsT=zrow[0:1, 0:NS], rhs=zrow[0:1, 0:512], start=True, stop=False), r=[zrow_b], w=[pOb[bk_]])
                    for h in range(8):
                        Pp(lambda e, h=h: e.matmul(out=oA[:, h * 64:(h + 1) * 64], lhsT=pts[0:nkp, h * 64:(h + 1) * 64], rhs=avt[0:nkp, h * 64:(h + 1) * 64], start=False, stop=last),
                           r=[pts_b, avt_b], w=[pOb[0]])
                    Pp(lambda e: e.matmul(out=zA, lhsT=ones1[0:nkp, 0:1], rhs=pts[0:nkp, :], start=first, stop=last), r=[pts_b, ones1_b], w=[pPb[0]])
                    for g in range(8):
                        h_ = g // 2
                        Pp(lambda e, g=g, h_=h_: e.matmul(out=oB[:, g // 4, (g % 4) * 128:(g % 4 + 1) * 128], lhsT=ptd[0:nkp, g * 64:(g + 1) * 64], rhs=bvt[0:nkp, h_ * 128:(h_ + 1) * 128], start=False, stop=last),
                           r=[ptd_b, bvt_b], w=[pOb[1 + g // 4]])
                    Pp(lambda e: e.matmul(out=zB, lhsT=ones1[0:nkp, 0:1], rhs=ptd[0:nkp, :], start=first, stop=last), r=[ptd_b, ones1_b], w=[pPb[0]])

                for n_ in range(nsteps + 3):
                    if n_ < nsteps:
                        s3_L(n_)
                    if 1 <= n_ <= nsteps:
                        s3_T(n_ - 1)
                    if 2 <= n_ <= nsteps + 1:
                        s3_Sc(n_ - 2)
                    if n_ >= 3:
                        s3_V(n_ - 3)
                Vv(lambda e: e.tensor_copy(out=zr[:, 0, :], in_=zA), r=[pPb[0]], w=[zr_b])
                Vv(lambda e: e.tensor_copy(out=zr[:, 1, :], in_=zB), r=[pPb[0]], w=[zr_b])
                for a in range(2):
                    for g in range(8):
                        Pp(lambda e, a=a, g=g: e.matmul(out=pS[0:NS, 0, a * 8 + g:a * 8 + g + 1], lhsT=zr[0:1, a, g * 64:(g + 1) * 64], rhs=identf[0:1, 0:1], start=True, stop=True), r=[zr_b, identf_b], w=[pSb[0]])
                Vv(lambda e: e.reciprocal(out=zc[:, :], in_=pS[0:NS, 0, 0:16]), r=[pSb[0]], w=[zc_b])
                Vv(lambda e: e.tensor_tensor(out=oa_s[:], in0=oA.rearrange("p (h d) -> p h d", h=8), in1=zc[:, 0:8].unsqueeze(2).to_broadcast([NS, 8, 64]), op=ALU.mult), r=[pOb[0], zc_b], w=[oa_s_b])
                Vv(lambda e: e.tensor_copy(out=mixed_s[:, 0:512], in_=oa_s[:].rearrange("p h d -> p (h d)")), r=[oa_s_b], w=[mixed_s_b])
                for h_ in range(4):
                    for c_ in range(2):
                        g = h_ * 2 + c_
                        Vv(lambda e, g=g, c_=c_: e.tensor_scalar(out=bo_s[:, c_, :], in0=oB[:, g // 4, (g % 4) * 128:(g % 4 + 1) * 128], scalar1=zc[:, 8 + g:9 + g], scalar2=None, op0=ALU.mult), r=[pOb[1 + g // 4], zc_b], w=[bo_s_b])
                    diff_finish(bo_s, bo_s_b, mixed_s, mixed_s_b, h_, NS, osm_s, osm_s_b)
                S.dma("sp", lambda e: e.dma_start(out=mbuf[T:T + NS, :], in_=mixed_s[:, :]), r=[mixed_s_b], w=[mbuf_b])
                if debug:
                    S.dma("pool", lambda e: e.dma_start(out=dbg_mixed[:, :], in_=mixed_s[:, :]), r=[mixed_s_b], is_output=True)
                    S.dma("sp", lambda e: e.dma_start(out=dbg_scs[:, :], in_=scs[:, :]), r=[scs_b], is_output=True)
                    S.dma("pool", lambda e: e.dma_start(out=dbg_mbs[:, :], in_=mbs[:, :]), r=[mbs_b], is_output=True)
                    S.dma("sp", lambda e: e.dma_start(out=dbg_z[:, :], in_=zc[:, :]), r=[zc_b], is_output=True)

        S.barrier()
        if "B" in phases:
            with contextlib.ExitStack() as pb_:
                NTT = NT + 1
                xn2T, xn2T_b = sbt(pb_, "xn2T", [128, 8, NTT * 128], BF16)
                gates, gates_b = sbt(pb_, "gates", [128, NTT, 32], F32)
                facc, _ = sbt(pb_, "facc", [128, NTT, D], F32)
                faccb = [Buf("facc%d" % t) for t in range(NTT)]
                wr_sb, wr_b = sbt(pb_, "wr_sb", [128, 8, 36], BF16)
                wout_sb, wout_b = sbt(pb_, "wout_sb", [128, 8, D], BF16)
                gffn_t, gffn_b = sbt(pb_, "gffn_t", [128, D], F32)
                mxl = [sbt(pb_, "mxl%d" % k, [128, D], BF16) for k in range(2)]
                xrl = [sbt(pb_, "xrl%d" % k, [128, D], F32) for k in range(2)]
                mixT, mixT_b = sbt(pb_, "mixT", [128, 8, 128], BF16)
                xn2, xn2_b = sbt(pb_, "xn2", [128, D], BF16)
                rt, rt_b = sbt(pb_, "rt", [128, 160], F32)
                Lall, Lall_b = sbt(pb_, "Lall", [128, NT + 1, 36], F32)
                rq, rq_b = sbt(pb_, "rq", [128, 8, NT + 1], F32)
                goh, _g1 = sbt(pb_, "goh", [128, NT + 1, 4], F32)
                gex, _g2 = sbt(pb_, "gex", [128, NT + 1, 4], F32)
                Mq, _g3 = sbt(pb_, "Mq", [128, NT + 1, 32], F32)
                O1q, _g4 = sbt(pb_, "O1q", [128, NT + 1, 32], F32)
                O2q, _g5 = sbt(pb_, "O2q", [128, NT + 1, 32], F32)
                wgu = [sbt(pb_, "wgu%d" % k, [128, 8, 512], BF16) for k in range(2)]
                wdn = [sbt(pb_, "wdn%d" % k, [128, 2, D], BF16) for k in range(2)]
                sil = [sbt(pb_, "sil%d" % k, [128, 256], F32) for k in range(2)]
                actt = [sbt(pb_, "actt%d" % k, [128, 256], BF16) for k in range(2)]
                actT = [sbt(pb_, "actT%d" % k, [128, 2, 128], BF16) for k in range(2)]
                S.dma("pool", lambda e: e.dma_start(out=wout_sb[:], in_=w_out.rearrange("(c p) n -> p c n", p=128)), w=[wout_b])
                S.dma("sp", lambda e: e.dma_start(out=gffn_t[:], in_=g_ffn.partition_broadcast(128)), w=[gffn_b])
                Vv(lambda e: e.memset(gates[:], 0.0), w=[gates_b])
                Vv(lambda e: e.memset(Lall[:], 0.0), w=[Lall_b])
                S.dma("pool", lambda e: e.dma_start(out=wr_sb[:], in_=w_r.rearrange("(c p) n -> p c n", p=128)), w=[wr_b])
                Vv(lambda e: e.memset(xn2T[:, :, NT * 128 + NS:NTT * 128], 0.0), w=[xn2T_b])

                def rows(t):
                    return (t * 128, 128) if t < NT else (T, NS)

                for t in range(NTT):
                    r0, n = rows(t)
                    mx_t, mx_b = mxl[t % 2]
                    xr_t, xr_b = xrl[t % 2]
                    x_src = xp[r0:r0 + n, :] if t < NT else xs[:, :]
                    S.dma("sp", lambda e, mx_t=mx_t, r0=r0, n=n: e.dma_start(out=mx_t[0:n, :], in_=mbuf[r0:r0 + n, :]), r=[mbuf_b], w=[mx_b])
                    S.dma("sp", lambda e, xr_t=xr_t, x_src=x_src, n=n: e.dma_start(out=xr_t[0:n, :], in_=x_src), w=[xr_b])
                    for c in range(8):
                        Pp(lambda e, c=c, n=n, mx_t=mx_t: e.transpose(out=pT[:, c * 128:c * 128 + n], in_=mx_t[0:n, c * 128:(c + 1) * 128], identity=ident[0:n, 0:n]), r=[mx_b, ident_b], w=[pT_b])
                    Aa(lambda e, n=n: e.copy(out=mixT[:, :, 0:n], in_=pT[:, :].rearrange("p (c t) -> p c t", c=8)[:, :, 0:n]), r=[pT_b], w=[mixT_b])
                    h_t = facc[:, t, :]
                    h_b = faccb[t]
                    for half in range(2):
                        for c in range(8):
                            Pp(lambda e, c=c, half=half, n=n: e.matmul(out=pO[0:n, half, :], lhsT=mixT[:, c, 0:n], rhs=wout_sb[:, c, half * 512:(half + 1) * 512], start=(c == 0), stop=(c == 7)),
                               r=[mixT_b, wout_b], w=[pOb[half]])
                        Vv(lambda e, half=half, n=n, h_t=h_t, xr_t=xr_t: e.tensor_tensor(out=h_t[0:n, half * 512:(half + 1) * 512], in0=pO[0:n, half, :], in1=xr_t[0:n, half * 512:(half + 1) * 512], op=ALU.add),
                           r=[pOb[half], xr_b], w=[h_b])
                    Aa(lambda e, h_t=h_t, n=n: e.activation(out=xn2[0:n, :], in_=h_t[0:n, :], func=AF.Square, accum_out=rt[0:n, 0:1]), r=[h_b], w=[xn2_b, rt_b])
                    rstd_from_ssq(rt[0:n, 0:1], 1.0 / D, [rt_b])
                    Vv(lambda e, h_t=h_t, n=n: e.scalar_tensor_tensor(out=xn2[0:n, :], in0=h_t[0:n, :], scalar=rt[0:n, 0:1], in1=gffn_t[0:n, :], op0=ALU.mult, op1=ALU.mult), r=[h_b, rt_b, gffn_b], w=[xn2_b])
                    for c in range(8):
                        Pp(lambda e, c=c, n=n: e.transpose(out=pT[:, c * 128:c * 128 + n], in_=xn2[0:n, c * 128:(c + 1) * 128], identity=ident[0:n, 0:n]), r=[xn2_b, ident_b], w=[pT_b])
                    Aa(lambda e, t=t, n=n: e.copy(out=xn2T[:, :, t * 128:t * 128 + n], in_=pT[:, :].rearrange("p (c t) -> p c t", c=8)[:, :, 0:n]), r=[pT_b], w=[xn2T_b])
                    for c in range(8):
                        Pp(lambda e, c=c, t=t, n=n: e.matmul(out=pS[0:n, 0, 0:36], lhsT=xn2T[:, c, t * 128:t * 128 + n], rhs=wr_sb[:, c, :], start=(c == 0), stop=(c == 7)), r=[xn2T_b, wr_b], w=[pSb[0]])
                    Vv(lambda e, n=n, t=t: e.tensor_tensor(out=Lall[0:n, t, :], in0=pS[0:n, 0, 0:36], in1=br_t[0:n, :], op=ALU.add), r=[pSb[0], br_b], w=[Lall_b])

                NTq = NTT
                Gv = Lall[:, :, 0:4]; Ev = Lall[:, :, 4:36]
                RB = [Lall_b, rq_b]

                def bc(ap2, k):
                    return ap2.unsqueeze(2).to_broadcast([128, NTq, k])
                Vv(lambda e: e.tensor_reduce(out=rq[:, 0, :], in_=Gv, axis=AX.X, op=ALU.max), r=RB, w=[rq_b])
                Vv(lambda e: e.tensor_tensor(out=goh[:], in0=Gv, in1=bc(rq[:, 0, :], 4), op=ALU.is_ge), r=RB, w=[rq_b])
                Vv(lambda e: e.tensor_tensor(out=gex[:], in0=Gv, in1=bc(rq[:, 0, :], 4), op=ALU.subtract), r=RB, w=[rq_b])
                Aa(lambda e: e.activation(out=gex[:], in_=gex[:], func=AF.Exp), r=RB, w=[rq_b])
                Vv(lambda e: e.tensor_reduce(out=rq[:, 1, :], in_=gex[:], axis=AX.X, op=ALU.add), r=RB, w=[rq_b])
                Vv(lambda e: e.reciprocal(out=rq[:, 1, :], in_=rq[:, 1, :]), r=RB, w=[rq_b])
                Vv(lambda e: e.tensor_scalar(out=goh[:], in0=goh[:], scalar1=-1.0, scalar2=1.0e9, op0=ALU.add, op1=ALU.mult), r=RB, w=[rq_b])
                for g_ in range(4):
                    Vv(lambda e, g_=g_: e.tensor_tensor(out=Mq[:, :, g_ * 8:(g_ + 1) * 8], in0=Lall[:, :, 4 + g_ * 8:4 + (g_ + 1) * 8],
                                                        in1=goh[:, :, g_:g_ + 1].to_broadcast([128, NTq, 8]), op=ALU.add), r=RB, w=[rq_b])
                Vv(lambda e: e.tensor_reduce(out=rq[:, 2, :], in_=Mq[:], axis=AX.X, op=ALU.max), r=RB, w=[rq_b])
                Vv(lambda e: e.tensor_tensor(out=O1q[:], in0=Mq[:], in1=bc(rq[:, 2, :], 32), op=ALU.is_ge), r=RB, w=[rq_b])
                Vv(lambda e: e.scalar_tensor_tensor(out=Mq[:], in0=O1q[:], scalar=-1.0e9, in1=Mq[:], op0=ALU.mult, op1=ALU.add), r=RB, w=[rq_b])
                Vv(lambda e: e.tensor_reduce(out=rq[:, 3, :], in_=Mq[:], axis=AX.X, op=ALU.max), r=RB, w=[rq_b])
                Vv(lambda e: e.tensor_tensor(out=O2q[:], in0=Mq[:], in1=bc(rq[:, 3, :], 32), op=ALU.is_ge), r=RB, w=[rq_b])
                Vv(lambda e: e.tensor_tensor(out=rq[:, 4, :], in0=rq[:, 3, :], in1=rq[:, 2, :], op=ALU.subtract), r=RB, w=[rq_b])
                Aa(lambda e: e.activation(out=rq[:, 4, :], in_=rq[:, 4, :], func=AF.Exp), r=RB, w=[rq_b])
                Vv(lambda e: e.tensor_scalar(out=rq[:, 5, :], in0=rq[:, 4, :], scalar1=1.0, scalar2=None, op0=ALU.add), r=RB, w=[rq_b])
                Vv(lambda e: e.reciprocal(out=rq[:, 5, :], in_=rq[:, 5, :]), r=RB, w=[rq_b])
                Vv(lambda e: e.tensor_tensor(out=rq[:, 5, :], in0=rq[:, 5, :], in1=rq[:, 1, :], op=ALU.mult), r=RB, w=[rq_b])
                Vv(lambda e: e.tensor_tensor(out=rq[:, 6, :], in0=rq[:, 5, :], in1=rq[:, 4, :], op=ALU.mult), r=RB, w=[rq_b])
                Vv(lambda e: e.tensor_tensor(out=O1q[:], in0=O1q[:], in1=bc(rq[:, 5, :], 32), op=ALU.mult), r=RB, w=[rq_b])
                Vv(lambda e: e.tensor_tensor(out=O2q[:], in0=O2q[:], in1=bc(rq[:, 6, :], 32), op=ALU.mult), r=RB, w=[rq_b])
                Vv(lambda e: e.tensor_tensor(out=gates[:], in0=O1q[:], in1=O2q[:], op=ALU.add), r=RB, w=[gates_b])

                items = [(ex, t) for ex in range(NEXP) for t in range(NTT)]
                NI = len(items)

                def load_w(ex):
                    wg_t, wg_b = wgu[ex % 2]
                    wd_t, wd_b = wdn[ex % 2]
                    S.dma("pool", lambda e: e.dma_start(out=wg_t[:, :, 0:256], in_=w_g[ex].rearrange("(c p) n -> p c n", p=128)), w=[wg_b])
                    S.dma("pool", lambda e: e.dma_start(out=wg_t[:, :, 256:512], in_=w_u[ex].rearrange("(c p) n -> p c n", p=128)), w=[wg_b])
                    S.dma("pool", lambda e: e.dma_start(out=wd_t[:, :, :], in_=w_d[ex].rearrange("(c p) n -> p c n", p=128)), w=[wd_b])

                def stage1(i):
                    ex, t = items[i]
                    k2 = i % 2
                    wg_t, wg_b = wgu[ex % 2]
                    for c in range(8):
                        Pp(lambda e, c=c: e.matmul(out=pP[:, k2, :], lhsT=xn2T[:, c, t * 128:(t + 1) * 128], rhs=wg_t[:, c, :], start=(c == 0), stop=(c == 7)),
                           r=[xn2T_b, wg_b], w=[pPb[k2]])
                    s_t, s_b = sil[k2]
                    a_t, a_b = actt[k2]
                    Aa(lambda e: e.activation(out=s_t[:, :], in_=pP[:, k2, 0:256], func=AF.Silu), r=[pPb[k2]], w=[s_b])
                    Vv(lambda e: e.scalar_tensor_tensor(out=a_t[:, :], in0=pP[:, k2, 256:512], scalar=gates[:, t, ex:ex + 1], in1=s_t[:, :], op0=ALU.mult, op1=ALU.mult),
                       r=[pPb[k2], s_b, gates_b], w=[a_b])


                pT_alt = pO[:, 2, :].bitcast(BF16)
                tbanks = [(pT, pT_b), (pT_alt, pOb[2])]

                def stage2(i):
                    k2 = i % 2
                    a_t, a_b = actt[k2]
                    aT_t, aT_b = actT[k2]
                    tb, tb_b = tbanks[k2]
                    for c2 in range(2):
                        Pp(lambda e, c2=c2: e.transpose(out=tb[:, c2 * 128:(c2 + 1) * 128], in_=a_t[:, c2 * 128:(c2 + 1) * 128], identity=ident[:]), r=[a_b, ident_b], w=[tb_b])
                    Aa(lambda e: e.copy(out=aT_t[:, :, :], in_=tb[:, 0:256].rearrange("p (c t) -> p c t", c=2)), r=[tb_b], w=[aT_b])

                def stage3(i):
                    ex, t = items[i]
                    k2 = i % 2
                    aT_t, aT_b = actT[k2]
                    wd_t, wd_b = wdn[ex % 2]
                    for half in range(2):
                        if k2 == 0:
                            oap = pS[:, half, :]; obuf = pSb[half]
                        else:
                            oap = pO[:, half, :]; obuf = pOb[half]
                        for c2 in range(2):
                            Pp(lambda e, c2=c2, half=half, oap=oap: e.matmul(out=oap, lhsT=aT_t[:, c2, :], rhs=wd_t[:, c2, half * 512:(half + 1) * 512], start=(c2 == 0), stop=(c2 == 1)),
                               r=[aT_b, wd_b], w=[obuf])
                        fa = facc[:, t, half * 512:(half + 1) * 512]
                        Vv(lambda e, fa=fa, oap=oap: e.tensor_tensor(out=fa, in0=oap, in1=fa, op=ALU.add), r=[obuf, faccb[t]], w=[faccb[t]])

                load_w(0)
                load_w(1)
                for i in range(NI + 2):
                    if i < NI:
                        stage1(i)
                    if 1 <= i <= NI:
                        stage2(i - 1)
                    if 2 <= i:
                        stage3(i - 2)
                        exd, td = items[i - 2]
                        if td == NTT - 1 and exd + 2 < NEXP:
                            load_w(exd + 2)
                for t in range(NTT):
                    r0, n = rows(t)
                    if t < NT:
                        S.dma("sp", lambda e, r0=r0, n=n, t=t: e.dma_start(out=y_p[r0:r0 + n, :], in_=facc[0:n, t, :]), r=[faccb[t]], is_output=True)
                    else:
                        S.dma("sp", lambda e, n=n, t=t: e.dma_start(out=y_s[0:n, :], in_=facc[0:n, t, :]), r=[faccb[t]], is_output=True)
        S.finish()
    return nc


def _consts():
    half = 32
    inv = (10000.0 ** (-np.arange(half, dtype=np.float32) / half)).astype(np.float32)

    def table(pos):
        ang = pos.astype(np.float32)[:, None] * inv[None, :]
        c = np.cos(ang).astype(np.float32); s = np.sin(ang).astype(np.float32)
        return np.concatenate([c, c, -s, s], axis=1).astype(np.float32)

    rope_p = table(np.arange(T))
    rope_s = table(np.tile(2048 + np.arange(4), NB))
    q = np.arange(128)[:, None]; k = np.arange(128)[None, :]
    cm = np.where(k <= q, 0.0, NEG).astype(np.float32)
    cm30 = np.where(k <= q, 0.0, -BIG).astype(np.float32)
    r = np.arange(NS)
    same = (r[:, None] // 4) == (r[None, :] // 4)
    caus = (r[None, :] % 4) <= (r[:, None] % 4)
    nbq = np.where(same & caus, 0.0, NEG).astype(np.float32)
    nb30 = np.where(np.arange(4)[None, :] <= (r[:, None] % 4), 0.0, -BIG).astype(np.float32)
    bdiag = same.astype(np.float32)
    bsel = np.full((NB, 8, NS), NEG, np.float32)
    for b in range(NB):
        bsel[b, :, b * 4:(b + 1) * 4] = 0.0
    i8 = np.tile(np.eye(64, dtype=np.float32), (1, 8))
    return dict(rope_p=rope_p, rope_s=rope_s, c_ident=np.eye(128, dtype=np.float32), c_cm=cm, c_cm30=cm30,
                c_nbq=nbq, c_nb30=nb30, c_bdiag=bdiag, c_bsel=bsel.reshape(1, NB * 512), c_i8=i8)


def make_in_maps(inp, cores):
    f = lambda a: np.ascontiguousarray(np.asarray(a, dtype=np.float32))
    n_pool = inp["cache_a_k"].shape[1]
    cons = _consts()
    shared = dict(
        cak=f(inp["cache_a_k"]).reshape(n_pool * 128, 512), cav=f(inp["cache_a_v"]).reshape(n_pool * 128, 512),
        cbk=f(inp["cache_b_k"]).reshape(n_pool * 128, 512), cbv=f(inp["cache_b_v"]).reshape(n_pool * 128, 512),
        cik=f(inp["cache_idx_k"]).reshape(n_pool * 128, 64),
        g_mix=f(inp["g_mix"]).reshape(1, D), w_in=f(inp["w_in"]).reshape(D, INW),
        gains=np.concatenate([f(inp[k]).reshape(1, 64) for k in ("q_norm_a", "k_norm_a", "q_norm_b", "k_norm_b")], axis=1),
        lams=np.concatenate([f(inp[k]).reshape(1, 64) for k in ("lambda_q1", "lambda_k1", "lambda_q2", "lambda_k2")], axis=1),
        subln=f(inp["subln_b"]).reshape(1, 128), w_out=f(inp["w_out"]).reshape(D, D), g_ffn=f(inp["g_ffn"]).reshape(1, D),
        w_r=np.concatenate([f(inp["w_router_group"]).reshape(D, 4), f(inp["w_router_expert"]).reshape(D, 32)], axis=1),
        b_r=np.concatenate([f(inp["b_router_group"]).reshape(1, 4), f(inp["b_router_expert"]).reshape(1, 32)], axis=1),
        w_g=f(inp["w_exp_gate"]).reshape(NEXP, D, DE), w_u=f(inp["w_exp_up"]).reshape(NEXP, D, DE), w_d=f(inp["w_exp_down"]).reshape(NEXP, DE, D),
        **cons)
    xp = f(inp["x_prompt"]); xs = f(inp["x_sample"])
    pt = np.ascontiguousarray(np.asarray(inp["page_table"], dtype=np.int32))
    maps = []
    for c in cores:
        m = dict(shared)
        m["xp"] = xp[c]
        m["xs"] = xs[c * NB:(c + 1) * NB].reshape(NS, D)
        m["ptab"] = pt[c * NB:(c + 1) * NB].reshape(1, NB * NPG)
        m["ptabT"] = np.ascontiguousarray(pt[c * NB:(c + 1) * NB].T).reshape(1, NPG * NB)
        maps.append(m)
    return maps, n_pool


def assemble(results, n_cores=8):
    def cat(name, shp):
        return np.stack([np.asarray(r[name], dtype=np.float32) for r in results], axis=0).reshape(shp)
    B = n_cores
    y_p = cat("y_p", (B, T, D)); y_s = cat("y_s", (B * NB, 4, D))
    return (y_p, y_s,
            cat("oak_p", (1, B, T, 8, 64)), cat("oav_p", (1, B, T, 8, 64)), cat("oik_p", (1, B, T, 64)),
            cat("obk_p", (1, B, T, 4, 2, 64)), cat("obv_p", (1, B, T, 4, 128)),
            cat("oak_s", (1, B * NB, 4, 8, 64)), cat("oav_s", (1, B * NB, 4, 8, 64)), cat("oik_s", (1, B * NB, 4, 64)),
            cat("obk_s", (1, B * NB, 4, 4, 2, 64)), cat("obv_s", (1, B * NB, 4, 4, 128)))


def kernel(**inputs):
    maps, n_pool = make_in_maps(inputs, list(range(8)))
    nc = build(n_pool)
    res = run_bass_kernel_spmd(nc, maps, core_ids=list(range(8)))
    return assemble(res.results, 8)
```
